# Optimizing a Trainium2 kernel written in Bass

```python
import math
import jax
import jax.numpy as jnp
from jax import lax
import numpy as np


D_MODEL = 1024
BATCH = 8
SEQ = 2048
DEPTH = 4

GRID_W = 64
CTX_LEN = 256
N_HEADS = 8
HEAD_DIM = 64
V_DIM = 2 * HEAD_DIM
QK_W = N_HEADS * 2 * HEAD_DIM
ATTN_W = N_HEADS * V_DIM
Q_BLOCK = 128
ROPE_BASE = 10000.0
ROPE_AXIS_DIM = HEAD_DIM // 2
SSM_W = D_MODEL // 2
SSM_GROUP = 16
SSM_GROUPS = SSM_W // SSM_GROUP
SSM_STATE = 64
N_GROUPS = 4
EXPERTS_PER_GROUP = 8
N_EXPERTS = N_GROUPS * EXPERTS_PER_GROUP
TOP_K = 2
EXPERT_HIDDEN = D_MODEL // 2
MOE_BLOCK = 128
KVU_W = QK_W + ATTN_W + SSM_W
IN_W = KVU_W + QK_W + 2 * D_MODEL
ALPHA = (2.0 * DEPTH) ** 0.25
BETA = (8.0 * DEPTH) ** -0.25
LN_EPS = 1e-5
MOD_INIT = 0.5

kernel_name = 'hybrid_diffattn_s5_hmoe_prefix_trunk'


def _layer_norm(x, g, b):
    xf = x.astype(jnp.float32)
    xc = xf - xf.mean(-1, keepdims=True)
    var = (xc * xc).mean(-1, keepdims=True)
    return (xc * lax.rsqrt(var + LN_EPS) * g + b).astype(x.dtype)


def _axial_rope_tables(n_tokens):
    rows = n_tokens // GRID_W
    row = jnp.repeat(jnp.arange(rows, dtype=jnp.float32), GRID_W)
    col = jnp.tile(jnp.arange(GRID_W, dtype=jnp.float32), rows)
    inv = 1.0 / (ROPE_BASE ** (jnp.arange(0, ROPE_AXIS_DIM, 2, dtype=jnp.float32) / ROPE_AXIS_DIM))
    ang = jnp.concatenate([row[:, None] * inv, col[:, None] * inv], axis=-1)
    return jnp.cos(ang), jnp.sin(ang)


def _apply_axial_rope(t, cos, sin):
    half = ROPE_AXIS_DIM // 2
    tr = t.reshape(t.shape[:-1] + (2, 2, half))
    t1, t2 = tr[..., 0, :], tr[..., 1, :]
    c = cos.reshape(1, cos.shape[0], 1, 1, 2, half)
    s = sin.reshape(1, sin.shape[0], 1, 1, 2, half)
    return jnp.stack([t1 * c - t2 * s, t1 * s + t2 * c], axis=-2).reshape(t.shape)


def _diff_attention(q, k, v, lam):
    s = jnp.einsum('bqhmd,bkhmd->mbhqk', q, k)
    p = jax.nn.softmax(s, axis=-1)
    a = p[0] - lam * p[1]
    return jnp.einsum('bhqk,bkhe->bqhe', a, v)


def _head_norm(o, g, lam_init):
    on = o * lax.rsqrt(jnp.mean(o * o, axis=-1, keepdims=True) + LN_EPS) * g.astype(jnp.float32) * (1.0 - lam_init)
    return on.reshape(o.shape[0], o.shape[1], ATTN_W)


def _linear_scan(a_bar, bu, reverse):
    a = jnp.broadcast_to(a_bar, bu.shape)

    def combine(e1, e2):
        a1, b1 = e1
        a2, b2 = e2
        return a1 * a2, a2 * b1 + b2

    _, h = lax.associative_scan(combine, (a, bu), axis=1, reverse=reverse)
    return h


def _s5_bidirectional(uc, ux, a_re, a_im, log_dt, b_re, b_im, c_re, c_im, d_skip, need_ctx):
    f32 = jnp.float32
    bsz, n_ctx, _ = uc.shape
    n_lat = ux.shape[1]
    ucf, uxf = uc.astype(f32), ux.astype(f32)
    ucg = ucf.reshape(bsz, n_ctx, SSM_GROUPS, SSM_GROUP)
    uxg = uxf.reshape(bsz, n_lat, SSM_GROUPS, SSM_GROUP)
    d = d_skip.astype(f32)
    yx = uxf * d
    yc = ucf * d if need_ctx else None
    for di, rev in enumerate((False, True)):
        lam = lax.complex(a_re[di].astype(f32), a_im[di].astype(f32))
        dt = jnp.exp(log_dt[di].astype(f32))[:, None]
        a_bar = jnp.exp(lam * dt)
        b_bar = ((a_bar - 1.0) / lam)[..., None] * lax.complex(b_re[di].astype(f32), b_im[di].astype(f32))
        cm = lax.complex(c_re[di].astype(f32), c_im[di].astype(f32))
        h_ctx = _linear_scan(a_bar, jnp.einsum('btgc,gpc->btgp', ucg, b_bar), rev)
        h0 = h_ctx[:, 0] if rev else h_ctx[:, -1]
        bu_x = jnp.einsum('btgc,gpc->btgp', uxg, b_bar)
        first = n_lat - 1 if rev else 0
        bu_x = bu_x.at[:, first].add(a_bar * h0)
        h_lat = _linear_scan(a_bar, bu_x, rev)
        yx = yx + jnp.einsum('btgp,gcp->btgc', h_lat, cm).real.reshape(bsz, n_lat, SSM_W)
        if need_ctx:
            yc = yc + jnp.einsum('btgp,gcp->btgc', h_ctx, cm).real.reshape(bsz, n_ctx, SSM_W)
    return yx, yc


def _glu(y, w, b):
    y = jax.nn.gelu(y)
    return y * jax.nn.sigmoid(y @ w + b)


def _merge_branches(attn, ssm, ga, gs, w_pa, w_ps, w_o):
    m = jax.nn.sigmoid(ga.astype(jnp.float32)) * (attn @ w_pa) + jax.nn.sigmoid(gs.astype(jnp.float32)) * (ssm @ w_ps)
    return (m @ w_o).astype(ga.dtype)


def _token_mixer(hx, hc, lp, lam_init, need_ctx, rope_cos, rope_sin):
    f32 = jnp.float32
    bsz, n_lat, _ = hx.shape
    splits = [QK_W, QK_W + ATTN_W, KVU_W, KVU_W + QK_W, KVU_W + QK_W + D_MODEL]
    w_in = lp['w_in']
    kx, vx, ux, qx, gax, gsx = jnp.split(hx @ w_in, splits, axis=-1)
    if need_ctx:
        kc, vc, uc, qc, gac, gsc = jnp.split(hc @ w_in, splits, axis=-1)
    else:
        kc, vc, uc = jnp.split(hc @ w_in[:, :KVU_W], splits[:2], axis=-1)
    scale = HEAD_DIM ** -0.5

    def qk_heads(t):
        return t.astype(f32).reshape(bsz, t.shape[1], N_HEADS, 2, HEAD_DIM)

    def v_heads(t):
        return t.astype(f32).reshape(bsz, t.shape[1], N_HEADS, V_DIM)

    qx4 = _apply_axial_rope(qk_heads(qx), rope_cos, rope_sin) * scale
    kx4 = _apply_axial_rope(qk_heads(kx), rope_cos, rope_sin)
    kc4, vc4 = qk_heads(kc), v_heads(vc)
    k_all = jnp.concatenate([kc4, kx4], axis=1)
    v_all = jnp.concatenate([vc4, v_heads(vx)], axis=1)
    lq1, lk1 = lp['lam_q1'].astype(f32), lp['lam_k1'].astype(f32)
    lq2, lk2 = lp['lam_q2'].astype(f32), lp['lam_k2'].astype(f32)
    lam = jnp.exp(jnp.sum(lq1 * lk1)) - jnp.exp(jnp.sum(lq2 * lk2)) + lam_init
    n_blk = n_lat // Q_BLOCK
    q_blocks = qx4.reshape(bsz, n_blk, Q_BLOCK, N_HEADS, 2, HEAD_DIM).transpose(1, 0, 2, 3, 4, 5)
    o_x = lax.map(lambda qb: _diff_attention(qb, k_all, v_all, lam), q_blocks)
    o_x = o_x.transpose(1, 0, 2, 3, 4).reshape(bsz, n_lat, N_HEADS, V_DIM)
    attn_x = _head_norm(o_x, lp['subln_g'], lam_init)

    ssm_x, ssm_c = _s5_bidirectional(uc, ux, lp['ssm_a_re'], lp['ssm_a_im'], lp['ssm_log_dt'], lp['ssm_b_re'],
                                     lp['ssm_b_im'], lp['ssm_c_re'], lp['ssm_c_im'], lp['ssm_d'], need_ctx)
    yx = _merge_branches(attn_x, _glu(ssm_x, lp['w_glu'], lp['b_glu']), gax, gsx, lp['w_pa'], lp['w_ps'], lp['w_o'])
    yc = None
    if need_ctx:
        attn_c = _head_norm(_diff_attention(qk_heads(qc) * scale, kc4, vc4, lam), lp['subln_g'], lam_init)
        yc = _merge_branches(attn_c, _glu(ssm_c, lp['w_glu'], lp['b_glu']), gac, gsc, lp['w_pa'], lp['w_ps'], lp['w_o'])
    return yx, yc


def _hier_moe(h, wg, bg, we, be, w1, w3, w2):
    f32 = jnp.float32
    n_tok, d = h.shape
    hf = h.astype(f32)
    g_logits = hf @ wg.astype(f32) + bg.astype(f32)
    g_prob = jax.nn.softmax(g_logits, axis=-1)
    g_idx = jnp.argmax(g_logits, axis=-1).astype(jnp.int32)
    g_top = jnp.take_along_axis(g_prob, g_idx[:, None], axis=1)
    e_logits = (hf @ we.astype(f32) + be.astype(f32)).reshape(n_tok, N_GROUPS, EXPERTS_PER_GROUP)
    e_sel = jnp.take_along_axis(e_logits, g_idx[:, None, None], axis=1)[:, 0]
    top_val, top_idx = lax.top_k(e_sel, TOP_K)
    weights = jax.nn.softmax(top_val, axis=-1) * g_top
    expert = g_idx[:, None] * EXPERTS_PER_GROUP + top_idx.astype(jnp.int32)
    n_rows = n_tok * TOP_K
    n_blocks = (n_rows + N_EXPERTS * (MOE_BLOCK - 1) + MOE_BLOCK - 1) // MOE_BLOCK
    cap = n_blocks * MOE_BLOCK
    flat_e = expert.reshape(-1)
    flat_t = jnp.repeat(jnp.arange(n_tok, dtype=jnp.int32), TOP_K)
    flat_w = weights.reshape(-1)
    order = jnp.argsort(flat_e)
    se = flat_e[order]
    counts = jnp.bincount(flat_e, length=N_EXPERTS).astype(jnp.int32)
    padded = (counts + MOE_BLOCK - 1) // MOE_BLOCK * MOE_BLOCK
    pad_end = jnp.cumsum(padded)
    pad_start = pad_end - padded
    start = jnp.cumsum(counts) - counts
    dest = pad_start[se] + jnp.arange(n_rows, dtype=jnp.int32) - start[se]
    row_tok = jnp.full((cap,), n_tok, jnp.int32).at[dest].set(flat_t[order])
    row_w = jnp.zeros((cap,), f32).at[dest].set(flat_w[order])
    blk_expert = jnp.minimum(jnp.searchsorted(pad_end, jnp.arange(n_blocks, dtype=jnp.int32) * MOE_BLOCK, side='right'),
                             N_EXPERTS - 1).astype(jnp.int32)
    h_pad = jnp.concatenate([h, jnp.zeros((1, d), h.dtype)], axis=0)
    xs = h_pad[row_tok].reshape(n_blocks, MOE_BLOCK, d)

    def expert_block(args):
        xb, e = args
        return (jax.nn.silu(xb @ w1[e]) * (xb @ w3[e])) @ w2[e]

    ys = lax.map(expert_block, (xs, blk_expert)).reshape(cap, d)
    out = jnp.zeros((n_tok + 1, d), ys.dtype).at[row_tok].add(ys * row_w[:, None].astype(ys.dtype))
    return out[:n_tok]


def setup_inputs(seed: int = 0) -> dict:
    key = jax.random.key(seed)
    ks = jax.random.split(key, 40)
    f32 = jnp.float32

    def nrm(i, shape, scale):
        return jax.random.normal(ks[i], shape, f32) * scale

    D = D_MODEL
    G, P, E, F = SSM_GROUPS, SSM_STATE, N_EXPERTS, EXPERT_HIDDEN
    a_im = jnp.broadcast_to(jnp.pi * jnp.arange(P, dtype=f32), (DEPTH, 2, G, P))
    return {
        'x': nrm(0, (BATCH, SEQ, D), 1.0),
        'c': nrm(1, (BATCH, D), 1.0),
        'ctx': nrm(2, (BATCH, CTX_LEN, D), 1.0),
        'c_ctx': nrm(3, (D,), 1.0),
        'w_mod': nrm(4, (DEPTH, D, 6 * D), MOD_INIT * D ** -0.5),
        'b_mod': nrm(5, (DEPTH, 6 * D), 0.01),
        'w_in': nrm(6, (DEPTH, D, IN_W), D ** -0.5),
        'lam_q1': nrm(7, (DEPTH, HEAD_DIM), 0.1),
        'lam_k1': nrm(8, (DEPTH, HEAD_DIM), 0.1),
        'lam_q2': nrm(9, (DEPTH, HEAD_DIM), 0.1),
        'lam_k2': nrm(10, (DEPTH, HEAD_DIM), 0.1),
        'subln_g': 1.0 + nrm(11, (DEPTH, V_DIM), 0.02),
        'ssm_a_re': -0.5 * (1.0 + nrm(12, (DEPTH, 2, G, P), 0.01)),
        'ssm_a_im': a_im,
        'ssm_log_dt': jax.random.uniform(ks[13], (DEPTH, 2, G), f32, math.log(1e-3), math.log(1e-1)),
        'ssm_b_re': nrm(14, (DEPTH, 2, G, P, SSM_GROUP), (2 * SSM_GROUP) ** -0.5),
        'ssm_b_im': nrm(15, (DEPTH, 2, G, P, SSM_GROUP), (2 * SSM_GROUP) ** -0.5),
        'ssm_c_re': nrm(16, (DEPTH, 2, G, SSM_GROUP, P), P ** -0.5),
        'ssm_c_im': nrm(17, (DEPTH, 2, G, SSM_GROUP, P), P ** -0.5),
        'ssm_d': nrm(18, (DEPTH, SSM_W), 0.5),
        'w_glu': nrm(19, (DEPTH, SSM_W, SSM_W), SSM_W ** -0.5),
        'b_glu': nrm(20, (DEPTH, SSM_W), 0.01),
        'w_pa': nrm(21, (DEPTH, ATTN_W, D), ATTN_W ** -0.5),
        'w_ps': nrm(22, (DEPTH, SSM_W, D), SSM_W ** -0.5),
        'w_o': nrm(23, (DEPTH, D, D), BETA * D ** -0.5),
        'ln1_g': 1.0 + nrm(24, (DEPTH, D), 0.02),
        'ln1_b': nrm(25, (DEPTH, D), 0.01),
        'router_g_w': nrm(26, (DEPTH, D, N_GROUPS), D ** -0.5),
        'router_g_b': nrm(27, (DEPTH, N_GROUPS), 0.01),
        'router_e_w': nrm(28, (DEPTH, D, E), D ** -0.5),
        'router_e_b': nrm(29, (DEPTH, E), 0.01),
        'moe_w1': nrm(30, (DEPTH, E, D, F), D ** -0.5),
        'moe_w3': nrm(31, (DEPTH, E, D, F), D ** -0.5),
        'moe_w2': nrm(32, (DEPTH, E, F, D), BETA * F ** -0.5),
        'ln2_g': 1.0 + nrm(33, (DEPTH, D), 0.02),
        'ln2_b': nrm(34, (DEPTH, D), 0.01),
    }


def reference(x, c, ctx, c_ctx, w_mod, b_mod, w_in, lam_q1, lam_k1, lam_q2, lam_k2, subln_g,
              ssm_a_re, ssm_a_im, ssm_log_dt, ssm_b_re, ssm_b_im, ssm_c_re, ssm_c_im, ssm_d,
              w_glu, b_glu, w_pa, w_ps, w_o, ln1_g, ln1_b,
              router_g_w, router_g_b, router_e_w, router_e_b, moe_w1, moe_w3, moe_w2, ln2_g, ln2_b):
    bsz, n_lat, _ = x.shape
    rope_cos, rope_sin = _axial_rope_tables(n_lat)
    silu_c = jax.nn.silu(c)
    silu_cc = jax.nn.silu(c_ctx)
    xl, xc = x, ctx
    for l in range(DEPTH):
        last = l == DEPTH - 1
        lam_init = 0.8 - 0.6 * math.exp(-0.3 * l)
        lp = dict(w_in=w_in[l], lam_q1=lam_q1[l], lam_k1=lam_k1[l], lam_q2=lam_q2[l], lam_k2=lam_k2[l],
                  subln_g=subln_g[l], ssm_a_re=ssm_a_re[l], ssm_a_im=ssm_a_im[l], ssm_log_dt=ssm_log_dt[l],
                  ssm_b_re=ssm_b_re[l], ssm_b_im=ssm_b_im[l], ssm_c_re=ssm_c_re[l], ssm_c_im=ssm_c_im[l],
                  ssm_d=ssm_d[l], w_glu=w_glu[l], b_glu=b_glu[l], w_pa=w_pa[l], w_ps=w_ps[l], w_o=w_o[l])
        mod_x = (silu_c @ w_mod[l] + b_mod[l])[:, None, :]
        sh1, sc1, g1, sh2, sc2, g2 = jnp.split(mod_x, 6, axis=-1)
        n_mod_c = 2 if last else 6
        cmod = jnp.split(silu_cc @ w_mod[l][:, :n_mod_c * D_MODEL] + b_mod[l][:n_mod_c * D_MODEL], n_mod_c)
        hx = xl * (1.0 + sc1) + sh1
        hc = xc * (1.0 + cmod[1]) + cmod[0]
        yx, yc = _token_mixer(hx, hc, lp, lam_init, not last, rope_cos, rope_sin)
        xl = _layer_norm(ALPHA * xl + g1 * yx, ln1_g[l], ln1_b[l])
        hx = xl * (1.0 + sc2) + sh2
        if last:
            fx = _hier_moe(hx.reshape(-1, D_MODEL), router_g_w[l], router_g_b[l], router_e_w[l], router_e_b[l],
                           moe_w1[l], moe_w3[l], moe_w2[l]).reshape(hx.shape)
        else:
            xc = _layer_norm(ALPHA * xc + cmod[2] * yc, ln1_g[l], ln1_b[l])
            hc = xc * (1.0 + cmod[4]) + cmod[3]
            rows = jnp.concatenate([hx.reshape(-1, D_MODEL), hc.reshape(-1, D_MODEL)], axis=0)
            f = _hier_moe(rows, router_g_w[l], router_g_b[l], router_e_w[l], router_e_b[l],
                          moe_w1[l], moe_w3[l], moe_w2[l])
            fx = f[:bsz * n_lat].reshape(hx.shape)
            fc = f[bsz * n_lat:].reshape(hc.shape)
            xc = _layer_norm(ALPHA * xc + cmod[5] * fc, ln2_g[l], ln2_b[l])
        xl = _layer_norm(ALPHA * xl + g2 * fx, ln2_g[l], ln2_b[l])
    return xl
```

```python
import math
import numpy as np
import concourse.bass as bass
import concourse.mybir as mybir
from concourse.bass_utils import run_bass_kernel_spmd

F32 = mybir.dt.float32
BF16 = mybir.dt.bfloat16
I32 = mybir.dt.int32
AF = mybir.ActivationFunctionType
ALU = mybir.AluOpType
AX = mybir.AxisListType

DEPTH = 4
D = 1024
NCTX = 256
NLAT = 2048
NT = NCTX + NLAT
ALPHA = (2.0 * DEPTH) ** 0.25
LN_EPS = 1e-5
TCH = [(0, 256), (256, 512), (768, 512), (1280, 512), (1792, 512)]
ENGS = ("pe", "act", "dve", "pool", "sp")
WQ = "pool"
SB_BASE = 16512
SB_TOP = 229344


class Prog:
    def __init__(self, nc, n_dma_sems=8, same_engine_sync=True):
        self.nc = nc
        self.ops = []
        self.res = {}
        self.same = same_engine_sync
        self.n_dma_sems = n_dma_sems
        self.dma_rr = {e: 0 for e in ENGS}
        self.dma_last = {}
        self.last_op = {}
        self.open_dmas = []
        self.fence_deps = set()

    def op(self, eng, fn, rd=(), wr=(), kind="c"):
        wr = list(wr) + [r for r in rd if r.startswith("ps") and r not in wr]
        deps = set()
        for r in rd:
            st = self.res.get(r)
            if st is not None and st[0] is not None:
                deps.add(st[0])
        for r in wr:
            st = self.res.get(r)
            if st is not None:
                if st[0] is not None:
                    deps.add(st[0])
                for o in st[1].values():
                    deps.add(o)
        deps.update(self.fence_deps)
        oid = len(self.ops)
        rec = dict(eng=eng, fn=fn, deps=deps, kind=kind, slot=None)
        if kind == "dma":
            self.open_dmas.append(oid)
        else:
            self.last_op[eng] = oid
        if kind == "dma":
            slot = self.dma_rr[eng] % self.n_dma_sems
            self.dma_rr[eng] += 1
            rec["slot"] = slot
            prev = self.dma_last.get((eng, slot))
            if prev is not None:
                deps.add(prev)
            self.dma_last[(eng, slot)] = oid
        self.ops.append(rec)
        for r in rd:
            st = self.res.setdefault(r, [None, {}])
            st[1][(eng, oid if kind == "dma" else -1)] = oid
        for r in wr:
            self.res[r] = [oid, {}]
        return oid

    def fence(self):
        f = set(self.last_op.values())
        f.update(self.open_dmas)
        self.open_dmas = []
        self.fence_deps = f
        self.res = {}

    def dma(self, eng, out, in_, rd=(), wr=(), **kw):
        return self.op(eng, lambda e: e.dma_start(out=out, in_=in_, **kw), rd, wr, kind="dma")

    def emit(self, final_wait_ops=()):
        nc = self.nc
        ops = self.ops
        needed = set()
        for o in ops:
            needed.update(o["deps"])
        needed.update(final_wait_ops)
        esem = {e: nc.alloc_semaphore("s_" + e) for e in ENGS}
        dsem = {}
        for (e, s) in self.dma_last:
            dsem[(e, s)] = nc.alloc_semaphore("d_%s%d" % (e, s))
        ecount = {e: 0 for e in ENGS}
        dcount = {k: 0 for k in dsem}
        for i, o in enumerate(ops):
            if o["kind"] == "dma":
                k = (o["eng"], o["slot"])
                dcount[k] += 16
                o["sig"] = (dsem[k], dcount[k], ("d",) + k)
            elif i in needed:
                ecount[o["eng"]] += 1
                o["sig"] = (esem[o["eng"]], ecount[o["eng"]], ("e", o["eng"]))
            else:
                o["sig"] = None
        streams = {e: [] for e in ENGS}
        seen = {e: {} for e in ENGS}
        for i, o in enumerate(ops):
            e = o["eng"]
            waits = {}
            for d in o["deps"]:
                od = ops[d]
                if od["kind"] != "dma" and od["eng"] == e and (e == "pe" or not self.same):
                    continue
                sem, val, key = od["sig"]
                if seen[e].get(key, 0) >= val:
                    continue
                if key not in waits or waits[key][1] < val:
                    waits[key] = (sem, val)
            for key, (sem, val) in waits.items():
                seen[e][key] = val
            streams[e].append((list(waits.values()), o))
        fin = [(ops[d]["sig"][0], ops[d]["sig"][1]) for d in final_wait_ops]
        self.n_instr = {e: len(streams[e]) for e in ENGS}

        def run(engname, engobj):
            for waits, o in streams[engname]:
                for sem, val in waits:
                    engobj.wait_ge(sem, val)
                ins = o["fn"](engobj)
                if o["sig"] is not None:
                    ins.then_inc(o["sig"][0], 16 if o["kind"] == "dma" else 1)
            if engname == "sp":
                for sem, val in fin:
                    engobj.wait_ge(sem, val)

        with nc.Block() as block:
            @block.tensor
            def _(t):
                run("pe", t)

            @block.scalar
            def _(t):
                run("act", t)

            @block.vector
            def _(t):
                run("dve", t)

            @block.gpsimd
            def _(t):
                run("pool", t)

            @block.sync
            def _(t):
                run("sp", t)


def XAP(t, offset, dims):
    return bass.AP(t, offset, [list(d) for d in dims])


class Builder:
    def __init__(self, n_layers=DEPTH, dbg=(), skip=()):
        self.stop = [x[5:] for x in skip if x.startswith("stop:")]
        self.n_layers = n_layers
        self.dbg = set(dbg)
        self.skip = set(skip)
        self.nc = bass.Bass("TRN2", target_bir_lowering=False)
        self.P = Prog(self.nc)
        self.out_ops = []
        self.dram = {}
        self.uid = 0
        self.sb_ptr = SB_BASE
        self.ph_base = None

    def din(self, name, shape, dt=F32):
        if name not in self.dram:
            self.dram[name] = self.nc.dram_tensor(name, list(shape), dt, kind="ExternalInput").ap()
        return self.dram[name]

    def dout(self, name, shape, dt=F32):
        self.dram[name] = self.nc.dram_tensor(name, list(shape), dt, kind="ExternalOutput").ap()
        return self.dram[name]

    def sb(self, name, shape, dt=F32, at=None):
        esz = 2 if dt == BF16 else 4
        nbytes = esz
        for d_ in shape[1:]:
            nbytes *= d_
        nbytes = (nbytes + 31) // 32 * 32
        if at is None:
            off = self.sb_ptr
            self.sb_ptr += nbytes
        else:
            off = self.ph_base + at
        assert off + nbytes <= SB_TOP, (name, off, nbytes)
        self.uid += 1
        return self.nc.alloc_sbuf_tensor_at("%s_%d" % (name, self.uid), list(shape), dt, offset=off)

    def phase(self, reserve=0):
        self.P.fence()
        self.sb_ptr = self.ph_base + reserve

    def mm(self, out, lhsT, rhs, start, stop, rd, wr, **kw):
        self.P.op("pe", lambda e: e.matmul(out, lhsT=lhsT, rhs=rhs, start=start, stop=stop, **kw), rd, wr)

    def tr(self, out, in_, ident, rd, wr):
        self.P.op("pe", lambda e: e.transpose(out, in_, ident), rd, wr)

    def act(self, out, in_, func, rd, wr, **kw):
        self.P.op("act", lambda e: e.activation(out=out, in_=in_, func=func, **kw), rd, wr)

    def tt(self, eng, out, in0, in1, op, rd, wr):
        self.P.op(eng, lambda e: e.tensor_tensor(out=out, in0=in0, in1=in1, op=op), rd, wr)

    def ts(self, eng, out, in0, s1, s2, op0, op1, rd, wr):
        if op1 is None and eng == "pool" and op0 == ALU.mult:
            op1 = ALU.add
            s2 = 0.0
        if op1 is None:
            self.P.op(eng, lambda e: e.tensor_scalar(out=out, in0=in0, scalar1=s1, scalar2=None, op0=op0), rd, wr)
        else:
            self.P.op(eng, lambda e: e.tensor_scalar(out=out, in0=in0, scalar1=s1, scalar2=s2, op0=op0, op1=op1), rd, wr)

    def stt(self, out, in0, scalar, in1, op0, op1, rd, wr):
        self.P.op("dve", lambda e: e.scalar_tensor_tensor(out=out, in0=in0, scalar=scalar, in1=in1, op0=op0, op1=op1), rd, wr)

    def cp(self, eng, out, in_, rd, wr):
        if eng == "act":
            self.act(out, in_, AF.Copy, rd, wr)
        else:
            self.P.op(eng, lambda e: e.tensor_copy(out=out, in_=in_), rd, wr)

    def red(self, out, in_, op, rd, wr):
        self.P.op("dve", lambda e: e.tensor_reduce(out=out, in_=in_, axis=AX.X, op=op), rd, wr)

    def recip(self, out, in_, rd, wr):
        self.P.op("dve", lambda e: e.reciprocal(out=out, in_=in_), rd, wr)

    def memset(self, eng, ap, val, wr):
        self.P.op(eng, lambda e: e.memset(ap, val), (), wr)

    def dma(self, q, out, in_, rd, wr, **kw):
        return self.P.dma(q, out, in_, rd, wr, **kw)

    def dmas(self, q, out, in_, rd, wr):
        return self.P.dma(q, out, in_, rd, wr, allow_slow_non_contiguous=True)

    def dump(self, name, ap_sb, shape, rd, dt=F32):
        d = self.dout(name, shape, F32)
        if len(shape) == 3:
            for c in range(shape[1]):
                self.out_ops.append(self.dma(WQ, d[:, c, :], ap_sb[:, c, :], rd, ()))
        else:
            self.out_ops.append(self.dma(WQ, d, ap_sb, rd, ()))

    def W(self, name):
        shapes = {
            "w_mod": [DEPTH, D, 6 * D], "b_mod": [DEPTH, 6 * D], "w_in": [DEPTH, D, 5632], "lamv": [DEPTH, 4, 64],
            "subln_g": [DEPTH, 128], "w_pa": [DEPTH, D, D], "w_ps": [DEPTH, 512, D], "w_o": [DEPTH, D, D],
            "w_glu": [DEPTH, 512, 512], "b_glu": [DEPTH, 512], "ln1_g": [DEPTH, D], "ln1_b": [DEPTH, D],
            "ln2_g": [DEPTH, D], "ln2_b": [DEPTH, D], "router_g_w": [DEPTH, D, 4], "router_g_b": [DEPTH, 4],
            "router_e_w": [DEPTH, D, 32], "router_e_b": [DEPTH, 32], "moe_w1": [DEPTH, 32, D, 512],
            "moe_w3": [DEPTH, 32, D, 512], "moe_w2": [DEPTH, 32, 512, D],
            "ssm_a_re": [DEPTH, 2, 32, 64], "ssm_a_im": [DEPTH, 2, 32, 64], "ssm_log_dt": [DEPTH, 2, 32],
            "ssm_b_re": [DEPTH, 2, 32, 64, 16], "ssm_b_im": [DEPTH, 2, 32, 64, 16],
            "ssm_c_re": [DEPTH, 2, 32, 16, 64], "ssm_c_im": [DEPTH, 2, 32, 16, 64], "ssm_d": [DEPTH, 512],
            "x": [NLAT, D], "ctx": [NCTX, D], "cvec": [2, D], "ident": [128, 128], "rmat": [128, 128],
            "ropecos": [128, NLAT], "ropesin": [128, NLAT], "swp": [128, 128], "gmask": [128, 8], "sgn": [128, 2],
        }
        return self.din(name, shapes[name])

    @staticmethod
    def hk(c, j):
        return "hT%d_%d" % (c, j)

    @staticmethod
    def xk(c, j):
        return "xT%d_%d" % (c, j)

    @staticmethod
    def bk(c, j):
        return "bg%d_%d" % (c, j)

    @staticmethod
    def tj(ti):
        return 0 if ti < 2 else 1 + (ti - 2) // 4

    def vecload(self, dst, src_row_ap, key, n=None):
        n = dst.shape[1]
        st = self.vstage[self.vcnt % 2]; sk = "vstage%d" % (self.vcnt % 2)
        self.vcnt += 1
        self.dma("sp", st[0:n, :], src_row_ap.rearrange("(c p) -> c p", p=128), (), [sk])
        self.tr(self.ps[7][:, 0:n], st[0:n, :], self.identf[0:n, 0:n], [sk, "identf"], ["ps7"])
        self.cp("dve", dst, self.ps[7][:, 0:n], ["ps7"], [key])

    def bcast_load(self, dst, src_ap, n, key):
        st = self.bstage
        self.dma("sp", st[0:1, 0:n], src_ap, (), ["bstage"])
        self.mm(self.ps[7][:, 0:n], self.ones1[0:1, :], st[0:1, 0:n], True, True, ["bstage", "ones1"], ["ps7"])
        self.cp("dve", dst, self.ps[7][:, 0:n], ["ps7"], [key])

    def build(self):
        B = self
        nc = self.nc
        L = self.n_layers
        x_d = B.W("x"); ctx_d = B.W("ctx"); cvec_d = B.W("cvec")
        out_d = B.dout("out", [NLAT, D])
        self.xT = xT = B.sb("xT", [128, 8, NT]); self.hT = B.sb("hT", [128, 8, NT], BF16)
        self.identf = B.sb("identf", [128, 128]); self.identb = B.sb("identb", [128, 128], BF16)
        self.rmatb = B.sb("rmatb", [128, 128], BF16)
        self.cosb = B.sb("cosb", [128, NLAT], BF16); self.sinb = B.sb("sinb", [128, NLAT], BF16)
        self.onesf = B.sb("onesf", [128, 128]); self.zerob = B.sb("zerob", [128, 512], BF16)
        self.epsc = B.sb("epsc", [128, 1]); self.negpi = B.sb("negpi", [128, 1])
        self.modx = B.sb("modx", [128, 48]); self.cmodx = B.sb("cmodx", [128, 48]); self.bmod = B.sb("bmod", [128, 48])
        self.cv = B.sb("cv", [128, 2, 8]); self.scv = B.sb("scv", [128, 8, 2], BF16)
        self.vstage = [B.sb("vstage%d" % i, [48, 128]) for i in range(2)]; self.vcnt = 0
        self.bstage = B.sb("bstage", [1, 256]); self.ones1 = B.sb("ones1", [1, 128])
        self.swpf = B.sb("swpf", [128, 128]); self.gmaskf = B.sb("gmaskf", [128, 8]); self.sgnf = B.sb("sgnf", [128, 2])
        self.ph_base = (self.sb_ptr + 63) // 64 * 64
        self.ps = ps = [nc.alloc_psum_tensor("ps%d" % i, [128, 512], F32) for i in range(8)]
        PSK = lambda i: "ps%d" % i
        identf = self.identf

        B.dma("sp", identf[:, :], B.W("ident")[:, :], (), ["identf"])
        B.dma(WQ, self.identb[:, :], B.W("ident")[:, :], (), ["identb"])
        B.dma(WQ, self.rmatb[:, :], B.W("rmat")[:, :], (), ["rmatb"])
        B.dma(WQ, self.cosb[:, :], B.W("ropecos")[:, :], (), ["cosb"])
        B.dma(WQ, self.sinb[:, :], B.W("ropesin")[:, :], (), ["sinb"])
        B.dma("sp", self.swpf[:, :], B.W("swp")[:, :], (), ["swpf"])
        B.dma("sp", self.gmaskf[:, :], B.W("gmask")[:, :], (), ["gmaskf"])
        B.dma("sp", self.sgnf[:, :], B.W("sgn")[:, :], (), ["sgnf"])
        B.memset("dve", self.onesf[:, :], 1.0 / D, ["onesf"])
        B.memset("dve", self.zerob[:, :], 0.0, ["zerob"])
        B.memset("dve", self.epsc[:, :], LN_EPS, ["epsc"])
        B.memset("dve", self.negpi[:, :], -math.pi, ["negpi"])
        B.memset("dve", self.ones1[:, :], 1.0, ["ones1"])

        self.phase()
        stg = [B.sb("stg%d" % i, [128, D]) for i in range(2)]
        for ti in range(NT // 128):
            s = stg[ti % 2]; sk = "stg%d" % (ti % 2)
            src = ctx_d[ti * 128:(ti + 1) * 128, :] if ti < 2 else x_d[(ti - 2) * 128:(ti - 1) * 128, :]
            B.dma("sp", s[:, :], src, (), [sk])
            j = self.tj(ti)
            for half in range(2):
                pb = ps[half]
                for cc in range(4):
                    c = half * 4 + cc
                    B.tr(pb[:, cc * 128:(cc + 1) * 128], s[:, c * 128:(c + 1) * 128], identf[:, :], [sk, "identf"], [PSK(half)])
                eng = "dve" if half == 0 else "act"
                B.cp(eng, XAP(xT, half * 4 * NT + ti * 128, [[8 * NT, 128], [NT, 4], [1, 128]]),
                     pb[:, :].rearrange("p (c t) -> p c t", t=128), [PSK(half)], [self.xk(c, j) for c in range(half * 4, half * 4 + 4)])
        for r in range(2):
            self.vecload(self.cv[:, r, :], cvec_d[r, :], "cv%d" % r)
        B.act(self.scv[:, :, :].rearrange("p c r -> p r c"), self.cv[:, :, :], AF.Silu, ["cv0", "cv1"], ["scv"])

        for l in range(L):
            self.layer(l)

        self.phase()
        ostg = [B.sb("ostg%d" % i, [128, D]) for i in range(2)]
        for ti in range(2, NT // 128):
            s = ostg[ti % 2]; sk = "ostg%d" % (ti % 2)
            j = self.tj(ti)
            for half in range(2):
                pb = ps[half]
                for cc in range(4):
                    c = half * 4 + cc
                    B.tr(pb[:, cc * 128:(cc + 1) * 128], xT[:, c, ti * 128:(ti + 1) * 128], identf[:, :], [self.xk(c, j), "identf"], [PSK(half)])
                eng = "dve" if half == 0 else "act"
                B.cp(eng, s[:, half * 512:(half + 1) * 512], pb[:, :], [PSK(half)], [sk + "_%d" % half])
            self.out_ops.append(B.dma("sp", out_d[(ti - 2) * 128:(ti - 1) * 128, :], s[:, :], [sk + "_0", sk + "_1"], ()))
        self.P.emit(final_wait_ops=self.out_ops)
        return nc

    def layer(self, l):
        B = self
        self.last = (l == DEPTH - 1)
        ps = self.ps
        PSK = lambda i: "ps%d" % i
        xT, hT = self.xT, self.hT
        modx, cmodx, bmod = self.modx, self.cmodx, self.bmod
        w_mod = self.W("w_mod"); b_mod = self.W("b_mod")
        self.phase()
        wm = [B.sb("wm%d" % i, [128, 8, 512], BF16) for i in range(2)]
        self.vecload(bmod[:, :], b_mod[l, :], "bmod")
        for blk in range(12):
            wt = wm[blk % 2]; wk_ = "wm%d" % (blk % 2)
            B.dma(WQ, wt[:, :, :], w_mod[l, :, blk * 512:(blk + 1) * 512].rearrange("(c p) n -> p c n", p=128), (), [wk_])
            for jj in range(4):
                j = blk * 4 + jj
                for kc in range(8):
                    B.mm(ps[0][:, 2 * j:2 * j + 2], wt[:, kc, jj * 128:(jj + 1) * 128], self.scv[:, kc, :], kc == 0, kc == 7,
                         [wk_, "scv"], [PSK(0)])
        pv = ps[0][:, 0:96].rearrange("p (j t) -> p j t", t=2)
        B.tt("dve", modx[:, :], pv[:, :, 0], bmod[:, :], ALU.add, [PSK(0), "bmod"], ["modx"])
        B.tt("dve", cmodx[:, :], pv[:, :, 1], bmod[:, :], ALU.add, [PSK(0), "bmod"], ["cmodx"])
        for m_, k_ in ((modx, "modx"), (cmodx, "cmodx")):
            B.ts("dve", m_[:, 8:16], m_[:, 8:16], 1.0, None, ALU.add, None, [k_], [k_])
            B.ts("dve", m_[:, 32:40], m_[:, 32:40], 1.0, None, ALU.add, None, [k_], [k_])
        if "mod" in self.dbg and l == 0:
            self.dump("d_modx", modx[:, :], [128, 48], ["modx"]); self.dump("d_cmodx", cmodx[:, :], [128, 48], ["cmodx"])
        if "mod" in self.stop:
            return
        self.modulate(0)
        if "h" in self.dbg and l == 0:
            self.dump("d_hT", hT[:, :, :], [128, 8, NT], [self.hk(c, j) for c in range(8) for j in range(5)], BF16)
        if "h" in self.stop:
            return
        self.phase()
        self.big = B.sb("attnT", [128, 8, NT], BF16)
        if "attn" not in self.skip:
            self.attn_prep(l)
            import os
            self.att_stage = int(os.environ.get("ATT_STAGE", "9"))
            for h in range(int(os.environ.get("ATT_HEADS", "8"))):
                self.attention(l, h)
            if "attn" in self.dbg and l == 0:
                self.dump("d_attnT", self.big[:, :, :], [128, 8, NT], [self.bk(c, j) for c in range(8) for j in range(5)], BF16)
        if "attn" in self.stop:
            return
        self.phase(reserve=8 * NT * 2)
        self.scale_x()
        if "attn" not in self.skip:
            self.merge(l, "attn")
        if "amerge" in self.stop:
            return
        if "ssm" not in self.skip:
            self.ssm(l)
            self.merge(l, "ssm")
        self.phase()
        self.layernorm(l, 1)
        if "mid" in self.dbg and l == 0:
            self.dump("d_xmid", xT[:, :, :], [128, 8, NT], [self.xk(c, j) for c in range(8) for j in range(5)])
        self.modulate(24)
        if "moe" not in self.skip:
            self.router(l)
        self.scale_x()
        if "moe" not in self.skip:
            self.moe(l)
        self.phase()
        self.layernorm(l, 2)

    def modulate(self, base):
        B = self
        xT, hT = self.xT, self.hT
        for c in range(8):
            for j, (t0, n) in enumerate(TCH):
                if self.last and j == 0 and base != 0:
                    continue
                m_, k_ = (self.cmodx, "cmodx") if j == 0 else (self.modx, "modx")
                B.act(hT[:, c, t0:t0 + n], xT[:, c, t0:t0 + n], AF.Identity, [self.xk(c, j), k_], [self.hk(c, j)],
                      scale=m_[:, base + 8 + c:base + 9 + c], bias=m_[:, base + c:base + c + 1])

    def scale_x(self):
        B = self
        xT = self.xT
        for c in range(8):
            for j, (t0, n) in enumerate(TCH):
                eng = "pool" if (c + j) % 2 else "dve"
                B.ts(eng, xT[:, c, t0:t0 + n], xT[:, c, t0:t0 + n], float(ALPHA), None, ALU.mult, None, [self.xk(c, j)], [self.xk(c, j)])

    def attn_prep(self, l):
        B = self
        at = {}
        at["wa"] = [B.sb("wa%d" % i, [128, 8, 128], BF16) for i in range(2)]
        at["wb"] = [B.sb("wb%d" % i, [128, 8, 128], BF16) for i in range(2)]
        at["wc"] = [B.sb("wc%d" % i, [128, 8, 128], BF16) for i in range(2)]
        at["kT"] = B.sb("kT", [128, NT], BF16); at["qT"] = B.sb("qT", [128, NT], BF16)
        at["Vh"] = B.sb("Vh", [128, 18, 129], BF16)
        at["pt"] = [B.sb("pt%d" % i, [128, 512], BF16) for i in range(3)]
        at["tb"] = [B.sb("tb%d" % i, [128, 512], BF16) for i in range(2)]
        at["t1"] = [B.sb("t1_%d" % i, [128, 512]) for i in range(2)]
        at["t2"] = [B.sb("t2_%d" % i, [128, 512]) for i in range(2)]
        at["lamt"] = B.sb("lamt", [128, 4, 64]); at["lamp"] = B.sb("lamp", [128, 2, 64]); at["lams"] = B.sb("lams", [128, 4])
        at["nlam"] = B.sb("nlam", [128, 1]); at["g128"] = B.sb("g128", [128, 128])
        at["sm"] = [B.sb("sm%d" % i, [128, 8]) for i in range(4)]
        at["of"] = [B.sb("of%d" % i, [128, 128]) for i in range(2)]
        at["ot"] = [B.sb("ot%d" % i, [128, 128]) for i in range(2)]
        at["onb"] = [B.sb("onb%d" % i, [128, 128], BF16) for i in range(2)]
        at["osb"] = [B.sb("osb%d" % i, [128, 258]) for i in range(4)]
        self.at = at
        B.memset("pool", at["Vh"][:, :, :], 1.0, ["Vh"])
        lamv = self.W("lamv"); subg = self.W("subln_g")
        lam_init = 0.8 - 0.6 * math.exp(-0.3 * l)
        self.bcast_load(at["lamt"][:, :, :].rearrange("p a b -> p (a b)"), lamv[l:l + 1, :, :].rearrange("o a b -> o (a b)"), 256, "lamt")
        self.bcast_load(at["g128"][:, :], subg[l:l + 1, :], 128, "g128")
        B.ts("dve", at["g128"][:, :], at["g128"][:, :], float(1.0 - lam_init), None, ALU.mult, None, ["g128"], ["g128"])
        B.tt("dve", at["lamp"][:, 0, :], at["lamt"][:, 0, :], at["lamt"][:, 1, :], ALU.mult, ["lamt"], ["lamp"])
        B.tt("dve", at["lamp"][:, 1, :], at["lamt"][:, 2, :], at["lamt"][:, 3, :], ALU.mult, ["lamt"], ["lamp"])
        B.red(at["lams"][:, 0:2], at["lamp"][:, :, :], ALU.add, ["lamp"], ["lams"])
        B.act(at["lams"][:, 2:4], at["lams"][:, 0:2], AF.Exp, ["lams"], ["lams2"])
        B.tt("dve", at["nlam"][:, :], at["lams"][:, 3:4], at["lams"][:, 2:3], ALU.subtract, ["lams2"], ["nlam"])
        B.ts("dve", at["nlam"][:, :], at["nlam"][:, :], float(-lam_init), None, ALU.add, None, ["nlam"], ["nlam"])

    def proj_fm(self, wt, wkey, nkc, src, srck, dst_fn, dkey_fn, rope=False, tiles=None, evac="act"):
        B = self
        ps = self.ps
        for j, (t0, n) in enumerate(TCH):
            pb = ps[j % 2]; pk = "ps%d" % (j % 2)
            for kc in range(nkc):
                B.mm(pb[:, :n], wt[:, kc, :], src[:, kc, t0:t0 + n], kc == 0, kc == nkc - 1, [wkey, srck(kc, j)], [pk])
            import os
            rm = int(os.environ.get("ROPE_MODE", "2"))
            if j == 0 or not rope or rm == 0:
                B.cp(evac if j % 2 == 0 else "dve", dst_fn(t0, n), pb[:, :n], [pk], [dkey_fn(j)])
            else:
                tb = tiles["tb"][j % 2]; tbk = "tb%d" % (j % 2)
                t1 = tiles["t1"][j % 2]; t1k = "t1_%d" % (j % 2)
                t2 = tiles["t2"][j % 2]; t2k = "t2_%d" % (j % 2)
                pr = ps[2 + j % 2]; prk = "ps%d" % (2 + j % 2)
                B.cp("act", tb[:, :n], pb[:, :n], [pk], [tbk])
                B.mm(pr[:, :n], self.rmatb[:, :], tb[:, :n], True, True, ["rmatb", tbk], [prk])
                lo = t0 - NCTX
                if rm in (3, 4, 5):
                    if rm >= 4:
                        B.tt("dve", t1[:, :n], pb[:, :n], self.cosb[:, lo:lo + n], ALU.mult, [pk, "cosb", tbk], [t1k])
                    if rm >= 5:
                        B.tt("dve", t2[:, :n], pr[:, :n], self.sinb[:, lo:lo + n], ALU.mult, [prk, "sinb"], [t2k])
                    B.cp("dve", dst_fn(t0, n), pb[:, :n], [pk, prk], [dkey_fn(j)])
                    continue
                B.tt("dve", t1[:, :n], pb[:, :n], self.cosb[:, lo:lo + n], ALU.mult, [pk, "cosb", tbk], [t1k])
                B.tt("dve", t2[:, :n], pr[:, :n], self.sinb[:, lo:lo + n], ALU.mult, [prk, "sinb"], [t2k])
                B.tt("pool" if rm == 2 else "dve", dst_fn(t0, n), t1[:, :n], t2[:, :n], ALU.add, [t1k, t2k], [dkey_fn(j)])

    def attention(self, l, h):
        B = self
        at = self.at; ps = self.ps; hT = self.hT; big = self.big
        w_in = self.W("w_in")
        i2 = h % 2
        wk, wq, wv = at["wa"][i2], at["wb"][i2], at["wc"][i2]
        wkk, wqk, wvk = "wa%d" % i2, "wb%d" % i2, "wc%d" % i2
        for wt, key, off in ((wk, wkk, 0), (wq, wqk, 2560), (wv, wvk, 1024)):
            B.dma(WQ, wt[:, :, :], w_in[l, :, off + h * 128:off + (h + 1) * 128].rearrange("(c p) n -> p c n", p=128), (), [key])
        kT, qT, Vh = at["kT"], at["qT"], at["Vh"]
        self.proj_fm(wk, wkk, 8, hT, self.hk, lambda t0, n: kT[:, t0:t0 + n], lambda j: "kT_%d" % j, True, at)
        self.proj_fm(wq, wqk, 8, hT, self.hk, lambda t0, n: qT[:, t0:t0 + n], lambda j: "qT_%d" % j, True, at)
        if self.att_stage < 1:
            return
        if "qk" in self.dbg and l == 0 and h == 0:
            self.dump("d_kT", kT[:, :], [128, NT], ["kT_%d" % j for j in range(5)], BF16)
            self.dump("d_qT", qT[:, :], [128, NT], ["qT_%d" % j for j in range(5)], BF16)
        for t4 in range(5):
            tiles = list(range(t4 * 4, min(18, t4 * 4 + 4)))
            pb = ps[t4 % 2]; pk = "ps%d" % (t4 % 2)
            for ii, ti in enumerate(tiles):
                j = self.tj(ti)
                for kc in range(8):
                    B.mm(pb[:, ii * 128:(ii + 1) * 128], hT[:, kc, ti * 128:(ti + 1) * 128], wv[:, kc, :], kc == 0, kc == 7,
                         [wvk, self.hk(kc, j)], [pk])
            nt_ = len(tiles)
            B.cp("act", XAP(Vh, tiles[0] * 129, [[18 * 129, 128], [129, nt_], [1, 128]]),
                 pb[:, 0:nt_ * 128].rearrange("p (a b) -> p a b", b=128), [pk], ["Vh"])
        if self.att_stage < 2:
            return
        cnt = 0
        for qi, (q0, qn, nk) in enumerate([(0, 256, 2)] + [(256 + 512 * i, 512, 18) for i in range(4)]):
            if self.last and qi == 0:
                continue
            nsub = qn // 128
            nb = (nsub + 1) // 2
            for m in range(2):
                for bb in range(nb):
                    bk_ = 4 + 2 * m + bb
                    B.mm(ps[bk_][:, :], self.zerob[:, 0:128], self.zerob[:, :], True, True, ["zerob"], ["ps%d" % bk_])
            for m in range(2):
                prev = None
                for kt in range(nk + 1):
                    cur = None
                    if kt < nk:
                        sbk = 2 + cnt % 2
                        pt = at["pt"][cnt % 3]; ptk = "pt%d" % (cnt % 3)
                        cnt += 1
                        jk = self.tj(kt)
                        B.mm(ps[sbk][:, :qn], kT[64 * m:64 * m + 64, kt * 128:(kt + 1) * 128], qT[64 * m:64 * m + 64, q0:q0 + qn], True, True,
                             ["kT_%d" % jk, "qT_%d" % qi], ["ps%d" % sbk])
                        B.act(pt[:, :qn], ps[sbk][:, :qn], AF.Exp, ["ps%d" % sbk], [ptk], scale=0.125)
                        cur = (pt, ptk, kt)
                    if prev is not None:
                        ppt, pptk, pkt = prev
                        for sub in range(nsub):
                            bk_ = 4 + 2 * m + sub // 2
                            col = (sub % 2) * 129
                            B.mm(ps[bk_][:, col:col + 129], ppt[:, sub * 128:(sub + 1) * 128], Vh[:, pkt, :], False, pkt == nk - 1,
                                 [pptk, "Vh"], ["ps%d" % bk_], skip_group_check=True)
                    prev = cur
            osb = at["osb"]
            for m in range(2):
                for bb in range(nb):
                    bk_ = 4 + 2 * m + bb
                    B.cp("act" if (m + bb) % 2 else "dve", osb[bk_ - 4][:, :], ps[bk_][:, 0:258], ["ps%d" % bk_], ["osb%d" % (bk_ - 4)])
            if self.att_stage < 3:
                continue
            for sub in range(nsub):
                b0 = sub // 2; b1 = 2 + sub // 2
                col = (sub % 2) * 129
                sm = at["sm"][sub % 4]; smk = "sm%d" % (sub % 4)
                of = at["of"][sub % 2]; ofk = "of%d" % (sub % 2)
                ot = at["ot"][sub % 2]; otk = "ot%d" % (sub % 2)
                onb = at["onb"][sub % 2]; onk = "onb%d" % (sub % 2)
                o0, o1 = at["osb"][b0], at["osb"][b1]
                B.recip(sm[:, 0:1], o0[:, col + 128:col + 129], ["osb%d" % b0], [smk])
                B.recip(sm[:, 1:2], o1[:, col + 128:col + 129], ["osb%d" % b1], [smk])
                B.tt("dve", sm[:, 2:3], sm[:, 1:2], at["nlam"][:, :], ALU.mult, [smk, "nlam"], [smk])
                B.ts("dve", ot[:, :], o1[:, col:col + 128], sm[:, 2:3], None, ALU.mult, None, ["osb%d" % b1, smk], [otk])
                B.stt(of[:, :], o0[:, col:col + 128], sm[:, 0:1], ot[:, :], ALU.mult, ALU.add, ["osb%d" % b0, smk, otk], [ofk])
                B.tt("dve", ot[:, :], of[:, :], of[:, :], ALU.mult, [ofk], [otk])
                B.red(sm[:, 3:4], ot[:, :], ALU.add, [otk], [smk])
                B.act(sm[:, 4:5], sm[:, 3:4], AF.Ln, [smk], [smk], scale=1.0 / 128.0, bias=self.epsc[:, :])
                B.act(sm[:, 5:6], sm[:, 4:5], AF.Exp, [smk], [smk], scale=-0.5)
                B.stt(onb[:, :], of[:, :], sm[:, 5:6], at["g128"][:, :], ALU.mult, ALU.mult, [ofk, smk, "g128"], [onk])
                pbf = ps[sub % 2][:, 0:64].bitcast(BF16)
                B.tr(pbf, onb[:, :], self.identb[:, :], [onk, "identb"], ["ps%d" % (sub % 2)])
                tq = q0 + sub * 128
                jq = 0 if tq < 256 else 1 + (tq - 256) // 512
                B.cp("act", big[:, h, tq:tq + 128], pbf, ["ps%d" % (sub % 2)], [self.bk(h, jq)])

    def merge(self, l, kind):
        B = self
        ps = self.ps; hT = self.hT; xT = self.xT
        w_in = self.W("w_in"); w_o = self.W("w_o")
        if kind == "attn":
            goff = 3584; wp = self.W("w_pa"); nkc = 8; src = self.big; srck = self.bk
        else:
            goff = 4608; wp = self.W("w_ps"); nkc = 4; src = self.ssmT; srck = lambda c, j: "ss%d_%d" % (c, j)
        wa = [B.sb("mwa%d" % i, [128, 8, 128], BF16) for i in range(2)]
        wb = [B.sb("mwb%d" % i, [128, 8, 128], BF16) for i in range(2)]
        wo = [B.sb("mwo%d" % i, [128, 1024], BF16) for i in range(2)]
        mC = [B.sb("mC%d" % i, [128, NT], BF16) for i in range(2)]
        sg = [B.sb("msg%d" % i, [128, 512]) for i in range(2)]
        ycnt = 0
        for c in range(8):
            i2 = c % 2
            B.dma(WQ, wa[i2][:, :, :], w_in[l, :, goff + c * 128:goff + (c + 1) * 128].rearrange("(c p) n -> p c n", p=128), (), ["mwa%d" % i2])
            B.dma(WQ, wb[i2][:, 0:nkc, :], wp[l, :, c * 128:(c + 1) * 128].rearrange("(c p) n -> p c n", p=128), (), ["mwb%d" % i2])
            B.dma(WQ, wo[i2][:, :], w_o[l, c * 128:(c + 1) * 128, :], (), ["mwo%d" % i2])
            for j, (t0, n) in enumerate(TCH):
                if self.last and j == 0:
                    continue
                pg = ps[j % 2]; pgk = "ps%d" % (j % 2)
                pp = ps[2 + j % 2]; ppk = "ps%d" % (2 + j % 2)
                for kc in range(8):
                    B.mm(pg[:, :n], wa[i2][:, kc, :], hT[:, kc, t0:t0 + n], kc == 0, kc == 7, ["mwa%d" % i2, self.hk(kc, j)], [pgk])
                for kc in range(nkc):
                    B.mm(pp[:, :n], wb[i2][:, kc, :], src[:, kc, t0:t0 + n], kc == 0, kc == nkc - 1, ["mwb%d" % i2, srck(kc, j)], [ppk])
                B.act(sg[j % 2][:, :n], pg[:, :n], AF.Sigmoid, [pgk], ["msg%d" % (j % 2)])
                B.tt("dve", mC[i2][:, t0:t0 + n], sg[j % 2][:, :n], pp[:, :n], ALU.mult, ["msg%d" % (j % 2), ppk], ["mC%d_%d" % (i2, j)])
            for o in range(8):
                for j, (t0, n) in enumerate(TCH):
                    if self.last and j == 0:
                        continue
                    py = ps[4 + ycnt % 4]; pyk = "ps%d" % (4 + ycnt % 4)
                    ycnt += 1
                    B.mm(py[:, :n], wo[i2][:, o * 128:(o + 1) * 128], mC[i2][:, t0:t0 + n], True, True, ["mwo%d" % i2, "mC%d_%d" % (i2, j)], [pyk])
                    m_, k_ = (self.cmodx, "cmodx") if j == 0 else (self.modx, "modx")
                    B.stt(xT[:, o, t0:t0 + n], py[:, :n], m_[:, 16 + o:17 + o], xT[:, o, t0:t0 + n], ALU.mult, ALU.add,
                          [pyk, k_, self.xk(o, j)], [self.xk(o, j)])

    def layernorm(self, l, which):
        B = self
        ps = self.ps; xT = self.xT
        g_d = self.W("ln%d_g" % which); b_d = self.W("ln%d_b" % which)
        lng = B.sb("lng", [128, 8]); lnb = B.sb("lnb", [128, 8])
        sq = [B.sb("lnsq%d" % i, [128, 512]) for i in range(2)]
        ta = [B.sb("lnta%d" % i, [128, 512]) for i in range(2)]
        tv = [B.sb("lntv%d" % i, [128, 512]) for i in range(2)]
        self.vecload(lng[:, :], g_d[l, :], "lng"); self.vecload(lnb[:, :], b_d[l, :], "lnb")
        for j, (t0, n) in enumerate(TCH):
            if self.last and j == 0:
                continue
            a = (j % 2) * 2
            p0 = ps[a]; p0k = "ps%d" % a; p1 = ps[a + 1]; p1k = "ps%d" % (a + 1)
            i2 = j % 2
            for c in range(8):
                B.mm(p0[:, :n], self.onesf[:, :], xT[:, c, t0:t0 + n], c == 0, c == 7, ["onesf", self.xk(c, j)], [p0k])
            for c in range(8):
                s_ = sq[c % 2]; sk_ = "lnsq%d" % (c % 2)
                B.act(s_[:, :n], xT[:, c, t0:t0 + n], AF.Square, [self.xk(c, j)], [sk_])
                B.mm(p1[:, :n], self.onesf[:, :], s_[:, :n], c == 0, c == 7, ["onesf", sk_], [p1k])
            tak = "lnta%d" % i2; tvk = "lntv%d" % i2
            B.act(ta[i2][:, :n], p0[:, :n], AF.Square, [p0k], [tak])
            B.tt("dve", tv[i2][:, :n], p1[:, :n], ta[i2][:, :n], ALU.subtract, [p1k, tak], [tvk])
            B.act(tv[i2][:, :n], tv[i2][:, :n], AF.Ln, [tvk], [tvk], bias=self.epsc[:, :])
            B.act(tv[i2][:, :n], tv[i2][:, :n], AF.Exp, [tvk], [tvk], scale=-0.5)
            B.stt(ta[i2][:, :n], p0[:, :n], -1.0, tv[i2][:, :n], ALU.mult, ALU.mult, [p0k, tvk], [tak])
            for c in range(8):
                xs = xT[:, c, t0:t0 + n]
                B.tt("dve", xs, xs, tv[i2][:, :n], ALU.mult, [self.xk(c, j), tvk], [self.xk(c, j)])
                B.tt("pool", xs, xs, ta[i2][:, :n], ALU.add, [self.xk(c, j), tak], [self.xk(c, j)])
                B.act(xs, xs, AF.Identity, [self.xk(c, j), "lng", "lnb"], [self.xk(c, j)], scale=lng[:, c:c + 1], bias=lnb[:, c:c + 1])

    def router(self, l):
        B = self
        ps = self.ps; xT = self.xT
        self.phase()
        self.WT = WT = B.sb("WT", [32, NT])
        wr = B.sb("wr", [128, 8, 36]); rb = B.sb("rb", [128, 36])
        h2 = [B.sb("h2_%d" % i, [128, 8, 128]) for i in range(2)]
        lg = [B.sb("lg%d" % i, [128, 36]) for i in range(2)]
        me = [B.sb("me%d" % i, [128, 32]) for i in range(2)]
        rs = [B.sb("rs%d" % i, [128, 48]) for i in range(2)]
        wt_ = [B.sb("rwt%d" % i, [128, 32]) for i in range(2)]
        mk = [B.sb("rmk%d" % i, [128, 32]) for i in range(2)]
        rgw = self.W("router_g_w"); rgb = self.W("router_g_b"); rew = self.W("router_e_w"); reb = self.W("router_e_b")
        B.dmas("sp", wr[:, :, 0:4], rgw[l, :, :].rearrange("(c p) n -> p c n", p=128), (), ["wr"])
        B.dmas("sp", wr[:, :, 4:36], rew[l, :, :].rearrange("(c p) n -> p c n", p=128), (), ["wr"])
        self.bcast_load(rb[:, 0:4], rgb[l:l + 1, :], 4, "rb")
        self.bcast_load(rb[:, 4:36], reb[l:l + 1, :], 32, "rb")
        for ti in range(18):
            if self.last and ti < 2:
                continue
            i2 = ti % 2
            j = self.tj(ti)
            m_, k_ = (self.cmodx, "cmodx") if ti < 2 else (self.modx, "modx")
            hk_ = "h2_%d" % i2; lk = "lg%d" % i2; mek = "me%d" % i2; rk = "rs%d" % i2; wk_ = "rwt%d" % i2; mkk = "rmk%d" % i2
            for c in range(8):
                B.act(h2[i2][:, c, :], xT[:, c, ti * 128:(ti + 1) * 128], AF.Identity, [self.xk(c, j), k_], [hk_],
                      scale=m_[:, 32 + c:33 + c], bias=m_[:, 24 + c:25 + c])
            for c in range(8):
                B.mm(ps[i2][:, 0:36], h2[i2][:, c, :], wr[:, c, :], c == 0, c == 7, [hk_, "wr"], ["ps%d" % i2])
            L_ = lg[i2]; R_ = rs[i2]; M_ = me[i2]
            B.tt("dve", L_[:, :], ps[i2][:, 0:36], rb[:, :], ALU.add, ["ps%d" % i2, "rb"], [lk])
            B.red(R_[:, 0:1], L_[:, 0:4], ALU.max, [lk], [rk])
            B.ts("dve", R_[:, 1:2], R_[:, 0:1], -1.0, None, ALU.mult, None, [rk], [rk])
            B.ts("dve", R_[:, 8:12], L_[:, 0:4], R_[:, 0:1], None, ALU.is_equal, None, [lk, rk], [rk])
            B.act(R_[:, 12:16], L_[:, 0:4], AF.Exp, [lk, rk], [rk], bias=R_[:, 1:2])
            B.red(R_[:, 2:3], R_[:, 12:16], ALU.add, [rk], [rk])
            B.recip(R_[:, 3:4], R_[:, 2:3], [rk], [rk])
            B.ts("dve", R_[:, 16:20], R_[:, 8:12], -1.0, 1.0e30, ALU.add, ALU.mult, [rk], [rk])
            B.tt("dve", M_[:, :].rearrange("p (a b) -> p a b", b=8), L_[:, 4:36].rearrange("p (a b) -> p a b", b=8),
                 XAP(R_, 16, [[48, 128], [1, 4], [0, 8]]), ALU.add, [lk, rk], [mek])
            B.P.op("dve", lambda e, R_=R_, M_=M_: e.max(out=R_[:, 24:32], in_=M_[:, :]), [mek], [rk])
            B.ts("dve", mk[i2][:, :], M_[:, :], R_[:, 24:25], None, ALU.is_equal, None, [mek, rk], [mkk])
            B.tt("dve", R_[:, 4:5], R_[:, 25:26], R_[:, 24:25], ALU.subtract, [rk], [rk])
            B.act(R_[:, 5:6], R_[:, 4:5], AF.Exp, [rk], [rk])
            B.ts("dve", R_[:, 6:7], R_[:, 5:6], 1.0, None, ALU.add, None, [rk], [rk])
            B.recip(R_[:, 6:7], R_[:, 6:7], [rk], [rk])
            B.tt("dve", R_[:, 7:8], R_[:, 5:6], R_[:, 6:7], ALU.mult, [rk], [rk])
            B.tt("dve", R_[:, 32:33], R_[:, 6:7], R_[:, 3:4], ALU.mult, [rk], [rk])
            B.tt("dve", R_[:, 33:34], R_[:, 7:8], R_[:, 3:4], ALU.mult, [rk], [rk])
            B.ts("dve", wt_[i2][:, :], mk[i2][:, :], R_[:, 32:33], None, ALU.mult, None, [mkk, rk], [wk_])
            B.ts("dve", mk[i2][:, :], M_[:, :], R_[:, 25:26], None, ALU.is_equal, None, [mek, rk, wk_], [mkk])
            B.stt(wt_[i2][:, :], mk[i2][:, :], R_[:, 33:34], wt_[i2][:, :], ALU.mult, ALU.add, [mkk, rk, wk_], [wk_])
            B.tr(ps[2 + i2][0:32, 0:128], wt_[i2][:, :], self.identf[:, :], [wk_, "identf"], ["ps%d" % (2 + i2)])
            B.cp("act", WT[:, ti * 128:(ti + 1) * 128], ps[2 + i2][0:32, 0:128], ["ps%d" % (2 + i2)], ["WT_%d" % j])
        if "rt" in self.dbg and l == 0:
            self.dump("d_WT", WT[:, :], [32, NT], ["WT_%d" % j for j in range(5)])

    def moe(self, l):
        B = self
        ps = self.ps; xT = self.xT; hT = self.hT; WT = self.WT
        mw1 = self.W("moe_w1"); mw3 = self.W("moe_w3"); mw2 = self.W("moe_w2")
        self.sb_ptr = self.ph_base + 32 * 0 + NT * 4
        w1 = [B.sb("w1_%d" % i, [128, 8, 512], BF16) for i in range(2)]
        w3 = [B.sb("w3_%d" % i, [128, 8, 512], BF16) for i in range(2)]
        w2 = [B.sb("w2_%d" % i, [128, 4, 1024], BF16) for i in range(2)]
        gb = [B.sb("gb%d" % i, [128, 4, 512], BF16) for i in range(2)]
        s1 = [B.sb("s1_%d" % i, [128, 512]) for i in range(2)]
        s2 = [B.sb("s2_%d" % i, [128, 512]) for i in range(2)]
        self.P.fence()
        cnt = 0
        for e in range(32):
            i2 = e % 2
            B.dma(WQ, w1[i2][:, :, :], mw1[l, e, :, :].rearrange("(c p) n -> p c n", p=128), (), ["w1_%d" % i2])
            B.dma(WQ, w3[i2][:, :, :], mw3[l, e, :, :].rearrange("(c p) n -> p c n", p=128), (), ["w3_%d" % i2])
            B.dma(WQ, w2[i2][:, :, :], mw2[l, e, :, :].rearrange("(c p) n -> p c n", p=128), (), ["w2_%d" % i2])
            selT = XAP(self.identf, e, [[128, 32], [0, 128]])
            for j, (t0, n) in enumerate(TCH):
                if self.last and j == 0:
                    continue
                pw = ps[j % 2]; pwk = "ps%d" % (j % 2)
                B.mm(pw[:, :n], selT, WT[:, t0:t0 + n], True, True, ["identf", "WT_%d" % j], [pwk])
                g_ = gb[j % 2]
                for hc in range(4):
                    p1 = ps[2 + hc % 2]; p1k = "ps%d" % (2 + hc % 2)
                    p3 = ps[4 + hc % 2]; p3k = "ps%d" % (4 + hc % 2)
                    for kc in range(8):
                        B.mm(p1[:, :n], w1[i2][:, kc, hc * 128:(hc + 1) * 128], hT[:, kc, t0:t0 + n], kc == 0, kc == 7,
                             ["w1_%d" % i2, self.hk(kc, j)], [p1k])
                    for kc in range(8):
                        B.mm(p3[:, :n], w3[i2][:, kc, hc * 128:(hc + 1) * 128], hT[:, kc, t0:t0 + n], kc == 0, kc == 7,
                             ["w3_%d" % i2, self.hk(kc, j)], [p3k])
                    B.act(s1[hc % 2][:, :n], p1[:, :n], AF.Silu, [p1k], ["s1_%d" % (hc % 2)])
                    B.tt("dve", s2[hc % 2][:, :n], s1[hc % 2][:, :n], p3[:, :n], ALU.mult, ["s1_%d" % (hc % 2), p3k], ["s2_%d" % (hc % 2)])
                    B.tt("dve", g_[:, hc, :n], s2[hc % 2][:, :n], pw[:, :n], ALU.mult, ["s2_%d" % (hc % 2), pwk], ["gb%d_%d" % (j % 2, hc)])
                for o in range(8):
                    py = ps[6 + cnt % 2]; pyk = "ps%d" % (6 + cnt % 2)
                    cnt += 1
                    for hc in range(4):
                        B.mm(py[:, :n], w2[i2][:, hc, o * 128:(o + 1) * 128], g_[:, hc, :n], hc == 0, hc == 3,
                             ["w2_%d" % i2, "gb%d_%d" % (j % 2, hc)], [pyk])
                    m_, k_ = (self.cmodx, "cmodx") if j == 0 else (self.modx, "modx")
                    B.stt(xT[:, o, t0:t0 + n], py[:, :n], m_[:, 40 + o:41 + o], xT[:, o, t0:t0 + n], ALU.mult, ALU.add,
                          [pyk, k_, self.xk(o, j)], [self.xk(o, j)])

    def ssm(self, l):
        B = self
        ps = self.ps; hT = self.hT
        NLV = 12
        self.phase()
        self.ssmT = ssmT = B.sb("ssmT", [128, 4, NT], BF16)
        uT = B.sb("uT", [128, 4, NT], BF16)
        ARn = [B.sb("ARn%d" % d, [128, NLV, 32]) for d in range(2)]
        OFn = [B.sb("OFn%d" % d, [128, NLV, 32]) for d in range(2)]
        BT = [B.sb("BT%d" % d, [128, 4, 128], BF16) for d in range(2)]
        Cst = [B.sb("Cst%d" % d, [128, 512], BF16) for d in range(2)]
        dsk = B.sb("dsk", [128, 4]); bglu = B.sb("bglu", [128, 4])
        main_base = self.sb_ptr
        uk = lambda c, j: "uT%d_%d" % (c, j)
        w_in = self.W("w_in")
        self.vecload(dsk[:, :], self.W("ssm_d")[l, :], "dsk")
        self.vecload(bglu[:, :], self.W("b_glu")[l, :], "bglu")
        wu = [B.sb("wu%d" % i, [128, 8, 128], BF16) for i in range(2)]
        for oc in range(4):
            i2 = oc % 2
            B.dma(WQ, wu[i2][:, :, :], w_in[l, :, 2048 + oc * 128:2048 + (oc + 1) * 128].rearrange("(c p) n -> p c n", p=128), (), ["wu%d" % i2])
            self.proj_fm(wu[i2], "wu%d" % i2, 8, hT, self.hk, lambda t0, n, oc=oc: uT[:, oc, t0:t0 + n], lambda j, oc=oc: uk(oc, j))
        if "u" in self.dbg and l == 0:
            self.dump("d_uT", uT[:, :, :], [128, 4, NT], [uk(c, j) for c in range(4) for j in range(5)], BF16)
        a_re = self.W("ssm_a_re"); a_im = self.W("ssm_a_im"); ldt = self.W("ssm_log_dt")
        b_re = self.W("ssm_b_re"); b_im = self.W("ssm_b_im"); c_re = self.W("ssm_c_re"); c_im = self.W("ssm_c_im")
        anat = B.sb("anat", [32, 128])
        are2 = B.sb("are2", [128, 32]); aim2 = B.sb("aim2", [128, 32]); dt2 = B.sb("dt2", [128, 32])
        zre = B.sb("zre", [128, 32]); zim = B.sb("zim", [128, 32])
        NV = NLV * 32
        zn = B.sb("zn", [128, NLV, 32]); yy = B.sb("yy", [128, NLV, 32]); yi = B.sb("yi", [128, NLV, 32], I32)
        yf = B.sb("yf", [128, NLV, 32]); ff = B.sb("ff", [128, NLV, 32]); sv = B.sb("sv", [128, NLV, 32]); cvv = B.sb("cvv", [128, NLV, 32])
        AIn = B.sb("AIn", [128, NLV, 32])
        cf = B.sb("cf", [128, 12, 32])
        X1 = B.sb("X1", [128, 32, 16]); X2 = B.sb("X2", [128, 32, 16]); Bst = B.sb("Bst", [128, 512]); Btmp = B.sb("Btmp", [128, 512])
        Cnat = B.sb("Cnat", [128, 4, 128])
        fl = lambda t: t[:, :, :].rearrange("p a b -> p (a b)")
        for d in range(2):
            kd = "_%d" % d
            for src_, dst_, kk in ((a_re, are2, "are2"), (a_im, aim2, "aim2")):
                for half in range(2):
                    B.dma("sp", anat[:, half * 64:(half + 1) * 64], src_[l, d, :, :], (), ["anat"])
                B.tr(ps[4][:, 0:32], anat[:, :], self.identf[0:32, 0:32], ["anat", "identf"], ["ps4"])
                B.cp("dve", dst_[:, :], ps[4][:, 0:32], ["ps4"], [kk])
            self.bcast_load(dt2[:, :], ldt[l, d:d + 1, :], 32, "dt2")
            B.act(dt2[:, :], dt2[:, :], AF.Exp, ["dt2"], ["dt2"])
            B.tt("dve", zre[:, :], are2[:, :], dt2[:, :], ALU.mult, ["are2", "dt2"], ["zre"])
            B.tt("dve", zim[:, :], aim2[:, :], dt2[:, :], ALU.mult, ["aim2", "dt2"], ["zim"])
            for i in range(NLV):
                n_ = float(2 ** i)
                B.ts("dve", zn[:, i, :], zre[:, :], n_, None, ALU.mult, None, ["zre"], ["zn"])
                B.ts("dve", yy[:, i, :], zim[:, :], n_ / (2.0 * math.pi), 8.5, ALU.mult, ALU.add, ["zim"], ["yy"])
            B.act(fl(zn), fl(zn), AF.Exp, ["zn"], ["zn"])
            for which, dst in ((0, sv), (1, cvv)):
                if which == 1:
                    B.ts("dve", fl(yy), fl(yy), 0.25, None, ALU.add, None, ["yy"], ["yy"])
                B.cp("dve", fl(yi), fl(yy), ["yy"], ["yi"])
                B.cp("dve", fl(yf), fl(yi), ["yi"], ["yf"])
                B.tt("dve", fl(ff), fl(yy), fl(yf), ALU.subtract, ["yy", "yf"], ["ff"])
                B.stt(fl(ff), fl(ff), 0.0, fl(ff), ALU.is_lt, ALU.add, ["ff"], ["ff"])
                B.ts("dve", fl(ff), fl(ff), 1.0, None, ALU.min, None, ["ff"], ["ff"])
                B.act(fl(dst), fl(ff), AF.Sin, ["ff", "negpi"], ["sc%d" % which], scale=2.0 * math.pi, bias=self.negpi[:, :])
            B.tt("dve", fl(ARn[d]), fl(zn), fl(cvv), ALU.mult, ["zn", "sc1"], ["ARn" + kd])
            B.tt("dve", fl(AIn), fl(zn), fl(sv), ALU.mult, ["zn", "sc0"], ["AIn"])
            B.ts("dve", fl(OFn[d]), fl(AIn), self.sgnf[:, 0:1], None, ALU.mult, None, ["AIn", "sgnf"], ["OFn" + kd])
            a1r = ARn[d][:, 0, :]; a1i = AIn[:, 0, :]
            c_ = lambda i: cf[:, i, :]
            B.ts("dve", c_(0), a1r, -1.0, None, ALU.add, None, ["ARn" + kd], ["cf"])
            B.tt("dve", c_(1), c_(0), are2[:, :], ALU.mult, ["cf", "are2"], ["cf"])
            B.tt("dve", c_(2), a1i, aim2[:, :], ALU.mult, ["AIn", "aim2"], ["cf"])
            B.tt("dve", c_(1), c_(1), c_(2), ALU.add, ["cf"], ["cf"])
            B.tt("dve", c_(3), a1i, are2[:, :], ALU.mult, ["AIn", "are2"], ["cf"])
            B.tt("dve", c_(4), c_(0), aim2[:, :], ALU.mult, ["cf", "aim2"], ["cf"])
            B.tt("dve", c_(3), c_(3), c_(4), ALU.subtract, ["cf"], ["cf"])
            B.tt("dve", c_(5), are2[:, :], are2[:, :], ALU.mult, ["are2"], ["cf"])
            B.tt("dve", c_(6), aim2[:, :], aim2[:, :], ALU.mult, ["aim2"], ["cf"])
            B.tt("dve", c_(5), c_(5), c_(6), ALU.add, ["cf"], ["cf"])
            B.recip(c_(5), c_(5), ["cf"], ["cf"])
            B.tt("dve", c_(7), c_(1), c_(5), ALU.mult, ["cf"], ["cf"])
            B.tt("dve", c_(8), c_(3), c_(5), ALU.mult, ["cf"], ["cf"])
            B.ts("dve", c_(8), c_(8), self.sgnf[:, 1:2], None, ALU.mult, None, ["cf", "sgnf"], ["cf"])
            for q4 in range(4):
                gs = slice(q4 * 8, (q4 + 1) * 8)
                B.dmas("sp", X1[0:64, gs, :], b_re[l, d, gs, :, :].rearrange("g p c -> p g c"), (), ["X1"])
                B.dmas("sp", X1[64:128, gs, :], b_im[l, d, gs, :, :].rearrange("g p c -> p g c"), (), ["X1"])
                B.dmas("sp", X2[0:64, gs, :], b_im[l, d, gs, :, :].rearrange("g p c -> p g c"), (), ["X2"])
                B.dmas("sp", X2[64:128, gs, :], b_re[l, d, gs, :, :].rearrange("g p c -> p g c"), (), ["X2"])
            crb = XAP(cf, 7 * 32, [[12 * 32, 128], [1, 32], [0, 16]])
            cib = XAP(cf, 8 * 32, [[12 * 32, 128], [1, 32], [0, 16]])
            v3 = lambda t: t[:, :].rearrange("p (g c) -> p g c", c=16)
            B.tt("dve", v3(Bst), X1[:, :, :], crb, ALU.mult, ["X1", "cf"], ["Bst"])
            B.tt("dve", v3(Btmp), X2[:, :, :], cib, ALU.mult, ["X2", "cf"], ["Btmp"])
            B.tt("dve", Bst[:, :], Bst[:, :], Btmp[:, :], ALU.add, ["Bst", "Btmp"], ["Bst"])
            for k in range(4):
                B.tr(ps[k % 2][:, 0:128], Bst[:, k * 128:(k + 1) * 128], self.identf[:, :], ["Bst", "identf"], ["ps%d" % (k % 2)])
                B.cp("act", BT[d][:, k, :], ps[k % 2][:, 0:128], ["ps%d" % (k % 2)], ["BT" + kd])
            B.dma("sp", Cnat[:, :, 0:64], c_re[l, d, :, :, :].rearrange("(k a) c p -> (a c) k p", k=4), (), ["Cnat"])
            B.dma("sp", Cnat[:, :, 64:128], c_im[l, d, :, :, :].rearrange("(k a) c p -> (a c) k p", k=4), (), ["Cnat"])
            for k in range(4):
                B.tr(ps[2 + k % 2][:, 0:128], Cnat[:, k, :], self.identf[:, :], ["Cnat", "identf"], ["ps%d" % (2 + k % 2)])
                B.cp("dve", Cst[d][0:64, k * 128:(k + 1) * 128], ps[2 + k % 2][0:64, 0:128], ["ps%d" % (2 + k % 2)], ["Cst" + kd])
                B.act(Cst[d][64:128, k * 128:(k + 1) * 128], ps[2 + k % 2][64:128, 0:128], AF.Copy, ["ps%d" % (2 + k % 2)], ["Cst" + kd], scale=-1.0)
        if "coef" in self.dbg and l == 0:
            self.dump("d_ARn0", ARn[0][:, :, :], [128, NLV, 32], ["ARn_0"]); self.dump("d_OFn0", OFn[0][:, :, :], [128, NLV, 32], ["OFn_0"])
            self.dump("d_Cst0", Cst[0][:, :], [128, 512], ["Cst_0"]); self.dump("d_BT0", BT[0][:, :, :], [128, 4, 128], ["BT_0"], BF16)
        self.P.fence()
        NG = 4
        self.sb_ptr = self.ph_base
        Cbuf = [B.sb("Cbuf%d" % s, [128, 8 * 144], BF16) for s in range(NG)]
        Spad = [B.sb("Spad%d" % s, [128, 146], BF16) for s in range(NG)]
        BTm = [B.sb("BTm%d" % s, [128, 128], BF16) for s in range(NG)]
        Cpj = [B.sb("Cpj%d" % s, [128, 128], BF16) for s in range(NG)]
        assert self.sb_ptr <= self.ph_base + 4 * NT * 2
        self.sb_ptr = main_base
        Hb = [B.sb("Hb%d" % s, [128, NT], BF16) for s in range(NG)]
        yacc = B.sb("yacc", [128, NT])
        Dm = [[B.sb("Dm%d_%d" % (s, i), [128, 128], BF16) for i in range(2)] for s in range(NG)]
        gt = [B.sb("gt%d" % i, [128, 256]) for i in range(2)]
        for s_ in range(NG):
            B.memset("pool", Spad[s_][:, :], 0.0, ["Sp%d" % s_])
        CH = [(c * 512, min(512, NT - c * 512)) for c in range(5)]
        NC16 = NT // 16

        def cmap(d, t0):
            c0 = t0 // 16
            if d == 0:
                return c0
            return c0 - 16 if t0 >= NCTX else 128 + c0

        def hkeys(s, a, b):
            return ["H%d_%d" % (s, c) for c in range(a // 512, (b - 1) // 512 + 1)]

        def job(s, k, g8, d):
            g = k * 8 + g8
            H = Hb[s]; Cb = Cbuf[s]; Sp = Spad[s]; btm = BTm[s]; cpj = Cpj[s]
            spk = "Sp%d" % s; cbk = "Cb%d" % s
            pA = ps[2 * s]; pAk = "ps%d" % (2 * s); pB = ps[2 * s + 1]; pBk = "ps%d" % (2 * s + 1)
            banks = [(pA, pAk), (pB, pBk)]
            allH = hkeys(s, 0, NT)
            dcnt = [0]

            def build_D(i):
                dm = Dm[s][dcnt[0] % 2]; dmk = "Dm%d_%d" % (s, dcnt[0] % 2)
                dcnt[0] += 1
                B.ts("pool", dm[:, :], self.identf[:, :], ARn[d][:, i, g:g + 1], None, ALU.mult, None, ["identf", "ARn_%d" % d], [dmk])
                B.stt(dm[:, :], self.swpf[:, :], OFn[d][:, i, g:g + 1], dm[:, :], ALU.mult, ALU.add, ["swpf", "OFn_%d" % d, dmk], [dmk])
                return dm, dmk

            B.ts("pool", btm[:, :], BT[d][:, k, :], self.gmaskf[:, g8:g8 + 1], None, ALU.mult, None, ["BT_%d" % d, "gmaskf"], ["BTm%d" % s])
            B.memset("pool", cpj[:, :], 0.0, ["Cpj%d" % s])
            B.cp("pool", cpj[:, g8 * 16:(g8 + 1) * 16], Cst[d][:, g * 16:(g + 1) * 16], ["Cst_%d" % d, "Cpj%d" % s], ["Cpj%d" % s])
            for j, (t0, n) in enumerate(TCH):
                pb, pk = banks[j % 2]
                B.mm(pb[:, :n], btm[:, :], uT[:, k, t0:t0 + n], True, True, ["BTm%d" % s, uk(k, j)], [pk])
                B.cp("act" if j % 2 else "dve", XAP(H, cmap(d, t0), [[NT, 128], [1, n // 16], [NC16, 16]]),
                     pb[:, :n].rearrange("p (c s) -> p c s", s=16), [pk], allH)
                yield
            for i in range(4):
                sh = (2 ** i) * NC16
                dm, dmk = build_D(i)
                order = list(range(4, -1, -1)) if d == 0 else list(range(5))
                for ci, c in enumerate(order):
                    lo, n = CH[c]; hi = lo + n
                    pb, pk = banks[ci % 2]
                    if d == 0:
                        a = max(lo, sh); b = hi
                        has = b > a
                        sa, sb_ = a - sh, b - sh
                    else:
                        a = lo; b = min(hi, NT - sh)
                        has = b > a
                        sa, sb_ = a + sh, b + sh
                    if not has:
                        continue
                    if ci % 2 == 0:
                        B.mm(pb[:, :n], self.identb[:, :], H[:, lo:hi], True, False, ["identb"] + hkeys(s, lo, hi), [pk])
                        B.mm(pb[:, a - lo:b - lo], dm[:, :], H[:, sa:sb_], False, True, [dmk] + hkeys(s, sa, sb_), [pk], skip_group_check=True)
                        B.cp("act", H[:, lo:hi], pb[:, :n], [pk], hkeys(s, lo, hi))
                    else:
                        B.mm(pb[:, a - lo:b - lo], dm[:, :], H[:, sa:sb_], True, True, [dmk] + hkeys(s, sa, sb_), [pk])
                        B.tt("dve", H[:, a:b], H[:, a:b], pb[:, a - lo:b - lo], ALU.add, [pk] + hkeys(s, lo, hi), hkeys(s, lo, hi))
                    yield
            zlo = 15 * NC16 if d == 0 else 0
            B.cp("act", Sp[:, 1:1 + NC16], H[:, zlo:zlo + NC16], hkeys(s, zlo, zlo + NC16), [spk])
            yield
            for i in range(4, 12):
                s2 = 2 ** (i - 4)
                m_ = NC16 - s2
                dm, dmk = build_D(i)
                pb, pk = banks[i % 2]
                if d == 0:
                    B.mm(pb[:, 0:m_], dm[:, :], Sp[:, 1:1 + m_], True, True, [dmk, spk], [pk])
                    B.tt("dve", Sp[:, 1 + s2:1 + NC16], Sp[:, 1 + s2:1 + NC16], pb[:, 0:m_], ALU.add, [pk, spk], [spk])
                else:
                    B.mm(pb[:, 0:m_], dm[:, :], Sp[:, 1 + s2:1 + NC16], True, True, [dmk, spk], [pk])
                    B.tt("dve", Sp[:, 1:1 + m_], Sp[:, 1:1 + m_], pb[:, 0:m_], ALU.add, [pk, spk], [spk])
                yield
            sprev = Sp[:, 0:NC16] if d == 0 else Sp[:, 2:2 + NC16]
            hcol = (lambda q: q) if d == 0 else (lambda q: 15 - q)
            steps = [(0, None, 0, 1), (0, 0, 1, 1), (1, 0, 2, 2), (2, 0, 4, 4), (3, 0, 8, 8)]
            pc = 0
            for (pi, srcb, dstb, nb_) in steps:
                dm, dmk = build_D(pi)
                done = 0
                while done < nb_:
                    nn = min(3, nb_ - done)
                    pb, pk = banks[pc % 2]; pc += 1
                    if srcb is None:
                        src = sprev; srck = [spk]
                    else:
                        src = Cb[:, (srcb + done) * NC16:(srcb + done + nn) * NC16]; srck = [cbk]
                    B.mm(pb[:, 0:nn * NC16], dm[:, :], src, True, True, [dmk] + srck, [pk])
                    if dstb + done < 8:
                        B.cp("act", Cb[:, (dstb + done) * NC16:(dstb + done + nn) * NC16], pb[:, 0:nn * NC16], [pk], [cbk])
                    for q in range(nn):
                        hc_ = hcol(dstb + done + q) * NC16
                        B.tt("dve", H[:, hc_:hc_ + NC16], H[:, hc_:hc_ + NC16], pb[:, q * NC16:(q + 1) * NC16], ALU.add,
                             [pk] + hkeys(s, hc_, hc_ + NC16), hkeys(s, hc_, hc_ + NC16))
                    done += nn
                    yield
            for j, (t0, n) in enumerate(TCH):
                if self.last and j == 0:
                    continue
                pb, pk = banks[j % 2]
                B.mm(pb[:, :n], cpj[:, :], XAP(H, cmap(d, t0), [[NT, 128], [1, n // 16], [NC16, 16]]), True, True, ["Cpj%d" % s] + allH, [pk])
                B.tt("dve", yacc[:, t0:t0 + n], yacc[:, t0:t0 + n], pb[:, :n], ALU.add, ["yacc_%d" % j, pk], ["yacc_%d" % j])
                yield

        for k in range(4):
            for j, (t0, n) in enumerate(TCH):
                if self.last and j == 0:
                    continue
                B.ts("dve", yacc[:, t0:t0 + n], uT[:, k, t0:t0 + n], dsk[:, k:k + 1], None, ALU.mult, None, [uk(k, j), "dsk"], ["yacc_%d" % j])
            pending = [(g8, d) for g8 in range(8) for d in range(2)]
            active = {}
            free = list(range(NG))
            STAG = 12
            since = STAG
            while pending or active:
                if pending and free and (not active or since >= STAG):
                    s_ = free.pop(0)
                    g8_, d_ = pending.pop(0)
                    active[s_] = job(s_, k, g8_, d_)
                    since = 0
                for s_ in list(active):
                    try:
                        next(active[s_])
                    except StopIteration:
                        del active[s_]
                        free.append(s_)
                since += 1
            if "ssm" in self.dbg and l == 0:
                self.dump("d_yacc%d" % k, yacc[:, :], [128, NT], ["yacc_%d" % j for j in range(5)])
            for j, (t0_, n_) in enumerate(TCH):
                if self.last and j == 0:
                    continue
                for t0 in range(t0_, t0_ + n_, 256):
                    n = 256
                    ya = yacc[:, t0:t0 + n]
                    ga, gb_ = gt[0], gt[1]
                    gak, gbk = "gt0", "gt1"
                    B.act(ga[:, :n], ya, AF.Square, ["yacc_%d" % j], [gak])
                    B.ts("dve", ga[:, :n], ga[:, :n], 0.044715, 1.0, ALU.mult, ALU.add, [gak], [gak])
                    B.tt("pool", ga[:, :n], ga[:, :n], ya, ALU.mult, [gak, "yacc_%d" % j], [gak])
                    B.act(gb_[:, :n], ga[:, :n], AF.Sigmoid, [gak], [gbk], scale=1.5957691216057308)
                    B.tt("dve", uT[:, k, t0:t0 + n], ya, gb_[:, :n], ALU.mult, ["yacc_%d" % j, gbk], [uk(k, j)])
        self.P.fence()
        self.sb_ptr = main_base
        wg = B.sb("wglu", [128, 4, 512], BF16)
        sgl = [B.sb("sgl%d" % i, [128, 512]) for i in range(2)]
        B.dma(WQ, wg[:, :, :], self.W("w_glu")[l, :, :].rearrange("(c p) n -> p c n", p=128), (), ["wglu"])
        cnt = 0
        for oc in range(4):
            for j, (t0, n) in enumerate(TCH):
                if self.last and j == 0:
                    continue
                pb = ps[cnt % 4]; pk = "ps%d" % (cnt % 4)
                sg_ = sgl[cnt % 2]; sgk = "sgl%d" % (cnt % 2)
                cnt += 1
                for kc in range(4):
                    B.mm(pb[:, :n], wg[:, kc, oc * 128:(oc + 1) * 128], uT[:, kc, t0:t0 + n], kc == 0, kc == 3, ["wglu", uk(kc, j)], [pk])
                B.act(sg_[:, :n], pb[:, :n], AF.Sigmoid, [pk, "bglu"], [sgk], bias=bglu[:, oc:oc + 1])
                B.tt("dve", ssmT[:, oc, t0:t0 + n], uT[:, oc, t0:t0 + n], sg_[:, :n], ALU.mult, [uk(oc, j), sgk], ["ss%d_%d" % (oc, j)])
        if "glu" in self.dbg and l == 0:
            self.dump("d_ssmT", ssmT[:, :, :], [128, 4, NT], ["ss%d_%d" % (c, j) for c in range(4) for j in range(5)], BF16)
        self.phase(reserve=4 * NT * 2)


def _consts():
    ident = np.eye(128, dtype=np.float32)
    rmat = np.zeros((128, 128), np.float32)
    for m in range(128):
        d = m % 32
        if d < 16:
            rmat[m + 16, m] = -1.0
        else:
            rmat[m - 16, m] = 1.0
    t = np.arange(NLAT)
    row = (t // 64).astype(np.float32)
    col = (t % 64).astype(np.float32)
    inv = (1.0 / (np.float32(10000.0) ** (np.arange(0, 32, 2, dtype=np.float32) / np.float32(32.0)))).astype(np.float32)
    cos = np.zeros((128, NLAT), np.float32)
    sin = np.zeros((128, NLAT), np.float32)
    for p in range(128):
        d = p % 64
        axis = d // 32
        f = d % 16
        posv = row if axis == 0 else col
        ang = (posv * inv[f]).astype(np.float32)
        cos[p] = np.cos(ang)
        sin[p] = np.sin(ang)
    swp = np.zeros((128, 128), np.float32)
    for k in range(128):
        swp[k, (k + 64) % 128] = 1.0
    gmask = np.zeros((128, 8), np.float32)
    for p in range(128):
        gmask[p, p // 16] = 1.0
    sgn = np.ones((128, 2), np.float32)
    sgn[64:, 0] = -1.0
    sgn[:64, 1] = -1.0
    return dict(ident=ident, rmat=rmat, ropecos=cos, ropesin=sin, swp=swp, gmask=gmask, sgn=sgn)


_CACHE = {}


def _get_prog(n_layers, dbg, skip):
    key = (n_layers, tuple(dbg), tuple(skip))
    if key not in _CACHE:
        b = Builder(n_layers, dbg, skip)
        b.build()
        _CACHE[key] = b
    return _CACHE[key]


def _in_maps(inputs, names, cores):
    f = lambda a: np.ascontiguousarray(np.asarray(a, dtype=np.float32))
    skipk = ("x", "c", "ctx", "c_ctx", "lam_q1", "lam_k1", "lam_q2", "lam_k2")
    shared = {k: f(v) for k, v in inputs.items() if k not in skipk and k in names}
    if "lamv" in names:
        shared["lamv"] = np.ascontiguousarray(np.stack([f(inputs["lam_q1"]), f(inputs["lam_k1"]), f(inputs["lam_q2"]), f(inputs["lam_k2"])], axis=1))
    for k, v in _consts().items():
        if k in names:
            shared[k] = v
    maps = []
    for b in range(cores):
        m = dict(shared)
        m["x"] = f(inputs["x"][b])
        m["ctx"] = f(inputs["ctx"][b])
        m["cvec"] = np.ascontiguousarray(np.stack([f(inputs["c"][b]), f(inputs["c_ctx"])], axis=0))
        maps.append(m)
    return maps


def run(inputs, n_layers=DEPTH, dbg=(), skip=(), cores=8):
    b = _get_prog(n_layers, dbg, skip)
    in_names = set(k for k, v in b.dram.items())
    maps = _in_maps(inputs, in_names, cores)
    maps = [{k: v for k, v in m.items() if k in in_names} for m in maps]
    res = run_bass_kernel_spmd(b.nc, maps, core_ids=list(range(cores)))
    return res.results


def kernel(**inputs):
    res = run(inputs)
    return np.stack([np.asarray(r["out"], dtype=np.float32) for r in res], axis=0)
```

```python
import math
import numpy as np
import concourse.bass as bass
import concourse.mybir as mybir
from concourse.bass_utils import run_bass_kernel_spmd

F32 = mybir.dt.float32
BF16 = mybir.dt.bfloat16
I32 = mybir.dt.int32
AF = mybir.ActivationFunctionType
ALU = mybir.AluOpType
AX = mybir.AxisListType

DEPTH = 4
D = 1024
NCTX = 256
NLAT = 2048
NT = NCTX + NLAT
ALPHA = (2.0 * DEPTH) ** 0.25
LN_EPS = 1e-5
TCH = [(0, 256), (256, 512), (768, 512), (1280, 512), (1792, 512)]
ENGS = ("pe", "act", "dve", "pool", "sp")
WQ = "pool"
SB_BASE = 16512
SB_TOP = 229344


class Prog:
    def __init__(self, nc, n_dma_sems=8, same_engine_sync=True):
        self.nc = nc
        self.ops = []
        self.res = {}
        self.same = same_engine_sync
        self.n_dma_sems = n_dma_sems
        self.dma_rr = {e: 0 for e in ENGS}
        self.dma_last = {}
        self.last_op = {}
        self.open_dmas = []
        self.fence_deps = set()

    def op(self, eng, fn, rd=(), wr=(), kind="c"):
        wr = list(wr) + [r for r in rd if r.startswith("ps") and r not in wr]
        deps = set()
        for r in rd:
            st = self.res.get(r)
            if st is not None and st[0] is not None:
                deps.add(st[0])
        for r in wr:
            st = self.res.get(r)
            if st is not None:
                if st[0] is not None:
                    deps.add(st[0])
                for o in st[1].values():
                    deps.add(o)
        deps.update(self.fence_deps)
        oid = len(self.ops)
        rec = dict(eng=eng, fn=fn, deps=deps, kind=kind, slot=None)
        if kind == "dma":
            self.open_dmas.append(oid)
        else:
            self.last_op[eng] = oid
        if kind == "dma":
            slot = self.dma_rr[eng] % self.n_dma_sems
            self.dma_rr[eng] += 1
            rec["slot"] = slot
            prev = self.dma_last.get((eng, slot))
            if prev is not None:
                deps.add(prev)
            self.dma_last[(eng, slot)] = oid
        self.ops.append(rec)
        for r in rd:
            st = self.res.setdefault(r, [None, {}])
            st[1][(eng, oid if kind == "dma" else -1)] = oid
        for r in wr:
            self.res[r] = [oid, {}]
        return oid

    def fence(self):
        f = set(self.last_op.values())
        f.update(self.open_dmas)
        self.open_dmas = []
        self.fence_deps = f
        self.res = {}

    def dma(self, eng, out, in_, rd=(), wr=(), **kw):
        return self.op(eng, lambda e: e.dma_start(out=out, in_=in_, **kw), rd, wr, kind="dma")

    def emit(self, final_wait_ops=()):
        nc = self.nc
        ops = self.ops
        needed = set()
        for o in ops:
            needed.update(o["deps"])
        needed.update(final_wait_ops)
        esem = {e: nc.alloc_semaphore("s_" + e) for e in ENGS}
        dsem = {}
        for (e, s) in self.dma_last:
            dsem[(e, s)] = nc.alloc_semaphore("d_%s%d" % (e, s))
        ecount = {e: 0 for e in ENGS}
        dcount = {k: 0 for k in dsem}
        for i, o in enumerate(ops):
            if o["kind"] == "dma":
                k = (o["eng"], o["slot"])
                dcount[k] += 16
                o["sig"] = (dsem[k], dcount[k], ("d",) + k)
            elif i in needed:
                ecount[o["eng"]] += 1
                o["sig"] = (esem[o["eng"]], ecount[o["eng"]], ("e", o["eng"]))
            else:
                o["sig"] = None
        streams = {e: [] for e in ENGS}
        seen = {e: {} for e in ENGS}
        for i, o in enumerate(ops):
            e = o["eng"]
            waits = {}
            for d in o["deps"]:
                od = ops[d]
                if od["kind"] != "dma" and od["eng"] == e and (e == "pe" or not self.same):
                    continue
                sem, val, key = od["sig"]
                if seen[e].get(key, 0) >= val:
                    continue
                if key not in waits or waits[key][1] < val:
                    waits[key] = (sem, val)
            for key, (sem, val) in waits.items():
                seen[e][key] = val
            streams[e].append((list(waits.values()), o))
        fin = [(ops[d]["sig"][0], ops[d]["sig"][1]) for d in final_wait_ops]
        self.n_instr = {e: len(streams[e]) for e in ENGS}

        def run(engname, engobj):
            for waits, o in streams[engname]:
                for sem, val in waits:
                    engobj.wait_ge(sem, val)
                ins = o["fn"](engobj)
                if o["sig"] is not None:
                    ins.then_inc(o["sig"][0], 16 if o["kind"] == "dma" else 1)
            if engname == "sp":
                for sem, val in fin:
                    engobj.wait_ge(sem, val)

        with nc.Block() as block:
            @block.tensor
            def _(t):
                run("pe", t)

            @block.scalar
            def _(t):
                run("act", t)

            @block.vector
            def _(t):
                run("dve", t)

            @block.gpsimd
            def _(t):
                run("pool", t)

            @block.sync
            def _(t):
                run("sp", t)


def XAP(t, offset, dims):
    return bass.AP(t, offset, [list(d) for d in dims])


class Builder:
    def __init__(self, n_layers=DEPTH, dbg=(), skip=()):
        self.stop = [x[5:] for x in skip if x.startswith("stop:")]
        self.n_layers = n_layers
        self.dbg = set(dbg)
        self.skip = set(skip)
        self.nc = bass.Bass("TRN2", target_bir_lowering=False)
        self.P = Prog(self.nc)
        self.out_ops = []
        self.dram = {}
        self.uid = 0
        self.sb_ptr = SB_BASE
        self.ph_base = None

    def din(self, name, shape, dt=F32):
        if name not in self.dram:
            self.dram[name] = self.nc.dram_tensor(name, list(shape), dt, kind="ExternalInput").ap()
        return self.dram[name]

    def dout(self, name, shape, dt=F32):
        self.dram[name] = self.nc.dram_tensor(name, list(shape), dt, kind="ExternalOutput").ap()
        return self.dram[name]

    def sb(self, name, shape, dt=F32, at=None):
        esz = 2 if dt == BF16 else 4
        nbytes = esz
        for d_ in shape[1:]:
            nbytes *= d_
        nbytes = (nbytes + 31) // 32 * 32
        if at is None:
            off = self.sb_ptr
            self.sb_ptr += nbytes
        else:
            off = self.ph_base + at
        assert off + nbytes <= SB_TOP, (name, off, nbytes)
        self.uid += 1
        return self.nc.alloc_sbuf_tensor_at("%s_%d" % (name, self.uid), list(shape), dt, offset=off)

    def phase(self, reserve=0):
        self.P.fence()
        self.sb_ptr = self.ph_base + reserve

    def mm(self, out, lhsT, rhs, start, stop, rd, wr, **kw):
        self.P.op("pe", lambda e: e.matmul(out, lhsT=lhsT, rhs=rhs, start=start, stop=stop, **kw), rd, wr)

    def tr(self, out, in_, ident, rd, wr):
        self.P.op("pe", lambda e: e.transpose(out, in_, ident), rd, wr)

    def act(self, out, in_, func, rd, wr, **kw):
        self.P.op("act", lambda e: e.activation(out=out, in_=in_, func=func, **kw), rd, wr)

    def tt(self, eng, out, in0, in1, op, rd, wr):
        self.P.op(eng, lambda e: e.tensor_tensor(out=out, in0=in0, in1=in1, op=op), rd, wr)

    def ts(self, eng, out, in0, s1, s2, op0, op1, rd, wr):
        if op1 is None and eng == "pool" and op0 == ALU.mult:
            op1 = ALU.add
            s2 = 0.0
        if op1 is None:
            self.P.op(eng, lambda e: e.tensor_scalar(out=out, in0=in0, scalar1=s1, scalar2=None, op0=op0), rd, wr)
        else:
            self.P.op(eng, lambda e: e.tensor_scalar(out=out, in0=in0, scalar1=s1, scalar2=s2, op0=op0, op1=op1), rd, wr)

    def stt(self, out, in0, scalar, in1, op0, op1, rd, wr):
        self.P.op("dve", lambda e: e.scalar_tensor_tensor(out=out, in0=in0, scalar=scalar, in1=in1, op0=op0, op1=op1), rd, wr)

    def cp(self, eng, out, in_, rd, wr):
        if eng == "act":
            self.act(out, in_, AF.Copy, rd, wr)
        else:
            self.P.op(eng, lambda e: e.tensor_copy(out=out, in_=in_), rd, wr)

    def red(self, out, in_, op, rd, wr):
        self.P.op("dve", lambda e: e.tensor_reduce(out=out, in_=in_, axis=AX.X, op=op), rd, wr)

    def recip(self, out, in_, rd, wr):
        self.P.op("dve", lambda e: e.reciprocal(out=out, in_=in_), rd, wr)

    def memset(self, eng, ap, val, wr):
        self.P.op(eng, lambda e: e.memset(ap, val), (), wr)

    def dma(self, q, out, in_, rd, wr, **kw):
        return self.P.dma(q, out, in_, rd, wr, **kw)

    def dmas(self, q, out, in_, rd, wr):
        return self.P.dma(q, out, in_, rd, wr, allow_slow_non_contiguous=True)

    def dump(self, name, ap_sb, shape, rd, dt=F32):
        d = self.dout(name, shape, F32)
        if len(shape) == 3:
            for c in range(shape[1]):
                self.out_ops.append(self.dma(WQ, d[:, c, :], ap_sb[:, c, :], rd, ()))
        else:
            self.out_ops.append(self.dma(WQ, d, ap_sb, rd, ()))

    def W(self, name):
        shapes = {
            "w_mod": [DEPTH, D, 6 * D], "b_mod": [DEPTH, 6 * D], "w_in": [DEPTH, D, 5632], "lamv": [DEPTH, 4, 64],
            "subln_g": [DEPTH, 128], "w_pa": [DEPTH, D, D], "w_ps": [DEPTH, 512, D], "w_o": [DEPTH, D, D],
            "w_glu": [DEPTH, 512, 512], "b_glu": [DEPTH, 512], "ln1_g": [DEPTH, D], "ln1_b": [DEPTH, D],
            "ln2_g": [DEPTH, D], "ln2_b": [DEPTH, D], "router_g_w": [DEPTH, D, 4], "router_g_b": [DEPTH, 4],
            "router_e_w": [DEPTH, D, 32], "router_e_b": [DEPTH, 32], "moe_w1": [DEPTH, 32, D, 512],
            "moe_w3": [DEPTH, 32, D, 512], "moe_w2": [DEPTH, 32, 512, D],
            "ssm_a_re": [DEPTH, 2, 32, 64], "ssm_a_im": [DEPTH, 2, 32, 64], "ssm_log_dt": [DEPTH, 2, 32],
            "ssm_b_re": [DEPTH, 2, 32, 64, 16], "ssm_b_im": [DEPTH, 2, 32, 64, 16],
            "ssm_c_re": [DEPTH, 2, 32, 16, 64], "ssm_c_im": [DEPTH, 2, 32, 16, 64], "ssm_d": [DEPTH, 512],
            "x": [NLAT, D], "ctx": [NCTX, D], "cvec": [2, D], "ident": [128, 128], "rmat": [128, 128],
            "ropecos": [128, NLAT], "ropesin": [128, NLAT], "swp": [128, 128], "gmask": [128, 8], "sgn": [128, 2],
        }
        return self.din(name, shapes[name])

    @staticmethod
    def hk(c, j):
        return "hT%d_%d" % (c, j)

    @staticmethod
    def xk(c, j):
        return "xT%d_%d" % (c, j)

    @staticmethod
    def bk(c, j):
        return "bg%d_%d" % (c, j)

    @staticmethod
    def tj(ti):
        return 0 if ti < 2 else 1 + (ti - 2) // 4

    def vecload(self, dst, src_row_ap, key, n=None):
        n = dst.shape[1]
        st = self.vstage[self.vcnt % 2]; sk = "vstage%d" % (self.vcnt % 2)
        self.vcnt += 1
        self.dma("sp", st[0:n, :], src_row_ap.rearrange("(c p) -> c p", p=128), (), [sk])
        self.tr(self.ps[7][:, 0:n], st[0:n, :], self.identf[0:n, 0:n], [sk, "identf"], ["ps7"])
        self.cp("dve", dst, self.ps[7][:, 0:n], ["ps7"], [key])

    def bcast_load(self, dst, src_ap, n, key):
        st = self.bstage
        self.dma("sp", st[0:1, 0:n], src_ap, (), ["bstage"])
        self.mm(self.ps[7][:, 0:n], self.ones1[0:1, :], st[0:1, 0:n], True, True, ["bstage", "ones1"], ["ps7"])
        self.cp("dve", dst, self.ps[7][:, 0:n], ["ps7"], [key])

    def build(self):
        B = self
        nc = self.nc
        L = self.n_layers
        x_d = B.W("x"); ctx_d = B.W("ctx"); cvec_d = B.W("cvec")
        out_d = B.dout("out", [NLAT, D])
        self.xT = xT = B.sb("xT", [128, 8, NT]); self.hT = B.sb("hT", [128, 8, NT], BF16)
        self.identf = B.sb("identf", [128, 128]); self.identb = B.sb("identb", [128, 128], BF16)
        self.rmatb = B.sb("rmatb", [128, 128], BF16)
        self.cosb = B.sb("cosb", [128, NLAT], BF16); self.sinb = B.sb("sinb", [128, NLAT], BF16)
        self.onesf = B.sb("onesf", [128, 128]); self.zerob = B.sb("zerob", [128, 512], BF16)
        self.epsc = B.sb("epsc", [128, 1]); self.negpi = B.sb("negpi", [128, 1])
        self.modx = B.sb("modx", [128, 48]); self.cmodx = B.sb("cmodx", [128, 48]); self.bmod = B.sb("bmod", [128, 48])
        self.cv = B.sb("cv", [128, 2, 8]); self.scv = B.sb("scv", [128, 8, 2], BF16)
        self.vstage = [B.sb("vstage%d" % i, [48, 128]) for i in range(2)]; self.vcnt = 0
        self.bstage = B.sb("bstage", [1, 256]); self.ones1 = B.sb("ones1", [1, 128])
        self.swpf = B.sb("swpf", [128, 128]); self.gmaskf = B.sb("gmaskf", [128, 8]); self.sgnf = B.sb("sgnf", [128, 2])
        self.ph_base = (self.sb_ptr + 63) // 64 * 64
        self.ps = ps = [nc.alloc_psum_tensor("ps%d" % i, [128, 512], F32) for i in range(8)]
        PSK = lambda i: "ps%d" % i
        identf = self.identf

        B.dma("sp", identf[:, :], B.W("ident")[:, :], (), ["identf"])
        B.dma(WQ, self.identb[:, :], B.W("ident")[:, :], (), ["identb"])
        B.dma(WQ, self.rmatb[:, :], B.W("rmat")[:, :], (), ["rmatb"])
        B.dma(WQ, self.cosb[:, :], B.W("ropecos")[:, :], (), ["cosb"])
        B.dma(WQ, self.sinb[:, :], B.W("ropesin")[:, :], (), ["sinb"])
        B.dma("sp", self.swpf[:, :], B.W("swp")[:, :], (), ["swpf"])
        B.dma("sp", self.gmaskf[:, :], B.W("gmask")[:, :], (), ["gmaskf"])
        B.dma("sp", self.sgnf[:, :], B.W("sgn")[:, :], (), ["sgnf"])
        B.memset("dve", self.onesf[:, :], 1.0 / D, ["onesf"])
        B.memset("dve", self.zerob[:, :], 0.0, ["zerob"])
        B.memset("dve", self.epsc[:, :], LN_EPS, ["epsc"])
        B.memset("dve", self.negpi[:, :], -math.pi, ["negpi"])
        B.memset("dve", self.ones1[:, :], 1.0, ["ones1"])

        self.phase()
        stg = [B.sb("stg%d" % i, [128, D]) for i in range(2)]
        for ti in range(NT // 128):
            s = stg[ti % 2]; sk = "stg%d" % (ti % 2)
            src = ctx_d[ti * 128:(ti + 1) * 128, :] if ti < 2 else x_d[(ti - 2) * 128:(ti - 1) * 128, :]
            B.dma("sp", s[:, :], src, (), [sk])
            j = self.tj(ti)
            for half in range(2):
                pb = ps[half]
                for cc in range(4):
                    c = half * 4 + cc
                    B.tr(pb[:, cc * 128:(cc + 1) * 128], s[:, c * 128:(c + 1) * 128], identf[:, :], [sk, "identf"], [PSK(half)])
                eng = "dve" if half == 0 else "act"
                B.cp(eng, XAP(xT, half * 4 * NT + ti * 128, [[8 * NT, 128], [NT, 4], [1, 128]]),
                     pb[:, :].rearrange("p (c t) -> p c t", t=128), [PSK(half)], [self.xk(c, j) for c in range(half * 4, half * 4 + 4)])
        for r in range(2):
            self.vecload(self.cv[:, r, :], cvec_d[r, :], "cv%d" % r)
        B.act(self.scv[:, :, :].rearrange("p c r -> p r c"), self.cv[:, :, :], AF.Silu, ["cv0", "cv1"], ["scv"])

        for l in range(L):
            self.layer(l)

        self.phase()
        ostg = [B.sb("ostg%d" % i, [128, D]) for i in range(2)]
        for ti in range(2, NT // 128):
            s = ostg[ti % 2]; sk = "ostg%d" % (ti % 2)
            j = self.tj(ti)
            for half in range(2):
                pb = ps[half]
                for cc in range(4):
                    c = half * 4 + cc
                    B.tr(pb[:, cc * 128:(cc + 1) * 128], xT[:, c, ti * 128:(ti + 1) * 128], identf[:, :], [self.xk(c, j), "identf"], [PSK(half)])
                eng = "dve" if half == 0 else "act"
                B.cp(eng, s[:, half * 512:(half + 1) * 512], pb[:, :], [PSK(half)], [sk + "_%d" % half])
            self.out_ops.append(B.dma("sp", out_d[(ti - 2) * 128:(ti - 1) * 128, :], s[:, :], [sk + "_0", sk + "_1"], ()))
        self.P.emit(final_wait_ops=self.out_ops)
        return nc

    def layer(self, l):
        B = self
        self.last = (l == DEPTH - 1)
        ps = self.ps
        PSK = lambda i: "ps%d" % i
        xT, hT = self.xT, self.hT
        modx, cmodx, bmod = self.modx, self.cmodx, self.bmod
        w_mod = self.W("w_mod"); b_mod = self.W("b_mod")
        self.phase()
        wm = [B.sb("wm%d" % i, [128, 8, 512], BF16) for i in range(2)]
        self.vecload(bmod[:, :], b_mod[l, :], "bmod")
        for blk in range(12):
            wt = wm[blk % 2]; wk_ = "wm%d" % (blk % 2)
            B.dma(WQ, wt[:, :, :], w_mod[l, :, blk * 512:(blk + 1) * 512].rearrange("(c p) n -> p c n", p=128), (), [wk_])
            for jj in range(4):
                j = blk * 4 + jj
                for kc in range(8):
                    B.mm(ps[0][:, 2 * j:2 * j + 2], wt[:, kc, jj * 128:(jj + 1) * 128], self.scv[:, kc, :], kc == 0, kc == 7,
                         [wk_, "scv"], [PSK(0)])
        pv = ps[0][:, 0:96].rearrange("p (j t) -> p j t", t=2)
        B.tt("dve", modx[:, :], pv[:, :, 0], bmod[:, :], ALU.add, [PSK(0), "bmod"], ["modx"])
        B.tt("dve", cmodx[:, :], pv[:, :, 1], bmod[:, :], ALU.add, [PSK(0), "bmod"], ["cmodx"])
        for m_, k_ in ((modx, "modx"), (cmodx, "cmodx")):
            B.ts("dve", m_[:, 8:16], m_[:, 8:16], 1.0, None, ALU.add, None, [k_], [k_])
            B.ts("dve", m_[:, 32:40], m_[:, 32:40], 1.0, None, ALU.add, None, [k_], [k_])
        if "mod" in self.dbg and l == 0:
            self.dump("d_modx", modx[:, :], [128, 48], ["modx"]); self.dump("d_cmodx", cmodx[:, :], [128, 48], ["cmodx"])
        if "mod" in self.stop:
            return
        self.modulate(0)
        if "h" in self.dbg and l == 0:
            self.dump("d_hT", hT[:, :, :], [128, 8, NT], [self.hk(c, j) for c in range(8) for j in range(5)], BF16)
        if "h" in self.stop:
            return
        self.phase()
        self.big = B.sb("attnT", [128, 8, NT], BF16)
        if "attn" not in self.skip:
            self.attn_prep(l)
            import os
            self.att_stage = int(os.environ.get("ATT_STAGE", "9"))
            for h in range(int(os.environ.get("ATT_HEADS", "8"))):
                self.attention(l, h)
            if "attn" in self.dbg and l == 0:
                self.dump("d_attnT", self.big[:, :, :], [128, 8, NT], [self.bk(c, j) for c in range(8) for j in range(5)], BF16)
        if "attn" in self.stop:
            return
        self.phase(reserve=8 * NT * 2)
        self.scale_x()
        if "attn" not in self.skip:
            self.merge(l, "attn")
        if "amerge" in self.stop:
            return
        if "ssm" not in self.skip:
            self.ssm(l)
            self.merge(l, "ssm")
        self.phase()
        self.layernorm(l, 1)
        if "mid" in self.dbg and l == 0:
            self.dump("d_xmid", xT[:, :, :], [128, 8, NT], [self.xk(c, j) for c in range(8) for j in range(5)])
        self.modulate(24)
        if "moe" not in self.skip:
            self.router(l)
        self.scale_x()
        if "moe" not in self.skip:
            self.moe(l)
        self.phase()
        self.layernorm(l, 2)

    def modulate(self, base):
        B = self
        xT, hT = self.xT, self.hT
        for c in range(8):
            for j, (t0, n) in enumerate(TCH):
                if self.last and j == 0 and base != 0:
                    continue
                m_, k_ = (self.cmodx, "cmodx") if j == 0 else (self.modx, "modx")
                B.act(hT[:, c, t0:t0 + n], xT[:, c, t0:t0 + n], AF.Identity, [self.xk(c, j), k_], [self.hk(c, j)],
                      scale=m_[:, base + 8 + c:base + 9 + c], bias=m_[:, base + c:base + c + 1])

    def scale_x(self):
        B = self
        xT = self.xT
        for c in range(8):
            for j, (t0, n) in enumerate(TCH):
                eng = "pool" if (c + j) % 2 else "dve"
                B.ts(eng, xT[:, c, t0:t0 + n], xT[:, c, t0:t0 + n], float(ALPHA), None, ALU.mult, None, [self.xk(c, j)], [self.xk(c, j)])

    def attn_prep(self, l):
        B = self
        at = {}
        at["wa"] = [B.sb("wa%d" % i, [128, 8, 128], BF16) for i in range(2)]
        at["wb"] = [B.sb("wb%d" % i, [128, 8, 128], BF16) for i in range(2)]
        at["wc"] = [B.sb("wc%d" % i, [128, 8, 128], BF16) for i in range(2)]
        at["kT"] = B.sb("kT", [128, NT], BF16); at["qT"] = B.sb("qT", [128, NT], BF16)
        at["Vh"] = B.sb("Vh", [128, 18, 129], BF16)
        at["pt"] = [B.sb("pt%d" % i, [128, 512], BF16) for i in range(3)]
        at["tb"] = [B.sb("tb%d" % i, [128, 512], BF16) for i in range(2)]
        at["t1"] = [B.sb("t1_%d" % i, [128, 512]) for i in range(2)]
        at["t2"] = [B.sb("t2_%d" % i, [128, 512]) for i in range(2)]
        at["lamt"] = B.sb("lamt", [128, 4, 64]); at["lamp"] = B.sb("lamp", [128, 2, 64]); at["lams"] = B.sb("lams", [128, 4])
        at["nlam"] = B.sb("nlam", [128, 1]); at["g128"] = B.sb("g128", [128, 128])
        at["sm"] = [B.sb("sm%d" % i, [128, 8]) for i in range(4)]
        at["of"] = [B.sb("of%d" % i, [128, 128]) for i in range(2)]
        at["ot"] = [B.sb("ot%d" % i, [128, 128]) for i in range(2)]
        at["onb"] = [B.sb("onb%d" % i, [128, 128], BF16) for i in range(2)]
        at["osb"] = [B.sb("osb%d" % i, [128, 258]) for i in range(4)]
        self.at = at
        B.memset("pool", at["Vh"][:, :, :], 1.0, ["Vh"])
        lamv = self.W("lamv"); subg = self.W("subln_g")
        lam_init = 0.8 - 0.6 * math.exp(-0.3 * l)
        self.bcast_load(at["lamt"][:, :, :].rearrange("p a b -> p (a b)"), lamv[l:l + 1, :, :].rearrange("o a b -> o (a b)"), 256, "lamt")
        self.bcast_load(at["g128"][:, :], subg[l:l + 1, :], 128, "g128")
        B.ts("dve", at["g128"][:, :], at["g128"][:, :], float(1.0 - lam_init), None, ALU.mult, None, ["g128"], ["g128"])
        B.tt("dve", at["lamp"][:, 0, :], at["lamt"][:, 0, :], at["lamt"][:, 1, :], ALU.mult, ["lamt"], ["lamp"])
        B.tt("dve", at["lamp"][:, 1, :], at["lamt"][:, 2, :], at["lamt"][:, 3, :], ALU.mult, ["lamt"], ["lamp"])
        B.red(at["lams"][:, 0:2], at["lamp"][:, :, :], ALU.add, ["lamp"], ["lams"])
        B.act(at["lams"][:, 2:4], at["lams"][:, 0:2], AF.Exp, ["lams"], ["lams2"])
        B.tt("dve", at["nlam"][:, :], at["lams"][:, 3:4], at["lams"][:, 2:3], ALU.subtract, ["lams2"], ["nlam"])
        B.ts("dve", at["nlam"][:, :], at["nlam"][:, :], float(-lam_init), None, ALU.add, None, ["nlam"], ["nlam"])

    def proj_fm(self, wt, wkey, nkc, src, srck, dst_fn, dkey_fn, rope=False, tiles=None, evac="act"):
        B = self
        ps = self.ps
        for j, (t0, n) in enumerate(TCH):
            pb = ps[j % 2]; pk = "ps%d" % (j % 2)
            for kc in range(nkc):
                B.mm(pb[:, :n], wt[:, kc, :], src[:, kc, t0:t0 + n], kc == 0, kc == nkc - 1, [wkey, srck(kc, j)], [pk])
            import os
            rm = int(os.environ.get("ROPE_MODE", "2"))
            if j == 0 or not rope or rm == 0:
                B.cp(evac if j % 2 == 0 else "dve", dst_fn(t0, n), pb[:, :n], [pk], [dkey_fn(j)])
            else:
                tb = tiles["tb"][j % 2]; tbk = "tb%d" % (j % 2)
                t1 = tiles["t1"][j % 2]; t1k = "t1_%d" % (j % 2)
                t2 = tiles["t2"][j % 2]; t2k = "t2_%d" % (j % 2)
                pr = ps[2 + j % 2]; prk = "ps%d" % (2 + j % 2)
                B.cp("act", tb[:, :n], pb[:, :n], [pk], [tbk])
                B.mm(pr[:, :n], self.rmatb[:, :], tb[:, :n], True, True, ["rmatb", tbk], [prk])
                lo = t0 - NCTX
                if rm in (3, 4, 5):
                    if rm >= 4:
                        B.tt("dve", t1[:, :n], pb[:, :n], self.cosb[:, lo:lo + n], ALU.mult, [pk, "cosb", tbk], [t1k])
                    if rm >= 5:
                        B.tt("dve", t2[:, :n], pr[:, :n], self.sinb[:, lo:lo + n], ALU.mult, [prk, "sinb"], [t2k])
                    B.cp("dve", dst_fn(t0, n), pb[:, :n], [pk, prk], [dkey_fn(j)])
                    continue
                B.tt("dve", t1[:, :n], pb[:, :n], self.cosb[:, lo:lo + n], ALU.mult, [pk, "cosb", tbk], [t1k])
                B.tt("dve", t2[:, :n], pr[:, :n], self.sinb[:, lo:lo + n], ALU.mult, [prk, "sinb"], [t2k])
                B.tt("pool" if rm == 2 else "dve", dst_fn(t0, n), t1[:, :n], t2[:, :n], ALU.add, [t1k, t2k], [dkey_fn(j)])

    def attention(self, l, h):
        B = self
        at = self.at; ps = self.ps; hT = self.hT; big = self.big
        w_in = self.W("w_in")
        i2 = h % 2
        wk, wq, wv = at["wa"][i2], at["wb"][i2], at["wc"][i2]
        wkk, wqk, wvk = "wa%d" % i2, "wb%d" % i2, "wc%d" % i2
        for wt, key, off in ((wk, wkk, 0), (wq, wqk, 2560), (wv, wvk, 1024)):
            B.dma(WQ, wt[:, :, :], w_in[l, :, off + h * 128:off + (h + 1) * 128].rearrange("(c p) n -> p c n", p=128), (), [key])
        kT, qT, Vh = at["kT"], at["qT"], at["Vh"]
        self.proj_fm(wk, wkk, 8, hT, self.hk, lambda t0, n: kT[:, t0:t0 + n], lambda j: "kT_%d" % j, True, at)
        self.proj_fm(wq, wqk, 8, hT, self.hk, lambda t0, n: qT[:, t0:t0 + n], lambda j: "qT_%d" % j, True, at)
        if self.att_stage < 1:
            return
        if "qk" in self.dbg and l == 0 and h == 0:
            self.dump("d_kT", kT[:, :], [128, NT], ["kT_%d" % j for j in range(5)], BF16)
            self.dump("d_qT", qT[:, :], [128, NT], ["qT_%d" % j for j in range(5)], BF16)
        for t4 in range(5):
            tiles = list(range(t4 * 4, min(18, t4 * 4 + 4)))
            pb = ps[t4 % 2]; pk = "ps%d" % (t4 % 2)
            for ii, ti in enumerate(tiles):
                j = self.tj(ti)
                for kc in range(8):
                    B.mm(pb[:, ii * 128:(ii + 1) * 128], hT[:, kc, ti * 128:(ti + 1) * 128], wv[:, kc, :], kc == 0, kc == 7,
                         [wvk, self.hk(kc, j)], [pk])
            nt_ = len(tiles)
            B.cp("act", XAP(Vh, tiles[0] * 129, [[18 * 129, 128], [129, nt_], [1, 128]]),
                 pb[:, 0:nt_ * 128].rearrange("p (a b) -> p a b", b=128), [pk], ["Vh"])
        if self.att_stage < 2:
            return
        cnt = 0
        for qi, (q0, qn, nk) in enumerate([(0, 256, 2)] + [(256 + 512 * i, 512, 18) for i in range(4)]):
            if self.last and qi == 0:
                continue
            nsub = qn // 128
            nb = (nsub + 1) // 2
            for m in range(2):
                for bb in range(nb):
                    bk_ = 4 + 2 * m + bb
                    B.mm(ps[bk_][:, :], self.zerob[:, 0:128], self.zerob[:, :], True, True, ["zerob"], ["ps%d" % bk_])
            for m in range(2):
                prev = None
                for kt in range(nk + 1):
                    cur = None
                    if kt < nk:
                        sbk = 2 + cnt % 2
                        pt = at["pt"][cnt % 3]; ptk = "pt%d" % (cnt % 3)
                        cnt += 1
                        jk = self.tj(kt)
                        B.mm(ps[sbk][:, :qn], kT[64 * m:64 * m + 64, kt * 128:(kt + 1) * 128], qT[64 * m:64 * m + 64, q0:q0 + qn], True, True,
                             ["kT_%d" % jk, "qT_%d" % qi], ["ps%d" % sbk])
                        B.act(pt[:, :qn], ps[sbk][:, :qn], AF.Exp, ["ps%d" % sbk], [ptk], scale=0.125)
                        cur = (pt, ptk, kt)
                    if prev is not None:
                        ppt, pptk, pkt = prev
                        for sub in range(nsub):
                            bk_ = 4 + 2 * m + sub // 2
                            col = (sub % 2) * 129
                            B.mm(ps[bk_][:, col:col + 129], ppt[:, sub * 128:(sub + 1) * 128], Vh[:, pkt, :], False, pkt == nk - 1,
                                 [pptk, "Vh"], ["ps%d" % bk_], skip_group_check=True)
                    prev = cur
            osb = at["osb"]
            for m in range(2):
                for bb in range(nb):
                    bk_ = 4 + 2 * m + bb
                    B.cp("act" if (m + bb) % 2 else "dve", osb[bk_ - 4][:, :], ps[bk_][:, 0:258], ["ps%d" % bk_], ["osb%d" % (bk_ - 4)])
            if self.att_stage < 3:
                continue
            for sub in range(nsub):
                b0 = sub // 2; b1 = 2 + sub // 2
                col = (sub % 2) * 129
                sm = at["sm"][sub % 4]; smk = "sm%d" % (sub % 4)
                of = at["of"][sub % 2]; ofk = "of%d" % (sub % 2)
                ot = at["ot"][sub % 2]; otk = "ot%d" % (sub % 2)
                onb = at["onb"][sub % 2]; onk = "onb%d" % (sub % 2)
                o0, o1 = at["osb"][b0], at["osb"][b1]
                B.recip(sm[:, 0:1], o0[:, col + 128:col + 129], ["osb%d" % b0], [smk])
                B.recip(sm[:, 1:2], o1[:, col + 128:col + 129], ["osb%d" % b1], [smk])
                B.tt("dve", sm[:, 2:3], sm[:, 1:2], at["nlam"][:, :], ALU.mult, [smk, "nlam"], [smk])
                B.ts("dve", ot[:, :], o1[:, col:col + 128], sm[:, 2:3], None, ALU.mult, None, ["osb%d" % b1, smk], [otk])
                B.stt(of[:, :], o0[:, col:col + 128], sm[:, 0:1], ot[:, :], ALU.mult, ALU.add, ["osb%d" % b0, smk, otk], [ofk])
                B.tt("dve", ot[:, :], of[:, :], of[:, :], ALU.mult, [ofk], [otk])
                B.red(sm[:, 3:4], ot[:, :], ALU.add, [otk], [smk])
                B.act(sm[:, 4:5], sm[:, 3:4], AF.Ln, [smk], [smk], scale=1.0 / 128.0, bias=self.epsc[:, :])
                B.act(sm[:, 5:6], sm[:, 4:5], AF.Exp, [smk], [smk], scale=-0.5)
                B.stt(onb[:, :], of[:, :], sm[:, 5:6], at["g128"][:, :], ALU.mult, ALU.mult, [ofk, smk, "g128"], [onk])
                pbf = ps[sub % 2][:, 0:64].bitcast(BF16)
                B.tr(pbf, onb[:, :], self.identb[:, :], [onk, "identb"], ["ps%d" % (sub % 2)])
                tq = q0 + sub * 128
                jq = 0 if tq < 256 else 1 + (tq - 256) // 512
                B.cp("act", big[:, h, tq:tq + 128], pbf, ["ps%d" % (sub % 2)], [self.bk(h, jq)])

    def merge(self, l, kind):
        B = self
        ps = self.ps; hT = self.hT; xT = self.xT
        w_in = self.W("w_in"); w_o = self.W("w_o")
        if kind == "attn":
            goff = 3584; wp = self.W("w_pa"); nkc = 8; src = self.big; srck = self.bk
        else:
            goff = 4608; wp = self.W("w_ps"); nkc = 4; src = self.ssmT; srck = lambda c, j: "ss%d_%d" % (c, j)
        wa = [B.sb("mwa%d" % i, [128, 8, 128], BF16) for i in range(2)]
        wb = [B.sb("mwb%d" % i, [128, 8, 128], BF16) for i in range(2)]
        wo = [B.sb("mwo%d" % i, [128, 1024], BF16) for i in range(2)]
        mC = [B.sb("mC%d" % i, [128, NT], BF16) for i in range(2)]
        sg = [B.sb("msg%d" % i, [128, 512]) for i in range(2)]
        ycnt = 0
        for c in range(8):
            i2 = c % 2
            B.dma(WQ, wa[i2][:, :, :], w_in[l, :, goff + c * 128:goff + (c + 1) * 128].rearrange("(c p) n -> p c n", p=128), (), ["mwa%d" % i2])
            B.dma(WQ, wb[i2][:, 0:nkc, :], wp[l, :, c * 128:(c + 1) * 128].rearrange("(c p) n -> p c n", p=128), (), ["mwb%d" % i2])
            B.dma(WQ, wo[i2][:, :], w_o[l, c * 128:(c + 1) * 128, :], (), ["mwo%d" % i2])
            for j, (t0, n) in enumerate(TCH):
                if self.last and j == 0:
                    continue
                pg = ps[j % 2]; pgk = "ps%d" % (j % 2)
                pp = ps[2 + j % 2]; ppk = "ps%d" % (2 + j % 2)
                for kc in range(8):
                    B.mm(pg[:, :n], wa[i2][:, kc, :], hT[:, kc, t0:t0 + n], kc == 0, kc == 7, ["mwa%d" % i2, self.hk(kc, j)], [pgk])
                for kc in range(nkc):
                    B.mm(pp[:, :n], wb[i2][:, kc, :], src[:, kc, t0:t0 + n], kc == 0, kc == nkc - 1, ["mwb%d" % i2, srck(kc, j)], [ppk])
                B.act(sg[j % 2][:, :n], pg[:, :n], AF.Sigmoid, [pgk], ["msg%d" % (j % 2)])
                B.tt("dve", mC[i2][:, t0:t0 + n], sg[j % 2][:, :n], pp[:, :n], ALU.mult, ["msg%d" % (j % 2), ppk], ["mC%d_%d" % (i2, j)])
            for o in range(8):
                for j, (t0, n) in enumerate(TCH):
                    if self.last and j == 0:
                        continue
                    py = ps[4 + ycnt % 4]; pyk = "ps%d" % (4 + ycnt % 4)
                    ycnt += 1
                    B.mm(py[:, :n], wo[i2][:, o * 128:(o + 1) * 128], mC[i2][:, t0:t0 + n], True, True, ["mwo%d" % i2, "mC%d_%d" % (i2, j)], [pyk])
                    m_, k_ = (self.cmodx, "cmodx") if j == 0 else (self.modx, "modx")
                    B.stt(xT[:, o, t0:t0 + n], py[:, :n], m_[:, 16 + o:17 + o], xT[:, o, t0:t0 + n], ALU.mult, ALU.add,
                          [pyk, k_, self.xk(o, j)], [self.xk(o, j)])

    def layernorm(self, l, which):
        B = self
        ps = self.ps; xT = self.xT
        g_d = self.W("ln%d_g" % which); b_d = self.W("ln%d_b" % which)
        lng = B.sb("lng", [128, 8]); lnb = B.sb("lnb", [128, 8])
        sq = [B.sb("lnsq%d" % i, [128, 512]) for i in range(2)]
        ta = [B.sb("lnta%d" % i, [128, 512]) for i in range(2)]
        tv = [B.sb("lntv%d" % i, [128, 512]) for i in range(2)]
        self.vecload(lng[:, :], g_d[l, :], "lng"); self.vecload(lnb[:, :], b_d[l, :], "lnb")
        for j, (t0, n) in enumerate(TCH):
            if self.last and j == 0:
                continue
            a = (j % 2) * 2
            p0 = ps[a]; p0k = "ps%d" % a; p1 = ps[a + 1]; p1k = "ps%d" % (a + 1)
            i2 = j % 2
            for c in range(8):
                B.mm(p0[:, :n], self.onesf[:, :], xT[:, c, t0:t0 + n], c == 0, c == 7, ["onesf", self.xk(c, j)], [p0k])
            for c in range(8):
                s_ = sq[c % 2]; sk_ = "lnsq%d" % (c % 2)
                B.act(s_[:, :n], xT[:, c, t0:t0 + n], AF.Square, [self.xk(c, j)], [sk_])
                B.mm(p1[:, :n], self.onesf[:, :], s_[:, :n], c == 0, c == 7, ["onesf", sk_], [p1k])
            tak = "lnta%d" % i2; tvk = "lntv%d" % i2
            B.act(ta[i2][:, :n], p0[:, :n], AF.Square, [p0k], [tak])
            B.tt("dve", tv[i2][:, :n], p1[:, :n], ta[i2][:, :n], ALU.subtract, [p1k, tak], [tvk])
            B.act(tv[i2][:, :n], tv[i2][:, :n], AF.Ln, [tvk], [tvk], bias=self.epsc[:, :])
            B.act(tv[i2][:, :n], tv[i2][:, :n], AF.Exp, [tvk], [tvk], scale=-0.5)
            B.stt(ta[i2][:, :n], p0[:, :n], -1.0, tv[i2][:, :n], ALU.mult, ALU.mult, [p0k, tvk], [tak])
            for c in range(8):
                xs = xT[:, c, t0:t0 + n]
                B.tt("dve", xs, xs, tv[i2][:, :n], ALU.mult, [self.xk(c, j), tvk], [self.xk(c, j)])
                B.tt("pool", xs, xs, ta[i2][:, :n], ALU.add, [self.xk(c, j), tak], [self.xk(c, j)])
                B.act(xs, xs, AF.Identity, [self.xk(c, j), "lng", "lnb"], [self.xk(c, j)], scale=lng[:, c:c + 1], bias=lnb[:, c:c + 1])

    def router(self, l):
        B = self
        ps = self.ps; xT = self.xT
        self.phase()
        self.WT = WT = B.sb("WT", [32, NT])
        wr = B.sb("wr", [128, 8, 36]); rb = B.sb("rb", [128, 36])
        h2 = [B.sb("h2_%d" % i, [128, 8, 128]) for i in range(2)]
        lg = [B.sb("lg%d" % i, [128, 36]) for i in range(2)]
        me = [B.sb("me%d" % i, [128, 32]) for i in range(2)]
        rs = [B.sb("rs%d" % i, [128, 48]) for i in range(2)]
        wt_ = [B.sb("rwt%d" % i, [128, 32]) for i in range(2)]
        mk = [B.sb("rmk%d" % i, [128, 32]) for i in range(2)]
        rgw = self.W("router_g_w"); rgb = self.W("router_g_b"); rew = self.W("router_e_w"); reb = self.W("router_e_b")
        B.dmas("sp", wr[:, :, 0:4], rgw[l, :, :].rearrange("(c p) n -> p c n", p=128), (), ["wr"])
        B.dmas("sp", wr[:, :, 4:36], rew[l, :, :].rearrange("(c p) n -> p c n", p=128), (), ["wr"])
        self.bcast_load(rb[:, 0:4], rgb[l:l + 1, :], 4, "rb")
        self.bcast_load(rb[:, 4:36], reb[l:l + 1, :], 32, "rb")
        for ti in range(18):
            if self.last and ti < 2:
                continue
            i2 = ti % 2
            j = self.tj(ti)
            m_, k_ = (self.cmodx, "cmodx") if ti < 2 else (self.modx, "modx")
            hk_ = "h2_%d" % i2; lk = "lg%d" % i2; mek = "me%d" % i2; rk = "rs%d" % i2; wk_ = "rwt%d" % i2; mkk = "rmk%d" % i2
            for c in range(8):
                B.act(h2[i2][:, c, :], xT[:, c, ti * 128:(ti + 1) * 128], AF.Identity, [self.xk(c, j), k_], [hk_],
                      scale=m_[:, 32 + c:33 + c], bias=m_[:, 24 + c:25 + c])
            for c in range(8):
                B.mm(ps[i2][:, 0:36], h2[i2][:, c, :], wr[:, c, :], c == 0, c == 7, [hk_, "wr"], ["ps%d" % i2])
            L_ = lg[i2]; R_ = rs[i2]; M_ = me[i2]
            B.tt("dve", L_[:, :], ps[i2][:, 0:36], rb[:, :], ALU.add, ["ps%d" % i2, "rb"], [lk])
            B.red(R_[:, 0:1], L_[:, 0:4], ALU.max, [lk], [rk])
            B.ts("dve", R_[:, 1:2], R_[:, 0:1], -1.0, None, ALU.mult, None, [rk], [rk])
            B.ts("dve", R_[:, 8:12], L_[:, 0:4], R_[:, 0:1], None, ALU.is_equal, None, [lk, rk], [rk])
            B.act(R_[:, 12:16], L_[:, 0:4], AF.Exp, [lk, rk], [rk], bias=R_[:, 1:2])
            B.red(R_[:, 2:3], R_[:, 12:16], ALU.add, [rk], [rk])
            B.recip(R_[:, 3:4], R_[:, 2:3], [rk], [rk])
            B.ts("dve", R_[:, 16:20], R_[:, 8:12], -1.0, 1.0e30, ALU.add, ALU.mult, [rk], [rk])
            B.tt("dve", M_[:, :].rearrange("p (a b) -> p a b", b=8), L_[:, 4:36].rearrange("p (a b) -> p a b", b=8),
                 XAP(R_, 16, [[48, 128], [1, 4], [0, 8]]), ALU.add, [lk, rk], [mek])
            B.P.op("dve", lambda e, R_=R_, M_=M_: e.max(out=R_[:, 24:32], in_=M_[:, :]), [mek], [rk])
            B.ts("dve", mk[i2][:, :], M_[:, :], R_[:, 24:25], None, ALU.is_equal, None, [mek, rk], [mkk])
            B.tt("dve", R_[:, 4:5], R_[:, 25:26], R_[:, 24:25], ALU.subtract, [rk], [rk])
            B.act(R_[:, 5:6], R_[:, 4:5], AF.Exp, [rk], [rk])
            B.ts("dve", R_[:, 6:7], R_[:, 5:6], 1.0, None, ALU.add, None, [rk], [rk])
            B.recip(R_[:, 6:7], R_[:, 6:7], [rk], [rk])
            B.tt("dve", R_[:, 7:8], R_[:, 5:6], R_[:, 6:7], ALU.mult, [rk], [rk])
            B.tt("dve", R_[:, 32:33], R_[:, 6:7], R_[:, 3:4], ALU.mult, [rk], [rk])
            B.tt("dve", R_[:, 33:34], R_[:, 7:8], R_[:, 3:4], ALU.mult, [rk], [rk])
            B.ts("dve", wt_[i2][:, :], mk[i2][:, :], R_[:, 32:33], None, ALU.mult, None, [mkk, rk], [wk_])
            B.ts("dve", mk[i2][:, :], M_[:, :], R_[:, 25:26], None, ALU.is_equal, None, [mek, rk, wk_], [mkk])
            B.stt(wt_[i2][:, :], mk[i2][:, :], R_[:, 33:34], wt_[i2][:, :], ALU.mult, ALU.add, [mkk, rk, wk_], [wk_])
            B.tr(ps[2 + i2][0:32, 0:128], wt_[i2][:, :], self.identf[:, :], [wk_, "identf"], ["ps%d" % (2 + i2)])
            B.cp("act", WT[:, ti * 128:(ti + 1) * 128], ps[2 + i2][0:32, 0:128], ["ps%d" % (2 + i2)], ["WT_%d" % j])
        if "rt" in self.dbg and l == 0:
            self.dump("d_WT", WT[:, :], [32, NT], ["WT_%d" % j for j in range(5)])

    def moe(self, l):
        B = self
        ps = self.ps; xT = self.xT; hT = self.hT; WT = self.WT
        mw1 = self.W("moe_w1"); mw3 = self.W("moe_w3"); mw2 = self.W("moe_w2")
        self.sb_ptr = self.ph_base + 32 * 0 + NT * 4
        w1 = [B.sb("w1_%d" % i, [128, 8, 512], BF16) for i in range(2)]
        w3 = [B.sb("w3_%d" % i, [128, 8, 512], BF16) for i in range(2)]
        w2 = [B.sb("w2_%d" % i, [128, 4, 1024], BF16) for i in range(2)]
        gb = [B.sb("gb%d" % i, [128, 4, 512], BF16) for i in range(2)]
        s1 = [B.sb("s1_%d" % i, [128, 512]) for i in range(2)]
        s2 = [B.sb("s2_%d" % i, [128, 512]) for i in range(2)]
        self.P.fence()
        cnt = 0
        for e in range(32):
            i2 = e % 2
            B.dma(WQ, w1[i2][:, :, :], mw1[l, e, :, :].rearrange("(c p) n -> p c n", p=128), (), ["w1_%d" % i2])
            B.dma(WQ, w3[i2][:, :, :], mw3[l, e, :, :].rearrange("(c p) n -> p c n", p=128), (), ["w3_%d" % i2])
            B.dma(WQ, w2[i2][:, :, :], mw2[l, e, :, :].rearrange("(c p) n -> p c n", p=128), (), ["w2_%d" % i2])
            selT = XAP(self.identf, e, [[128, 32], [0, 128]])
            for j, (t0, n) in enumerate(TCH):
                if self.last and j == 0:
                    continue
                pw = ps[j % 2]; pwk = "ps%d" % (j % 2)
                B.mm(pw[:, :n], selT, WT[:, t0:t0 + n], True, True, ["identf", "WT_%d" % j], [pwk])
                g_ = gb[j % 2]
                for hc in range(4):
                    p1 = ps[2 + hc % 2]; p1k = "ps%d" % (2 + hc % 2)
                    p3 = ps[4 + hc % 2]; p3k = "ps%d" % (4 + hc % 2)
                    for kc in range(8):
                        B.mm(p1[:, :n], w1[i2][:, kc, hc * 128:(hc + 1) * 128], hT[:, kc, t0:t0 + n], kc == 0, kc == 7,
                             ["w1_%d" % i2, self.hk(kc, j)], [p1k])
                    for kc in range(8):
                        B.mm(p3[:, :n], w3[i2][:, kc, hc * 128:(hc + 1) * 128], hT[:, kc, t0:t0 + n], kc == 0, kc == 7,
                             ["w3_%d" % i2, self.hk(kc, j)], [p3k])
                    B.act(s1[hc % 2][:, :n], p1[:, :n], AF.Silu, [p1k], ["s1_%d" % (hc % 2)])
                    B.tt("dve", s2[hc % 2][:, :n], s1[hc % 2][:, :n], p3[:, :n], ALU.mult, ["s1_%d" % (hc % 2), p3k], ["s2_%d" % (hc % 2)])
                    B.tt("dve", g_[:, hc, :n], s2[hc % 2][:, :n], pw[:, :n], ALU.mult, ["s2_%d" % (hc % 2), pwk], ["gb%d_%d" % (j % 2, hc)])
                for o in range(8):
                    py = ps[6 + cnt % 2]; pyk = "ps%d" % (6 + cnt % 2)
                    cnt += 1
                    for hc in range(4):
                        B.mm(py[:, :n], w2[i2][:, hc, o * 128:(o + 1) * 128], g_[:, hc, :n], hc == 0, hc == 3,
                             ["w2_%d" % i2, "gb%d_%d" % (j % 2, hc)], [pyk])
                    m_, k_ = (self.cmodx, "cmodx") if j == 0 else (self.modx, "modx")
                    B.stt(xT[:, o, t0:t0 + n], py[:, :n], m_[:, 40 + o:41 + o], xT[:, o, t0:t0 + n], ALU.mult, ALU.add,
                          [pyk, k_, self.xk(o, j)], [self.xk(o, j)])

    def ssm(self, l):
        B = self
        ps = self.ps; hT = self.hT
        NLV = 12
        self.phase()
        self.ssmT = ssmT = B.sb("ssmT", [128, 4, NT], BF16)
        uT = B.sb("uT", [128, 4, NT], BF16)
        ARn = [B.sb("ARn%d" % d, [128, NLV, 32]) for d in range(2)]
        OFn = [B.sb("OFn%d" % d, [128, NLV, 32]) for d in range(2)]
        BT = [B.sb("BT%d" % d, [128, 4, 128], BF16) for d in range(2)]
        Cst = [B.sb("Cst%d" % d, [128, 512], BF16) for d in range(2)]
        dsk = B.sb("dsk", [128, 4]); bglu = B.sb("bglu", [128, 4])
        main_base = self.sb_ptr
        uk = lambda c, j: "uT%d_%d" % (c, j)
        w_in = self.W("w_in")
        self.vecload(dsk[:, :], self.W("ssm_d")[l, :], "dsk")
        self.vecload(bglu[:, :], self.W("b_glu")[l, :], "bglu")
        wu = [B.sb("wu%d" % i, [128, 8, 128], BF16) for i in range(2)]
        for oc in range(4):
            i2 = oc % 2
            B.dma(WQ, wu[i2][:, :, :], w_in[l, :, 2048 + oc * 128:2048 + (oc + 1) * 128].rearrange("(c p) n -> p c n", p=128), (), ["wu%d" % i2])
            self.proj_fm(wu[i2], "wu%d" % i2, 8, hT, self.hk, lambda t0, n, oc=oc: uT[:, oc, t0:t0 + n], lambda j, oc=oc: uk(oc, j))
        if "u" in self.dbg and l == 0:
            self.dump("d_uT", uT[:, :, :], [128, 4, NT], [uk(c, j) for c in range(4) for j in range(5)], BF16)
        a_re = self.W("ssm_a_re"); a_im = self.W("ssm_a_im"); ldt = self.W("ssm_log_dt")
        b_re = self.W("ssm_b_re"); b_im = self.W("ssm_b_im"); c_re = self.W("ssm_c_re"); c_im = self.W("ssm_c_im")
        anat = B.sb("anat", [32, 128])
        are2 = B.sb("are2", [128, 32]); aim2 = B.sb("aim2", [128, 32]); dt2 = B.sb("dt2", [128, 32])
        zre = B.sb("zre", [128, 32]); zim = B.sb("zim", [128, 32])
        NV = NLV * 32
        zn = B.sb("zn", [128, NLV, 32]); yy = B.sb("yy", [128, NLV, 32]); yi = B.sb("yi", [128, NLV, 32], I32)
        yf = B.sb("yf", [128, NLV, 32]); ff = B.sb("ff", [128, NLV, 32]); sv = B.sb("sv", [128, NLV, 32]); cvv = B.sb("cvv", [128, NLV, 32])
        AIn = B.sb("AIn", [128, NLV, 32])
        cf = B.sb("cf", [128, 12, 32])
        X1 = B.sb("X1", [128, 32, 16]); X2 = B.sb("X2", [128, 32, 16]); Bst = B.sb("Bst", [128, 512]); Btmp = B.sb("Btmp", [128, 512])
        Cnat = B.sb("Cnat", [128, 4, 128])
        fl = lambda t: t[:, :, :].rearrange("p a b -> p (a b)")
        for d in range(2):
            kd = "_%d" % d
            for src_, dst_, kk in ((a_re, are2, "are2"), (a_im, aim2, "aim2")):
                for half in range(2):
                    B.dma("sp", anat[:, half * 64:(half + 1) * 64], src_[l, d, :, :], (), ["anat"])
                B.tr(ps[4][:, 0:32], anat[:, :], self.identf[0:32, 0:32], ["anat", "identf"], ["ps4"])
                B.cp("dve", dst_[:, :], ps[4][:, 0:32], ["ps4"], [kk])
            self.bcast_load(dt2[:, :], ldt[l, d:d + 1, :], 32, "dt2")
            B.act(dt2[:, :], dt2[:, :], AF.Exp, ["dt2"], ["dt2"])
            B.tt("dve", zre[:, :], are2[:, :], dt2[:, :], ALU.mult, ["are2", "dt2"], ["zre"])
            B.tt("dve", zim[:, :], aim2[:, :], dt2[:, :], ALU.mult, ["aim2", "dt2"], ["zim"])
            for i in range(NLV):
                n_ = float(2 ** i)
                B.ts("dve", zn[:, i, :], zre[:, :], n_, None, ALU.mult, None, ["zre"], ["zn"])
                B.ts("dve", yy[:, i, :], zim[:, :], n_ / (2.0 * math.pi), 8.5, ALU.mult, ALU.add, ["zim"], ["yy"])
            B.act(fl(zn), fl(zn), AF.Exp, ["zn"], ["zn"])
            for which, dst in ((0, sv), (1, cvv)):
                if which == 1:
                    B.ts("dve", fl(yy), fl(yy), 0.25, None, ALU.add, None, ["yy"], ["yy"])
                B.cp("dve", fl(yi), fl(yy), ["yy"], ["yi"])
                B.cp("dve", fl(yf), fl(yi), ["yi"], ["yf"])
                B.tt("dve", fl(ff), fl(yy), fl(yf), ALU.subtract, ["yy", "yf"], ["ff"])
                B.stt(fl(ff), fl(ff), 0.0, fl(ff), ALU.is_lt, ALU.add, ["ff"], ["ff"])
                B.ts("dve", fl(ff), fl(ff), 1.0, None, ALU.min, None, ["ff"], ["ff"])
                B.act(fl(dst), fl(ff), AF.Sin, ["ff", "negpi"], ["sc%d" % which], scale=2.0 * math.pi, bias=self.negpi[:, :])
            B.tt("dve", fl(ARn[d]), fl(zn), fl(cvv), ALU.mult, ["zn", "sc1"], ["ARn" + kd])
            B.tt("dve", fl(AIn), fl(zn), fl(sv), ALU.mult, ["zn", "sc0"], ["AIn"])
            B.ts("dve", fl(OFn[d]), fl(AIn), self.sgnf[:, 0:1], None, ALU.mult, None, ["AIn", "sgnf"], ["OFn" + kd])
            a1r = ARn[d][:, 0, :]; a1i = AIn[:, 0, :]
            c_ = lambda i: cf[:, i, :]
            B.ts("dve", c_(0), a1r, -1.0, None, ALU.add, None, ["ARn" + kd], ["cf"])
            B.tt("dve", c_(1), c_(0), are2[:, :], ALU.mult, ["cf", "are2"], ["cf"])
            B.tt("dve", c_(2), a1i, aim2[:, :], ALU.mult, ["AIn", "aim2"], ["cf"])
            B.tt("dve", c_(1), c_(1), c_(2), ALU.add, ["cf"], ["cf"])
            B.tt("dve", c_(3), a1i, are2[:, :], ALU.mult, ["AIn", "are2"], ["cf"])
            B.tt("dve", c_(4), c_(0), aim2[:, :], ALU.mult, ["cf", "aim2"], ["cf"])
            B.tt("dve", c_(3), c_(3), c_(4), ALU.subtract, ["cf"], ["cf"])
            B.tt("dve", c_(5), are2[:, :], are2[:, :], ALU.mult, ["are2"], ["cf"])
            B.tt("dve", c_(6), aim2[:, :], aim2[:, :], ALU.mult, ["aim2"], ["cf"])
            B.tt("dve", c_(5), c_(5), c_(6), ALU.add, ["cf"], ["cf"])
            B.recip(c_(5), c_(5), ["cf"], ["cf"])
            B.tt("dve", c_(7), c_(1), c_(5), ALU.mult, ["cf"], ["cf"])
            B.tt("dve", c_(8), c_(3), c_(5), ALU.mult, ["cf"], ["cf"])
            B.ts("dve", c_(8), c_(8), self.sgnf[:, 1:2], None, ALU.mult, None, ["cf", "sgnf"], ["cf"])
            for q4 in range(4):
                gs = slice(q4 * 8, (q4 + 1) * 8)
                B.dmas("sp", X1[0:64, gs, :], b_re[l, d, gs, :, :].rearrange("g p c -> p g c"), (), ["X1"])
                B.dmas("sp", X1[64:128, gs, :], b_im[l, d, gs, :, :].rearrange("g p c -> p g c"), (), ["X1"])
                B.dmas("sp", X2[0:64, gs, :], b_im[l, d, gs, :, :].rearrange("g p c -> p g c"), (), ["X2"])
                B.dmas("sp", X2[64:128, gs, :], b_re[l, d, gs, :, :].rearrange("g p c -> p g c"), (), ["X2"])
            crb = XAP(cf, 7 * 32, [[12 * 32, 128], [1, 32], [0, 16]])
            cib = XAP(cf, 8 * 32, [[12 * 32, 128], [1, 32], [0, 16]])
            v3 = lambda t: t[:, :].rearrange("p (g c) -> p g c", c=16)
            B.tt("dve", v3(Bst), X1[:, :, :], crb, ALU.mult, ["X1", "cf"], ["Bst"])
            B.tt("dve", v3(Btmp), X2[:, :, :], cib, ALU.mult, ["X2", "cf"], ["Btmp"])
            B.tt("dve", Bst[:, :], Bst[:, :], Btmp[:, :], ALU.add, ["Bst", "Btmp"], ["Bst"])
            for k in range(4):
                B.tr(ps[k % 2][:, 0:128], Bst[:, k * 128:(k + 1) * 128], self.identf[:, :], ["Bst", "identf"], ["ps%d" % (k % 2)])
                B.cp("act", BT[d][:, k, :], ps[k % 2][:, 0:128], ["ps%d" % (k % 2)], ["BT" + kd])
            B.dma("sp", Cnat[:, :, 0:64], c_re[l, d, :, :, :].rearrange("(k a) c p -> (a c) k p", k=4), (), ["Cnat"])
            B.dma("sp", Cnat[:, :, 64:128], c_im[l, d, :, :, :].rearrange("(k a) c p -> (a c) k p", k=4), (), ["Cnat"])
            for k in range(4):
                B.tr(ps[2 + k % 2][:, 0:128], Cnat[:, k, :], self.identf[:, :], ["Cnat", "identf"], ["ps%d" % (2 + k % 2)])
                B.cp("dve", Cst[d][0:64, k * 128:(k + 1) * 128], ps[2 + k % 2][0:64, 0:128], ["ps%d" % (2 + k % 2)], ["Cst" + kd])
                B.act(Cst[d][64:128, k * 128:(k + 1) * 128], ps[2 + k % 2][64:128, 0:128], AF.Copy, ["ps%d" % (2 + k % 2)], ["Cst" + kd], scale=-1.0)
        if "coef" in self.dbg and l == 0:
            self.dump("d_ARn0", ARn[0][:, :, :], [128, NLV, 32], ["ARn_0"]); self.dump("d_OFn0", OFn[0][:, :, :], [128, NLV, 32], ["OFn_0"])
            self.dump("d_Cst0", Cst[0][:, :], [128, 512], ["Cst_0"]); self.dump("d_BT0", BT[0][:, :, :], [128, 4, 128], ["BT_0"], BF16)
        self.P.fence()
        self.sb_ptr = main_base
        NG = 4
        Hb = [B.sb("Hb%d" % s, [128, NT], BF16) for s in range(NG)]
        yacc = B.sb("yacc", [128, NT])
        Cpad = [B.sb("Cpad%d" % d, [128, 8, 128], BF16) for d in range(2)]
        Dm = [[B.sb("Dm%d_%d" % (s, i), [128, 128], BF16) for i in range(2)] for s in range(NG)]
        um = [[B.sb("um%d_%d" % (s, i), [128, 512], BF16) for i in range(1)] for s in range(NG)]
        gt = [B.sb("gt%d" % i, [128, 256]) for i in range(2)]
        for d in range(2):
            B.memset("pool", Cpad[d][:, :, :], 0.0, ["Cpad_%d" % d])
        CH = [(c * 512, min(512, NT - c * 512)) for c in range(5)]

        def pos(d, t0):
            if d == 0:
                return t0
            return t0 - NCTX if t0 >= NCTX else NLAT + t0

        def hkeys(s, a, b):
            return ["H%d_%d" % (s, c) for c in range(a // 512, (b - 1) // 512 + 1)]

        def job(s, k, g8, d):
            g = k * 8 + g8
            H = Hb[s]
            pA = ps[2 * s]; pAk = "ps%d" % (2 * s); pB = ps[2 * s + 1]; pBk = "ps%d" % (2 * s + 1)
            banks = [(pA, pAk), (pB, pBk)]
            ecnt = [0]

            def evac(dst, src, rd, wr):
                ecnt[0] += 1
                B.cp("act" if ecnt[0] % 2 else "dve", dst, src, rd, wr)
            for j, (t0, n) in enumerate(TCH):
                u_ = um[s][0]; umk = "um%d_0" % s
                pb, pk = banks[j % 2]
                B.ts("pool", u_[:, :n], uT[:, k, t0:t0 + n], self.gmaskf[:, g8:g8 + 1], None, ALU.mult, None, [uk(k, j), "gmaskf"], [umk])
                B.mm(pb[:, :n], BT[d][:, k, :], u_[:, :n], True, True, ["BT_%d" % d, umk], [pk])
                p0 = pos(d, t0)
                evac(H[:, p0:p0 + n], pb[:, :n], [pk], hkeys(s, p0, p0 + n))
                yield
            for i in range(NLV):
                sh = 2 ** i
                dm = Dm[s][i % 2]; dmk = "Dm%d_%d" % (s, i % 2)
                B.ts("pool", dm[:, :], self.identf[:, :], ARn[d][:, i, g:g + 1], None, ALU.mult, None, ["identf", "ARn_%d" % d], [dmk])
                B.stt(dm[:, :], self.swpf[:, :], OFn[d][:, i, g:g + 1], dm[:, :], ALU.mult, ALU.add, ["swpf", "OFn_%d" % d, dmk], [dmk])
                order = list(range(4, -1, -1)) if d == 0 else list(range(5))
                for ci, c in enumerate(order):
                    lo, n = CH[c]; hi = lo + n
                    pb, pk = banks[ci % 2]
                    if d == 0:
                        a = max(lo, sh); b = hi
                        has = b > a
                        sa, sb_ = a - sh, b - sh
                    else:
                        a = lo; b = min(hi, NT - sh)
                        has = b > a
                        sa, sb_ = a + sh, b + sh
                    if ci % 2 == 0:
                        B.mm(pb[:, :n], self.identb[:, :], H[:, lo:hi], True, not has, ["identb"] + hkeys(s, lo, hi), [pk])
                        if has:
                            B.mm(pb[:, a - lo:b - lo], dm[:, :], H[:, sa:sb_], False, True, [dmk] + hkeys(s, sa, sb_), [pk], skip_group_check=True)
                        B.cp("act", H[:, lo:hi], pb[:, :n], [pk], hkeys(s, lo, hi))
                    elif has:
                        B.mm(pb[:, a - lo:b - lo], dm[:, :], H[:, sa:sb_], True, True, [dmk] + hkeys(s, sa, sb_), [pk])
                        B.tt("dve", H[:, a:b], H[:, a:b], pb[:, a - lo:b - lo], ALU.add, [pk] + hkeys(s, lo, hi), hkeys(s, lo, hi))
                    yield
            for j, (t0, n) in enumerate(TCH):
                if self.last and j == 0:
                    continue
                pb, pk = banks[j % 2]
                p0 = pos(d, t0)
                B.mm(pb[:, :n], Cpad[d][:, g8, :], H[:, p0:p0 + n], True, True, ["Cpad_%d" % d] + hkeys(s, p0, p0 + n), [pk])
                B.tt("dve", yacc[:, t0:t0 + n], yacc[:, t0:t0 + n], pb[:, :n], ALU.add, ["yacc_%d" % j, pk], ["yacc_%d" % j])
                yield

        for k in range(4):
            for d in range(2):
                B.cp("pool", XAP(Cpad[d], 0, [[1024, 128], [144, 8], [1, 16]]),
                     Cst[d][:, k * 128:(k + 1) * 128].rearrange("p (g c) -> p g c", c=16), ["Cst_%d" % d], ["Cpad_%d" % d])
            for j, (t0, n) in enumerate(TCH):
                if self.last and j == 0:
                    continue
                B.ts("dve", yacc[:, t0:t0 + n], uT[:, k, t0:t0 + n], dsk[:, k:k + 1], None, ALU.mult, None, [uk(k, j), "dsk"], ["yacc_%d" % j])
            jobs = [(g8, d) for g8 in range(8) for d in range(2)]
            for r in range(0, 16, NG):
                gens = [job(s, k, jobs[r + s][0], jobs[r + s][1]) for s in range(NG)]
                alive = list(gens)
                while alive:
                    nxt = []
                    for g_ in alive:
                        try:
                            next(g_)
                            nxt.append(g_)
                        except StopIteration:
                            pass
                    alive = nxt
            if "ssm" in self.dbg and l == 0:
                self.dump("d_yacc%d" % k, yacc[:, :], [128, NT], ["yacc_%d" % j for j in range(5)])
            for j, (t0_, n_) in enumerate(TCH):
                if self.last and j == 0:
                    continue
                for t0 in range(t0_, t0_ + n_, 256):
                    n = 256
                    ya = yacc[:, t0:t0 + n]
                    ga, gb_ = gt[0], gt[1]
                    gak, gbk = "gt0", "gt1"
                    B.act(ga[:, :n], ya, AF.Square, ["yacc_%d" % j], [gak])
                    B.ts("dve", ga[:, :n], ga[:, :n], 0.044715, 1.0, ALU.mult, ALU.add, [gak], [gak])
                    B.tt("pool", ga[:, :n], ga[:, :n], ya, ALU.mult, [gak, "yacc_%d" % j], [gak])
                    B.act(gb_[:, :n], ga[:, :n], AF.Sigmoid, [gak], [gbk], scale=1.5957691216057308)
                    B.tt("dve", uT[:, k, t0:t0 + n], ya, gb_[:, :n], ALU.mult, ["yacc_%d" % j, gbk], [uk(k, j)])
        self.P.fence()
        self.sb_ptr = main_base
        wg = B.sb("wglu", [128, 4, 512], BF16)
        sgl = [B.sb("sgl%d" % i, [128, 512]) for i in range(2)]
        B.dma(WQ, wg[:, :, :], self.W("w_glu")[l, :, :].rearrange("(c p) n -> p c n", p=128), (), ["wglu"])
        cnt = 0
        for oc in range(4):
            for j, (t0, n) in enumerate(TCH):
                if self.last and j == 0:
                    continue
                pb = ps[cnt % 4]; pk = "ps%d" % (cnt % 4)
                sg_ = sgl[cnt % 2]; sgk = "sgl%d" % (cnt % 2)
                cnt += 1
                for kc in range(4):
                    B.mm(pb[:, :n], wg[:, kc, oc * 128:(oc + 1) * 128], uT[:, kc, t0:t0 + n], kc == 0, kc == 3, ["wglu", uk(kc, j)], [pk])
                B.act(sg_[:, :n], pb[:, :n], AF.Sigmoid, [pk, "bglu"], [sgk], bias=bglu[:, oc:oc + 1])
                B.tt("dve", ssmT[:, oc, t0:t0 + n], uT[:, oc, t0:t0 + n], sg_[:, :n], ALU.mult, [uk(oc, j), sgk], ["ss%d_%d" % (oc, j)])
        if "glu" in self.dbg and l == 0:
            self.dump("d_ssmT", ssmT[:, :, :], [128, 4, NT], ["ss%d_%d" % (c, j) for c in range(4) for j in range(5)], BF16)
        self.phase(reserve=4 * NT * 2)


def _consts():
    ident = np.eye(128, dtype=np.float32)
    rmat = np.zeros((128, 128), np.float32)
    for m in range(128):
        d = m % 32
        if d < 16:
            rmat[m + 16, m] = -1.0
        else:
            rmat[m - 16, m] = 1.0
    t = np.arange(NLAT)
    row = (t // 64).astype(np.float32)
    col = (t % 64).astype(np.float32)
    inv = (1.0 / (np.float32(10000.0) ** (np.arange(0, 32, 2, dtype=np.float32) / np.float32(32.0)))).astype(np.float32)
    cos = np.zeros((128, NLAT), np.float32)
    sin = np.zeros((128, NLAT), np.float32)
    for p in range(128):
        d = p % 64
        axis = d // 32
        f = d % 16
        posv = row if axis == 0 else col
        ang = (posv * inv[f]).astype(np.float32)
        cos[p] = np.cos(ang)
        sin[p] = np.sin(ang)
    swp = np.zeros((128, 128), np.float32)
    for k in range(128):
        swp[k, (k + 64) % 128] = 1.0
    gmask = np.zeros((128, 8), np.float32)
    for p in range(128):
        gmask[p, p // 16] = 1.0
    sgn = np.ones((128, 2), np.float32)
    sgn[64:, 0] = -1.0
    sgn[:64, 1] = -1.0
    return dict(ident=ident, rmat=rmat, ropecos=cos, ropesin=sin, swp=swp, gmask=gmask, sgn=sgn)


_CACHE = {}


def _get_prog(n_layers, dbg, skip):
    key = (n_layers, tuple(dbg), tuple(skip))
    if key not in _CACHE:
        b = Builder(n_layers, dbg, skip)
        b.build()
        _CACHE[key] = b
    return _CACHE[key]


def _in_maps(inputs, names, cores):
    f = lambda a: np.ascontiguousarray(np.asarray(a, dtype=np.float32))
    skipk = ("x", "c", "ctx", "c_ctx", "lam_q1", "lam_k1", "lam_q2", "lam_k2")
    shared = {k: f(v) for k, v in inputs.items() if k not in skipk and k in names}
    if "lamv" in names:
        shared["lamv"] = np.ascontiguousarray(np.stack([f(inputs["lam_q1"]), f(inputs["lam_k1"]), f(inputs["lam_q2"]), f(inputs["lam_k2"])], axis=1))
    for k, v in _consts().items():
        if k in names:
            shared[k] = v
    maps = []
    for b in range(cores):
        m = dict(shared)
        m["x"] = f(inputs["x"][b])
        m["ctx"] = f(inputs["ctx"][b])
        m["cvec"] = np.ascontiguousarray(np.stack([f(inputs["c"][b]), f(inputs["c_ctx"])], axis=0))
        maps.append(m)
    return maps


def run(inputs, n_layers=DEPTH, dbg=(), skip=(), cores=8):
    b = _get_prog(n_layers, dbg, skip)
    in_names = set(k for k, v in b.dram.items())
    maps = _in_maps(inputs, in_names, cores)
    maps = [{k: v for k, v in m.items() if k in in_names} for m in maps]
    res = run_bass_kernel_spmd(b.nc, maps, core_ids=list(range(cores)))
    return res.results


def kernel(**inputs):
    res = run(inputs)
    return np.stack([np.asarray(r["out"], dtype=np.float32) for r in res], axis=0)
```

```python
import math
import numpy as np
import concourse.bass as bass
import concourse.mybir as mybir
from concourse.bass_utils import run_bass_kernel_spmd

F32 = mybir.dt.float32
BF16 = mybir.dt.bfloat16
I32 = mybir.dt.int32
AF = mybir.ActivationFunctionType
ALU = mybir.AluOpType
AX = mybir.AxisListType

DEPTH = 4
D = 1024
NCTX = 256
NLAT = 2048
NT = NCTX + NLAT
ALPHA = (2.0 * DEPTH) ** 0.25
LN_EPS = 1e-5
TCH = [(0, 256), (256, 512), (768, 512), (1280, 512), (1792, 512)]
ENGS = ("pe", "act", "dve", "pool", "sp")
WQ = "pool"
SB_BASE = 16512
SB_TOP = 229344


class Prog:
    def __init__(self, nc, n_dma_sems=8, same_engine_sync=True):
        self.nc = nc
        self.ops = []
        self.res = {}
        self.same = same_engine_sync
        self.n_dma_sems = n_dma_sems
        self.dma_rr = {e: 0 for e in ENGS}
        self.dma_last = {}
        self.last_op = {}
        self.open_dmas = []
        self.fence_deps = set()

    def op(self, eng, fn, rd=(), wr=(), kind="c"):
        wr = list(wr) + [r for r in rd if r.startswith("ps") and r not in wr]
        deps = set()
        for r in rd:
            st = self.res.get(r)
            if st is not None and st[0] is not None:
                deps.add(st[0])
        for r in wr:
            st = self.res.get(r)
            if st is not None:
                if st[0] is not None:
                    deps.add(st[0])
                for o in st[1].values():
                    deps.add(o)
        deps.update(self.fence_deps)
        oid = len(self.ops)
        rec = dict(eng=eng, fn=fn, deps=deps, kind=kind, slot=None)
        if kind == "dma":
            self.open_dmas.append(oid)
        else:
            self.last_op[eng] = oid
        if kind == "dma":
            slot = self.dma_rr[eng] % self.n_dma_sems
            self.dma_rr[eng] += 1
            rec["slot"] = slot
            prev = self.dma_last.get((eng, slot))
            if prev is not None:
                deps.add(prev)
            self.dma_last[(eng, slot)] = oid
        self.ops.append(rec)
        for r in rd:
            st = self.res.setdefault(r, [None, {}])
            st[1][(eng, oid if kind == "dma" else -1)] = oid
        for r in wr:
            self.res[r] = [oid, {}]
        return oid

    def fence(self):
        f = set(self.last_op.values())
        f.update(self.open_dmas)
        self.open_dmas = []
        self.fence_deps = f
        self.res = {}

    def dma(self, eng, out, in_, rd=(), wr=(), **kw):
        return self.op(eng, lambda e: e.dma_start(out=out, in_=in_, **kw), rd, wr, kind="dma")

    def emit(self, final_wait_ops=()):
        nc = self.nc
        ops = self.ops
        needed = set()
        for o in ops:
            needed.update(o["deps"])
        needed.update(final_wait_ops)
        esem = {e: nc.alloc_semaphore("s_" + e) for e in ENGS}
        dsem = {}
        for (e, s) in self.dma_last:
            dsem[(e, s)] = nc.alloc_semaphore("d_%s%d" % (e, s))
        ecount = {e: 0 for e in ENGS}
        dcount = {k: 0 for k in dsem}
        for i, o in enumerate(ops):
            if o["kind"] == "dma":
                k = (o["eng"], o["slot"])
                dcount[k] += 16
                o["sig"] = (dsem[k], dcount[k], ("d",) + k)
            elif i in needed:
                ecount[o["eng"]] += 1
                o["sig"] = (esem[o["eng"]], ecount[o["eng"]], ("e", o["eng"]))
            else:
                o["sig"] = None
        streams = {e: [] for e in ENGS}
        seen = {e: {} for e in ENGS}
        for i, o in enumerate(ops):
            e = o["eng"]
            waits = {}
            for d in o["deps"]:
                od = ops[d]
                if od["kind"] != "dma" and od["eng"] == e and (e == "pe" or not self.same):
                    continue
                sem, val, key = od["sig"]
                if seen[e].get(key, 0) >= val:
                    continue
                if key not in waits or waits[key][1] < val:
                    waits[key] = (sem, val)
            for key, (sem, val) in waits.items():
                seen[e][key] = val
            streams[e].append((list(waits.values()), o))
        fin = [(ops[d]["sig"][0], ops[d]["sig"][1]) for d in final_wait_ops]
        self.n_instr = {e: len(streams[e]) for e in ENGS}

        def run(engname, engobj):
            for waits, o in streams[engname]:
                for sem, val in waits:
                    engobj.wait_ge(sem, val)
                ins = o["fn"](engobj)
                if o["sig"] is not None:
                    ins.then_inc(o["sig"][0], 16 if o["kind"] == "dma" else 1)
            if engname == "sp":
                for sem, val in fin:
                    engobj.wait_ge(sem, val)

        with nc.Block() as block:
            @block.tensor
            def _(t):
                run("pe", t)

            @block.scalar
            def _(t):
                run("act", t)

            @block.vector
            def _(t):
                run("dve", t)

            @block.gpsimd
            def _(t):
                run("pool", t)

            @block.sync
            def _(t):
                run("sp", t)


def XAP(t, offset, dims):
    return bass.AP(t, offset, [list(d) for d in dims])


class Builder:
    def __init__(self, n_layers=DEPTH, dbg=(), skip=()):
        self.stop = [x[5:] for x in skip if x.startswith("stop:")]
        self.n_layers = n_layers
        self.dbg = set(dbg)
        self.skip = set(skip)
        self.nc = bass.Bass("TRN2", target_bir_lowering=False)
        self.P = Prog(self.nc)
        self.out_ops = []
        self.dram = {}
        self.uid = 0
        self.sb_ptr = SB_BASE
        self.ph_base = None

    def din(self, name, shape, dt=F32):
        if name not in self.dram:
            self.dram[name] = self.nc.dram_tensor(name, list(shape), dt, kind="ExternalInput").ap()
        return self.dram[name]

    def dout(self, name, shape, dt=F32):
        self.dram[name] = self.nc.dram_tensor(name, list(shape), dt, kind="ExternalOutput").ap()
        return self.dram[name]

    def sb(self, name, shape, dt=F32, at=None):
        esz = 2 if dt == BF16 else 4
        nbytes = esz
        for d_ in shape[1:]:
            nbytes *= d_
        nbytes = (nbytes + 31) // 32 * 32
        if at is None:
            off = self.sb_ptr
            self.sb_ptr += nbytes
        else:
            off = self.ph_base + at
        assert off + nbytes <= SB_TOP, (name, off, nbytes)
        self.uid += 1
        return self.nc.alloc_sbuf_tensor_at("%s_%d" % (name, self.uid), list(shape), dt, offset=off)

    def phase(self, reserve=0):
        self.P.fence()
        self.sb_ptr = self.ph_base + reserve

    def mm(self, out, lhsT, rhs, start, stop, rd, wr, **kw):
        self.P.op("pe", lambda e: e.matmul(out, lhsT=lhsT, rhs=rhs, start=start, stop=stop, **kw), rd, wr)

    def tr(self, out, in_, ident, rd, wr):
        self.P.op("pe", lambda e: e.transpose(out, in_, ident), rd, wr)

    def act(self, out, in_, func, rd, wr, **kw):
        self.P.op("act", lambda e: e.activation(out=out, in_=in_, func=func, **kw), rd, wr)

    def tt(self, eng, out, in0, in1, op, rd, wr):
        self.P.op(eng, lambda e: e.tensor_tensor(out=out, in0=in0, in1=in1, op=op), rd, wr)

    def ts(self, eng, out, in0, s1, s2, op0, op1, rd, wr):
        if op1 is None and eng == "pool" and op0 == ALU.mult:
            op1 = ALU.add
            s2 = 0.0
        if op1 is None:
            self.P.op(eng, lambda e: e.tensor_scalar(out=out, in0=in0, scalar1=s1, scalar2=None, op0=op0), rd, wr)
        else:
            self.P.op(eng, lambda e: e.tensor_scalar(out=out, in0=in0, scalar1=s1, scalar2=s2, op0=op0, op1=op1), rd, wr)

    def stt(self, out, in0, scalar, in1, op0, op1, rd, wr):
        self.P.op("dve", lambda e: e.scalar_tensor_tensor(out=out, in0=in0, scalar=scalar, in1=in1, op0=op0, op1=op1), rd, wr)

    def cp(self, eng, out, in_, rd, wr):
        if eng == "act":
            self.act(out, in_, AF.Copy, rd, wr)
        else:
            self.P.op(eng, lambda e: e.tensor_copy(out=out, in_=in_), rd, wr)

    def red(self, out, in_, op, rd, wr):
        self.P.op("dve", lambda e: e.tensor_reduce(out=out, in_=in_, axis=AX.X, op=op), rd, wr)

    def recip(self, out, in_, rd, wr):
        self.P.op("dve", lambda e: e.reciprocal(out=out, in_=in_), rd, wr)

    def memset(self, eng, ap, val, wr):
        self.P.op(eng, lambda e: e.memset(ap, val), (), wr)

    def dma(self, q, out, in_, rd, wr, **kw):
        return self.P.dma(q, out, in_, rd, wr, **kw)

    def dmas(self, q, out, in_, rd, wr):
        return self.P.dma(q, out, in_, rd, wr, allow_slow_non_contiguous=True)

    def dump(self, name, ap_sb, shape, rd, dt=F32):
        d = self.dout(name, shape, F32)
        if len(shape) == 3:
            for c in range(shape[1]):
                self.out_ops.append(self.dma(WQ, d[:, c, :], ap_sb[:, c, :], rd, ()))
        else:
            self.out_ops.append(self.dma(WQ, d, ap_sb, rd, ()))

    def W(self, name):
        shapes = {
            "w_mod": [DEPTH, D, 6 * D], "b_mod": [DEPTH, 6 * D], "w_in": [DEPTH, D, 5632], "lamv": [DEPTH, 4, 64],
            "subln_g": [DEPTH, 128], "w_pa": [DEPTH, D, D], "w_ps": [DEPTH, 512, D], "w_o": [DEPTH, D, D],
            "w_glu": [DEPTH, 512, 512], "b_glu": [DEPTH, 512], "ln1_g": [DEPTH, D], "ln1_b": [DEPTH, D],
            "ln2_g": [DEPTH, D], "ln2_b": [DEPTH, D], "router_g_w": [DEPTH, D, 4], "router_g_b": [DEPTH, 4],
            "router_e_w": [DEPTH, D, 32], "router_e_b": [DEPTH, 32], "moe_w1": [DEPTH, 32, D, 512],
            "moe_w3": [DEPTH, 32, D, 512], "moe_w2": [DEPTH, 32, 512, D],
            "ssm_a_re": [DEPTH, 2, 32, 64], "ssm_a_im": [DEPTH, 2, 32, 64], "ssm_log_dt": [DEPTH, 2, 32],
            "ssm_b_re": [DEPTH, 2, 32, 64, 16], "ssm_b_im": [DEPTH, 2, 32, 64, 16],
            "ssm_c_re": [DEPTH, 2, 32, 16, 64], "ssm_c_im": [DEPTH, 2, 32, 16, 64], "ssm_d": [DEPTH, 512],
            "x": [NLAT, D], "ctx": [NCTX, D], "cvec": [2, D], "ident": [128, 128], "rmat": [128, 128],
            "ropecos": [128, NLAT], "ropesin": [128, NLAT], "swp": [128, 128], "gmask": [128, 8], "sgn": [128, 2],
        }
        return self.din(name, shapes[name])

    @staticmethod
    def hk(c, j):
        return "hT%d_%d" % (c, j)

    @staticmethod
    def xk(c, j):
        return "xT%d_%d" % (c, j)

    @staticmethod
    def bk(c, j):
        return "bg%d_%d" % (c, j)

    @staticmethod
    def tj(ti):
        return 0 if ti < 2 else 1 + (ti - 2) // 4

    def vecload(self, dst, src_row_ap, key, n=None):
        n = dst.shape[1]
        st = self.vstage[self.vcnt % 2]; sk = "vstage%d" % (self.vcnt % 2)
        self.vcnt += 1
        self.dma("sp", st[0:n, :], src_row_ap.rearrange("(c p) -> c p", p=128), (), [sk])
        self.tr(self.ps[7][:, 0:n], st[0:n, :], self.identf[0:n, 0:n], [sk, "identf"], ["ps7"])
        self.cp("dve", dst, self.ps[7][:, 0:n], ["ps7"], [key])

    def bcast_load(self, dst, src_ap, n, key):
        st = self.bstage
        self.dma("sp", st[0:1, 0:n], src_ap, (), ["bstage"])
        self.mm(self.ps[7][:, 0:n], self.ones1[0:1, :], st[0:1, 0:n], True, True, ["bstage", "ones1"], ["ps7"])
        self.cp("dve", dst, self.ps[7][:, 0:n], ["ps7"], [key])

    def build(self):
        B = self
        nc = self.nc
        L = self.n_layers
        x_d = B.W("x"); ctx_d = B.W("ctx"); cvec_d = B.W("cvec")
        out_d = B.dout("out", [NLAT, D])
        self.xT = xT = B.sb("xT", [128, 8, NT]); self.hT = B.sb("hT", [128, 8, NT], BF16)
        self.identf = B.sb("identf", [128, 128]); self.identb = B.sb("identb", [128, 128], BF16)
        self.rmatb = B.sb("rmatb", [128, 128], BF16)
        self.cosb = B.sb("cosb", [128, NLAT], BF16); self.sinb = B.sb("sinb", [128, NLAT], BF16)
        self.onesf = B.sb("onesf", [128, 128]); self.zerob = B.sb("zerob", [128, 512], BF16)
        self.epsc = B.sb("epsc", [128, 1]); self.negpi = B.sb("negpi", [128, 1])
        self.modx = B.sb("modx", [128, 48]); self.cmodx = B.sb("cmodx", [128, 48]); self.bmod = B.sb("bmod", [128, 48])
        self.cv = B.sb("cv", [128, 2, 8]); self.scv = B.sb("scv", [128, 8, 2], BF16)
        self.vstage = [B.sb("vstage%d" % i, [48, 128]) for i in range(2)]; self.vcnt = 0
        self.bstage = B.sb("bstage", [1, 256]); self.ones1 = B.sb("ones1", [1, 128])
        self.swpf = B.sb("swpf", [128, 128]); self.gmaskf = B.sb("gmaskf", [128, 8]); self.sgnf = B.sb("sgnf", [128, 2])
        self.ph_base = (self.sb_ptr + 63) // 64 * 64
        self.ps = ps = [nc.alloc_psum_tensor("ps%d" % i, [128, 512], F32) for i in range(8)]
        PSK = lambda i: "ps%d" % i
        identf = self.identf

        B.dma("sp", identf[:, :], B.W("ident")[:, :], (), ["identf"])
        B.dma(WQ, self.identb[:, :], B.W("ident")[:, :], (), ["identb"])
        B.dma(WQ, self.rmatb[:, :], B.W("rmat")[:, :], (), ["rmatb"])
        B.dma(WQ, self.cosb[:, :], B.W("ropecos")[:, :], (), ["cosb"])
        B.dma(WQ, self.sinb[:, :], B.W("ropesin")[:, :], (), ["sinb"])
        B.dma("sp", self.swpf[:, :], B.W("swp")[:, :], (), ["swpf"])
        B.dma("sp", self.gmaskf[:, :], B.W("gmask")[:, :], (), ["gmaskf"])
        B.dma("sp", self.sgnf[:, :], B.W("sgn")[:, :], (), ["sgnf"])
        B.memset("dve", self.onesf[:, :], 1.0 / D, ["onesf"])
        B.memset("dve", self.zerob[:, :], 0.0, ["zerob"])
        B.memset("dve", self.epsc[:, :], LN_EPS, ["epsc"])
        B.memset("dve", self.negpi[:, :], -math.pi, ["negpi"])
        B.memset("dve", self.ones1[:, :], 1.0, ["ones1"])

        self.phase()
        stg = [B.sb("stg%d" % i, [128, D]) for i in range(2)]
        for ti in range(NT // 128):
            s = stg[ti % 2]; sk = "stg%d" % (ti % 2)
            src = ctx_d[ti * 128:(ti + 1) * 128, :] if ti < 2 else x_d[(ti - 2) * 128:(ti - 1) * 128, :]
            B.dma("sp", s[:, :], src, (), [sk])
            j = self.tj(ti)
            for half in range(2):
                pb = ps[half]
                for cc in range(4):
                    c = half * 4 + cc
                    B.tr(pb[:, cc * 128:(cc + 1) * 128], s[:, c * 128:(c + 1) * 128], identf[:, :], [sk, "identf"], [PSK(half)])
                eng = "dve" if half == 0 else "act"
                B.cp(eng, XAP(xT, half * 4 * NT + ti * 128, [[8 * NT, 128], [NT, 4], [1, 128]]),
                     pb[:, :].rearrange("p (c t) -> p c t", t=128), [PSK(half)], [self.xk(c, j) for c in range(half * 4, half * 4 + 4)])
        for r in range(2):
            self.vecload(self.cv[:, r, :], cvec_d[r, :], "cv%d" % r)
        B.act(self.scv[:, :, :].rearrange("p c r -> p r c"), self.cv[:, :, :], AF.Silu, ["cv0", "cv1"], ["scv"])

        for l in range(L):
            self.layer(l)

        self.phase()
        ostg = [B.sb("ostg%d" % i, [128, D]) for i in range(2)]
        for ti in range(2, NT // 128):
            s = ostg[ti % 2]; sk = "ostg%d" % (ti % 2)
            j = self.tj(ti)
            for half in range(2):
                pb = ps[half]
                for cc in range(4):
                    c = half * 4 + cc
                    B.tr(pb[:, cc * 128:(cc + 1) * 128], xT[:, c, ti * 128:(ti + 1) * 128], identf[:, :], [self.xk(c, j), "identf"], [PSK(half)])
                eng = "dve" if half == 0 else "act"
                B.cp(eng, s[:, half * 512:(half + 1) * 512], pb[:, :], [PSK(half)], [sk + "_%d" % half])
            self.out_ops.append(B.dma("sp", out_d[(ti - 2) * 128:(ti - 1) * 128, :], s[:, :], [sk + "_0", sk + "_1"], ()))
        self.P.emit(final_wait_ops=self.out_ops)
        return nc

    def layer(self, l):
        B = self
        self.last = (l == DEPTH - 1)
        ps = self.ps
        PSK = lambda i: "ps%d" % i
        xT, hT = self.xT, self.hT
        modx, cmodx, bmod = self.modx, self.cmodx, self.bmod
        w_mod = self.W("w_mod"); b_mod = self.W("b_mod")
        self.phase()
        wm = [B.sb("wm%d" % i, [128, 8, 512], BF16) for i in range(2)]
        self.vecload(bmod[:, :], b_mod[l, :], "bmod")
        for blk in range(12):
            wt = wm[blk % 2]; wk_ = "wm%d" % (blk % 2)
            B.dma(WQ, wt[:, :, :], w_mod[l, :, blk * 512:(blk + 1) * 512].rearrange("(c p) n -> p c n", p=128), (), [wk_])
            for jj in range(4):
                j = blk * 4 + jj
                for kc in range(8):
                    B.mm(ps[0][:, 2 * j:2 * j + 2], wt[:, kc, jj * 128:(jj + 1) * 128], self.scv[:, kc, :], kc == 0, kc == 7,
                         [wk_, "scv"], [PSK(0)])
        pv = ps[0][:, 0:96].rearrange("p (j t) -> p j t", t=2)
        B.tt("dve", modx[:, :], pv[:, :, 0], bmod[:, :], ALU.add, [PSK(0), "bmod"], ["modx"])
        B.tt("dve", cmodx[:, :], pv[:, :, 1], bmod[:, :], ALU.add, [PSK(0), "bmod"], ["cmodx"])
        for m_, k_ in ((modx, "modx"), (cmodx, "cmodx")):
            B.ts("dve", m_[:, 8:16], m_[:, 8:16], 1.0, None, ALU.add, None, [k_], [k_])
            B.ts("dve", m_[:, 32:40], m_[:, 32:40], 1.0, None, ALU.add, None, [k_], [k_])
        if "mod" in self.dbg and l == 0:
            self.dump("d_modx", modx[:, :], [128, 48], ["modx"]); self.dump("d_cmodx", cmodx[:, :], [128, 48], ["cmodx"])
        if "mod" in self.stop:
            return
        self.modulate(0)
        if "h" in self.dbg and l == 0:
            self.dump("d_hT", hT[:, :, :], [128, 8, NT], [self.hk(c, j) for c in range(8) for j in range(5)], BF16)
        if "h" in self.stop:
            return
        self.phase()
        self.big = B.sb("attnT", [128, 8, NT], BF16)
        if "attn" not in self.skip:
            self.attn_prep(l)
            import os
            self.att_stage = int(os.environ.get("ATT_STAGE", "9"))
            for h in range(int(os.environ.get("ATT_HEADS", "8"))):
                self.attention(l, h)
            if "attn" in self.dbg and l == 0:
                self.dump("d_attnT", self.big[:, :, :], [128, 8, NT], [self.bk(c, j) for c in range(8) for j in range(5)], BF16)
        if "attn" in self.stop:
            return
        self.phase(reserve=8 * NT * 2)
        self.scale_x()
        if "attn" not in self.skip:
            self.merge(l, "attn")
        if "amerge" in self.stop:
            return
        if "ssm" not in self.skip:
            self.ssm(l)
            self.merge(l, "ssm")
        self.phase()
        self.layernorm(l, 1)
        if "mid" in self.dbg and l == 0:
            self.dump("d_xmid", xT[:, :, :], [128, 8, NT], [self.xk(c, j) for c in range(8) for j in range(5)])
        self.modulate(24)
        if "moe" not in self.skip:
            self.router(l)
        self.scale_x()
        if "moe" not in self.skip:
            self.moe(l)
        self.phase()
        self.layernorm(l, 2)

    def modulate(self, base):
        B = self
        xT, hT = self.xT, self.hT
        for c in range(8):
            for j, (t0, n) in enumerate(TCH):
                if self.last and j == 0 and base != 0:
                    continue
                m_, k_ = (self.cmodx, "cmodx") if j == 0 else (self.modx, "modx")
                B.act(hT[:, c, t0:t0 + n], xT[:, c, t0:t0 + n], AF.Identity, [self.xk(c, j), k_], [self.hk(c, j)],
                      scale=m_[:, base + 8 + c:base + 9 + c], bias=m_[:, base + c:base + c + 1])

    def scale_x(self):
        B = self
        xT = self.xT
        for c in range(8):
            for j, (t0, n) in enumerate(TCH):
                eng = "pool" if (c + j) % 2 else "dve"
                B.ts(eng, xT[:, c, t0:t0 + n], xT[:, c, t0:t0 + n], float(ALPHA), None, ALU.mult, None, [self.xk(c, j)], [self.xk(c, j)])

    def attn_prep(self, l):
        B = self
        at = {}
        at["wa"] = [B.sb("wa%d" % i, [128, 8, 128], BF16) for i in range(2)]
        at["wb"] = [B.sb("wb%d" % i, [128, 8, 128], BF16) for i in range(2)]
        at["wc"] = [B.sb("wc%d" % i, [128, 8, 128], BF16) for i in range(2)]
        at["kT"] = B.sb("kT", [128, NT], BF16); at["qT"] = B.sb("qT", [128, NT], BF16)
        at["Vh"] = B.sb("Vh", [128, 18, 129], BF16)
        at["pt"] = [B.sb("pt%d" % i, [128, 512], BF16) for i in range(4)]
        at["tb"] = [B.sb("tb%d" % i, [128, 512], BF16) for i in range(2)]
        at["t1"] = [B.sb("t1_%d" % i, [128, 512]) for i in range(1)]
        at["t2"] = [B.sb("t2_%d" % i, [128, 512]) for i in range(1)]
        at["lamt"] = B.sb("lamt", [128, 4, 64]); at["lamp"] = B.sb("lamp", [128, 2, 64]); at["lams"] = B.sb("lams", [128, 4])
        at["nlam"] = B.sb("nlam", [128, 1]); at["g128"] = B.sb("g128", [128, 128])
        at["sm"] = [B.sb("sm%d" % i, [128, 8]) for i in range(4)]
        at["of"] = [B.sb("of%d" % i, [128, 128]) for i in range(2)]
        at["ot"] = [B.sb("ot%d" % i, [128, 128]) for i in range(2)]
        at["onb"] = [B.sb("onb%d" % i, [128, 128], BF16) for i in range(2)]
        at["osb"] = [B.sb("osb%d" % i, [128, 258]) for i in range(8)]
        self.at = at
        B.memset("pool", at["Vh"][:, :, :], 1.0, ["Vh"])
        lamv = self.W("lamv"); subg = self.W("subln_g")
        lam_init = 0.8 - 0.6 * math.exp(-0.3 * l)
        self.bcast_load(at["lamt"][:, :, :].rearrange("p a b -> p (a b)"), lamv[l:l + 1, :, :].rearrange("o a b -> o (a b)"), 256, "lamt")
        self.bcast_load(at["g128"][:, :], subg[l:l + 1, :], 128, "g128")
        B.ts("dve", at["g128"][:, :], at["g128"][:, :], float(1.0 - lam_init), None, ALU.mult, None, ["g128"], ["g128"])
        B.tt("dve", at["lamp"][:, 0, :], at["lamt"][:, 0, :], at["lamt"][:, 1, :], ALU.mult, ["lamt"], ["lamp"])
        B.tt("dve", at["lamp"][:, 1, :], at["lamt"][:, 2, :], at["lamt"][:, 3, :], ALU.mult, ["lamt"], ["lamp"])
        B.red(at["lams"][:, 0:2], at["lamp"][:, :, :], ALU.add, ["lamp"], ["lams"])
        B.act(at["lams"][:, 2:4], at["lams"][:, 0:2], AF.Exp, ["lams"], ["lams2"])
        B.tt("dve", at["nlam"][:, :], at["lams"][:, 3:4], at["lams"][:, 2:3], ALU.subtract, ["lams2"], ["nlam"])
        B.ts("dve", at["nlam"][:, :], at["nlam"][:, :], float(-lam_init), None, ALU.add, None, ["nlam"], ["nlam"])

    def proj_fm(self, wt, wkey, nkc, src, srck, dst_fn, dkey_fn, rope=False, tiles=None, evac="act"):
        B = self
        ps = self.ps
        for j, (t0, n) in enumerate(TCH):
            pb = ps[j % 2]; pk = "ps%d" % (j % 2)
            for kc in range(nkc):
                B.mm(pb[:, :n], wt[:, kc, :], src[:, kc, t0:t0 + n], kc == 0, kc == nkc - 1, [wkey, srck(kc, j)], [pk])
            import os
            rm = int(os.environ.get("ROPE_MODE", "2"))
            if j == 0 or not rope or rm == 0:
                B.cp(evac if j % 2 == 0 else "dve", dst_fn(t0, n), pb[:, :n], [pk], [dkey_fn(j)])
            else:
                tb = tiles["tb"][j % 2]; tbk = "tb%d" % (j % 2)
                t1 = tiles["t1"][0]; t1k = "t1_0"
                t2 = tiles["t2"][0]; t2k = "t2_0"
                pr = ps[2 + j % 2]; prk = "ps%d" % (2 + j % 2)
                B.cp("act", tb[:, :n], pb[:, :n], [pk], [tbk])
                B.mm(pr[:, :n], self.rmatb[:, :], tb[:, :n], True, True, ["rmatb", tbk], [prk])
                lo = t0 - NCTX
                if rm in (3, 4, 5):
                    if rm >= 4:
                        B.tt("dve", t1[:, :n], pb[:, :n], self.cosb[:, lo:lo + n], ALU.mult, [pk, "cosb", tbk], [t1k])
                    if rm >= 5:
                        B.tt("dve", t2[:, :n], pr[:, :n], self.sinb[:, lo:lo + n], ALU.mult, [prk, "sinb"], [t2k])
                    B.cp("dve", dst_fn(t0, n), pb[:, :n], [pk, prk], [dkey_fn(j)])
                    continue
                B.tt("dve", t1[:, :n], pb[:, :n], self.cosb[:, lo:lo + n], ALU.mult, [pk, "cosb", tbk], [t1k])
                B.tt("dve", t2[:, :n], pr[:, :n], self.sinb[:, lo:lo + n], ALU.mult, [prk, "sinb"], [t2k])
                B.tt("pool" if rm == 2 else "dve", dst_fn(t0, n), t1[:, :n], t2[:, :n], ALU.add, [t1k, t2k], [dkey_fn(j)])

    def attention(self, l, h):
        B = self
        at = self.at; ps = self.ps; hT = self.hT; big = self.big
        w_in = self.W("w_in")
        i2 = h % 2
        wk, wq, wv = at["wa"][i2], at["wb"][i2], at["wc"][i2]
        wkk, wqk, wvk = "wa%d" % i2, "wb%d" % i2, "wc%d" % i2
        for wt, key, off in ((wk, wkk, 0), (wq, wqk, 2560), (wv, wvk, 1024)):
            B.dma(WQ, wt[:, :, :], w_in[l, :, off + h * 128:off + (h + 1) * 128].rearrange("(c p) n -> p c n", p=128), (), [key])
        kT, qT, Vh = at["kT"], at["qT"], at["Vh"]
        self.proj_fm(wk, wkk, 8, hT, self.hk, lambda t0, n: kT[:, t0:t0 + n], lambda j: "kT_%d" % j, True, at)
        self.proj_fm(wq, wqk, 8, hT, self.hk, lambda t0, n: qT[:, t0:t0 + n], lambda j: "qT_%d" % j, True, at)
        if self.att_stage < 1:
            return
        if "qk" in self.dbg and l == 0 and h == 0:
            self.dump("d_kT", kT[:, :], [128, NT], ["kT_%d" % j for j in range(5)], BF16)
            self.dump("d_qT", qT[:, :], [128, NT], ["qT_%d" % j for j in range(5)], BF16)
        for t4 in range(5):
            tiles = list(range(t4 * 4, min(18, t4 * 4 + 4)))
            pb = ps[t4 % 2]; pk = "ps%d" % (t4 % 2)
            for ii, ti in enumerate(tiles):
                j = self.tj(ti)
                for kc in range(8):
                    B.mm(pb[:, ii * 128:(ii + 1) * 128], hT[:, kc, ti * 128:(ti + 1) * 128], wv[:, kc, :], kc == 0, kc == 7,
                         [wvk, self.hk(kc, j)], [pk])
            nt_ = len(tiles)
            B.cp("act", XAP(Vh, tiles[0] * 129, [[18 * 129, 128], [129, nt_], [1, 128]]),
                 pb[:, 0:nt_ * 128].rearrange("p (a b) -> p a b", b=128), [pk], ["Vh"])
        if self.att_stage < 2:
            return
        blocks = [(0, 0, 256, 2)] + [(1 + i, 256 + 512 * i, 512, 18) for i in range(4)]
        if self.last:
            blocks = blocks[1:]
        cnt = 0
        pending_epi = None
        for bi, (qi, q0, qn, nk) in enumerate(blocks):
            nsub = qn // 128
            nb = (nsub + 1) // 2
            ob = (bi % 2) * 4
            for m in range(2):
                for bb in range(nb):
                    bk_ = 4 + 2 * m + bb
                    B.mm(ps[bk_][:, :], self.zerob[:, 0:128], self.zerob[:, :], True, True, ["zerob"], ["ps%d" % bk_])
            for m in range(2):
                queue = []
                for kt in range(nk + 2):
                    if kt < nk:
                        sbk = cnt % 3
                        pt = at["pt"][cnt % 4]; ptk = "pt%d" % (cnt % 4)
                        cnt += 1
                        jk = self.tj(kt)
                        B.mm(ps[sbk][:, :qn], kT[64 * m:64 * m + 64, kt * 128:(kt + 1) * 128], qT[64 * m:64 * m + 64, q0:q0 + qn], True, True,
                             ["kT_%d" % jk, "qT_%d" % qi], ["ps%d" % sbk])
                        B.act(pt[:, :qn], ps[sbk][:, :qn], AF.Exp, ["ps%d" % sbk], [ptk], scale=0.125)
                        queue.append((pt, ptk, kt))
                    if queue and (len(queue) > 2 or kt >= nk):
                        ppt, pptk, pkt = queue.pop(0)
                        for sub in range(nsub):
                            bk_ = 4 + 2 * m + sub // 2
                            col = (sub % 2) * 129
                            B.mm(ps[bk_][:, col:col + 129], ppt[:, sub * 128:(sub + 1) * 128], Vh[:, pkt, :], False, pkt == nk - 1,
                                 [pptk, "Vh"], ["ps%d" % bk_], skip_group_check=True)
            osb = at["osb"]
            for m in range(2):
                for bb in range(nb):
                    bk_ = 4 + 2 * m + bb
                    B.cp("dve", osb[ob + bk_ - 4][:, :], ps[bk_][:, 0:258], ["ps%d" % bk_], ["osb%d" % (ob + bk_ - 4)])
            if pending_epi is not None:
                self.attn_epilogue(h, *pending_epi)
            pending_epi = (q0, nsub, ob)
        if pending_epi is not None:
            self.attn_epilogue(h, *pending_epi)

    def attn_epilogue(self, h, q0, nsub, ob):
        B = self
        at = self.at; ps = self.ps; big = self.big
        if self.att_stage < 3:
            return
        for sub in range(nsub):
            b0 = ob + sub // 2; b1 = ob + 2 + sub // 2
            col = (sub % 2) * 129
            sm = at["sm"][sub % 4]; smk = "sm%d" % (sub % 4)
            of = at["of"][sub % 2]; ofk = "of%d" % (sub % 2)
            ot = at["ot"][sub % 2]; otk = "ot%d" % (sub % 2)
            onb = at["onb"][sub % 2]; onk = "onb%d" % (sub % 2)
            o0, o1 = at["osb"][b0], at["osb"][b1]
            B.recip(sm[:, 0:1], o0[:, col + 128:col + 129], ["osb%d" % b0], [smk])
            B.recip(sm[:, 1:2], o1[:, col + 128:col + 129], ["osb%d" % b1], [smk])
            B.tt("dve", sm[:, 2:3], sm[:, 1:2], at["nlam"][:, :], ALU.mult, [smk, "nlam"], [smk])
            B.ts("dve", ot[:, :], o1[:, col:col + 128], sm[:, 2:3], None, ALU.mult, None, ["osb%d" % b1, smk], [otk])
            B.stt(of[:, :], o0[:, col:col + 128], sm[:, 0:1], ot[:, :], ALU.mult, ALU.add, ["osb%d" % b0, smk, otk], [ofk])
            B.tt("dve", ot[:, :], of[:, :], of[:, :], ALU.mult, [ofk], [otk])
            B.red(sm[:, 3:4], ot[:, :], ALU.add, [otk], [smk])
            B.act(sm[:, 4:5], sm[:, 3:4], AF.Ln, [smk], [smk], scale=1.0 / 128.0, bias=self.epsc[:, :])
            B.act(sm[:, 5:6], sm[:, 4:5], AF.Exp, [smk], [smk], scale=-0.5)
            B.stt(onb[:, :], of[:, :], sm[:, 5:6], at["g128"][:, :], ALU.mult, ALU.mult, [ofk, smk, "g128"], [onk])
            pbf = ps[3][:, (sub % 2) * 64:(sub % 2) * 64 + 64].bitcast(BF16)
            B.tr(pbf, onb[:, :], self.identb[:, :], [onk, "identb"], ["ps3"])
            tq = q0 + sub * 128
            jq = 0 if tq < 256 else 1 + (tq - 256) // 512
            B.cp("dve", big[:, h, tq:tq + 128], pbf, ["ps3"], [self.bk(h, jq)])

    def merge(self, l, kind):
        B = self
        ps = self.ps; hT = self.hT; xT = self.xT
        w_in = self.W("w_in"); w_o = self.W("w_o")
        if kind == "attn":
            goff = 3584; wp = self.W("w_pa"); nkc = 8; src = self.big; srck = self.bk
        else:
            goff = 4608; wp = self.W("w_ps"); nkc = 4; src = self.ssmT; srck = lambda c, j: "ss%d_%d" % (c, j)
        wa = [B.sb("mwa%d" % i, [128, 8, 128], BF16) for i in range(2)]
        wb = [B.sb("mwb%d" % i, [128, 8, 128], BF16) for i in range(2)]
        wo = [B.sb("mwo%d" % i, [128, 1024], BF16) for i in range(2)]
        mC = [B.sb("mC%d" % i, [128, NT], BF16) for i in range(2)]
        sg = [B.sb("msg%d" % i, [128, 512]) for i in range(2)]
        ycnt = 0
        for c in range(8):
            i2 = c % 2
            B.dma(WQ, wa[i2][:, :, :], w_in[l, :, goff + c * 128:goff + (c + 1) * 128].rearrange("(c p) n -> p c n", p=128), (), ["mwa%d" % i2])
            B.dma(WQ, wb[i2][:, 0:nkc, :], wp[l, :, c * 128:(c + 1) * 128].rearrange("(c p) n -> p c n", p=128), (), ["mwb%d" % i2])
            B.dma(WQ, wo[i2][:, :], w_o[l, c * 128:(c + 1) * 128, :], (), ["mwo%d" % i2])
            for j, (t0, n) in enumerate(TCH):
                if self.last and j == 0:
                    continue
                pg = ps[j % 2]; pgk = "ps%d" % (j % 2)
                pp = ps[2 + j % 2]; ppk = "ps%d" % (2 + j % 2)
                for kc in range(8):
                    B.mm(pg[:, :n], wa[i2][:, kc, :], hT[:, kc, t0:t0 + n], kc == 0, kc == 7, ["mwa%d" % i2, self.hk(kc, j)], [pgk])
                for kc in range(nkc):
                    B.mm(pp[:, :n], wb[i2][:, kc, :], src[:, kc, t0:t0 + n], kc == 0, kc == nkc - 1, ["mwb%d" % i2, srck(kc, j)], [ppk])
                B.act(sg[j % 2][:, :n], pg[:, :n], AF.Sigmoid, [pgk], ["msg%d" % (j % 2)])
                B.tt("dve", mC[i2][:, t0:t0 + n], sg[j % 2][:, :n], pp[:, :n], ALU.mult, ["msg%d" % (j % 2), ppk], ["mC%d_%d" % (i2, j)])
            for o in range(8):
                for j, (t0, n) in enumerate(TCH):
                    if self.last and j == 0:
                        continue
                    py = ps[4 + ycnt % 4]; pyk = "ps%d" % (4 + ycnt % 4)
                    ycnt += 1
                    B.mm(py[:, :n], wo[i2][:, o * 128:(o + 1) * 128], mC[i2][:, t0:t0 + n], True, True, ["mwo%d" % i2, "mC%d_%d" % (i2, j)], [pyk])
                    m_, k_ = (self.cmodx, "cmodx") if j == 0 else (self.modx, "modx")
                    B.stt(xT[:, o, t0:t0 + n], py[:, :n], m_[:, 16 + o:17 + o], xT[:, o, t0:t0 + n], ALU.mult, ALU.add,
                          [pyk, k_, self.xk(o, j)], [self.xk(o, j)])

    def layernorm(self, l, which):
        B = self
        ps = self.ps; xT = self.xT
        g_d = self.W("ln%d_g" % which); b_d = self.W("ln%d_b" % which)
        lng = B.sb("lng", [128, 8]); lnb = B.sb("lnb", [128, 8])
        sq = [B.sb("lnsq%d" % i, [128, 512]) for i in range(2)]
        ta = [B.sb("lnta%d" % i, [128, 512]) for i in range(2)]
        tv = [B.sb("lntv%d" % i, [128, 512]) for i in range(2)]
        self.vecload(lng[:, :], g_d[l, :], "lng"); self.vecload(lnb[:, :], b_d[l, :], "lnb")
        for j, (t0, n) in enumerate(TCH):
            if self.last and j == 0:
                continue
            a = (j % 2) * 2
            p0 = ps[a]; p0k = "ps%d" % a; p1 = ps[a + 1]; p1k = "ps%d" % (a + 1)
            i2 = j % 2
            for c in range(8):
                B.mm(p0[:, :n], self.onesf[:, :], xT[:, c, t0:t0 + n], c == 0, c == 7, ["onesf", self.xk(c, j)], [p0k])
            for c in range(8):
                s_ = sq[c % 2]; sk_ = "lnsq%d" % (c % 2)
                B.act(s_[:, :n], xT[:, c, t0:t0 + n], AF.Square, [self.xk(c, j)], [sk_])
                B.mm(p1[:, :n], self.onesf[:, :], s_[:, :n], c == 0, c == 7, ["onesf", sk_], [p1k])
            tak = "lnta%d" % i2; tvk = "lntv%d" % i2
            B.act(ta[i2][:, :n], p0[:, :n], AF.Square, [p0k], [tak])
            B.tt("dve", tv[i2][:, :n], p1[:, :n], ta[i2][:, :n], ALU.subtract, [p1k, tak], [tvk])
            B.act(tv[i2][:, :n], tv[i2][:, :n], AF.Ln, [tvk], [tvk], bias=self.epsc[:, :])
            B.act(tv[i2][:, :n], tv[i2][:, :n], AF.Exp, [tvk], [tvk], scale=-0.5)
            B.stt(ta[i2][:, :n], p0[:, :n], -1.0, tv[i2][:, :n], ALU.mult, ALU.mult, [p0k, tvk], [tak])
            for c in range(8):
                xs = xT[:, c, t0:t0 + n]
                B.tt("dve", xs, xs, tv[i2][:, :n], ALU.mult, [self.xk(c, j), tvk], [self.xk(c, j)])
                B.tt("pool", xs, xs, ta[i2][:, :n], ALU.add, [self.xk(c, j), tak], [self.xk(c, j)])
                B.act(xs, xs, AF.Identity, [self.xk(c, j), "lng", "lnb"], [self.xk(c, j)], scale=lng[:, c:c + 1], bias=lnb[:, c:c + 1])

    def router(self, l):
        B = self
        ps = self.ps; xT = self.xT
        self.phase()
        self.WT = WT = B.sb("WT", [32, NT])
        wr = B.sb("wr", [128, 8, 36]); rb = B.sb("rb", [128, 36])
        h2 = [B.sb("h2_%d" % i, [128, 8, 128]) for i in range(2)]
        lg = [B.sb("lg%d" % i, [128, 36]) for i in range(2)]
        me = [B.sb("me%d" % i, [128, 32]) for i in range(2)]
        rs = [B.sb("rs%d" % i, [128, 48]) for i in range(2)]
        wt_ = [B.sb("rwt%d" % i, [128, 32]) for i in range(2)]
        mk = [B.sb("rmk%d" % i, [128, 32]) for i in range(2)]
        rgw = self.W("router_g_w"); rgb = self.W("router_g_b"); rew = self.W("router_e_w"); reb = self.W("router_e_b")
        B.dmas("sp", wr[:, :, 0:4], rgw[l, :, :].rearrange("(c p) n -> p c n", p=128), (), ["wr"])
        B.dmas("sp", wr[:, :, 4:36], rew[l, :, :].rearrange("(c p) n -> p c n", p=128), (), ["wr"])
        self.bcast_load(rb[:, 0:4], rgb[l:l + 1, :], 4, "rb")
        self.bcast_load(rb[:, 4:36], reb[l:l + 1, :], 32, "rb")
        for ti in range(18):
            if self.last and ti < 2:
                continue
            i2 = ti % 2
            j = self.tj(ti)
            m_, k_ = (self.cmodx, "cmodx") if ti < 2 else (self.modx, "modx")
            hk_ = "h2_%d" % i2; lk = "lg%d" % i2; mek = "me%d" % i2; rk = "rs%d" % i2; wk_ = "rwt%d" % i2; mkk = "rmk%d" % i2
            for c in range(8):
                B.act(h2[i2][:, c, :], xT[:, c, ti * 128:(ti + 1) * 128], AF.Identity, [self.xk(c, j), k_], [hk_],
                      scale=m_[:, 32 + c:33 + c], bias=m_[:, 24 + c:25 + c])
            for c in range(8):
                B.mm(ps[i2][:, 0:36], h2[i2][:, c, :], wr[:, c, :], c == 0, c == 7, [hk_, "wr"], ["ps%d" % i2])
            L_ = lg[i2]; R_ = rs[i2]; M_ = me[i2]
            B.tt("dve", L_[:, :], ps[i2][:, 0:36], rb[:, :], ALU.add, ["ps%d" % i2, "rb"], [lk])
            B.red(R_[:, 0:1], L_[:, 0:4], ALU.max, [lk], [rk])
            B.ts("dve", R_[:, 1:2], R_[:, 0:1], -1.0, None, ALU.mult, None, [rk], [rk])
            B.ts("dve", R_[:, 8:12], L_[:, 0:4], R_[:, 0:1], None, ALU.is_equal, None, [lk, rk], [rk])
            B.act(R_[:, 12:16], L_[:, 0:4], AF.Exp, [lk, rk], [rk], bias=R_[:, 1:2])
            B.red(R_[:, 2:3], R_[:, 12:16], ALU.add, [rk], [rk])
            B.recip(R_[:, 3:4], R_[:, 2:3], [rk], [rk])
            B.ts("dve", R_[:, 16:20], R_[:, 8:12], -1.0, 1.0e30, ALU.add, ALU.mult, [rk], [rk])
            B.tt("dve", M_[:, :].rearrange("p (a b) -> p a b", b=8), L_[:, 4:36].rearrange("p (a b) -> p a b", b=8),
                 XAP(R_, 16, [[48, 128], [1, 4], [0, 8]]), ALU.add, [lk, rk], [mek])
            B.P.op("dve", lambda e, R_=R_, M_=M_: e.max(out=R_[:, 24:32], in_=M_[:, :]), [mek], [rk])
            B.ts("dve", mk[i2][:, :], M_[:, :], R_[:, 24:25], None, ALU.is_equal, None, [mek, rk], [mkk])
            B.tt("dve", R_[:, 4:5], R_[:, 25:26], R_[:, 24:25], ALU.subtract, [rk], [rk])
            B.act(R_[:, 5:6], R_[:, 4:5], AF.Exp, [rk], [rk])
            B.ts("dve", R_[:, 6:7], R_[:, 5:6], 1.0, None, ALU.add, None, [rk], [rk])
            B.recip(R_[:, 6:7], R_[:, 6:7], [rk], [rk])
            B.tt("dve", R_[:, 7:8], R_[:, 5:6], R_[:, 6:7], ALU.mult, [rk], [rk])
            B.tt("dve", R_[:, 32:33], R_[:, 6:7], R_[:, 3:4], ALU.mult, [rk], [rk])
            B.tt("dve", R_[:, 33:34], R_[:, 7:8], R_[:, 3:4], ALU.mult, [rk], [rk])
            B.ts("dve", wt_[i2][:, :], mk[i2][:, :], R_[:, 32:33], None, ALU.mult, None, [mkk, rk], [wk_])
            B.ts("dve", mk[i2][:, :], M_[:, :], R_[:, 25:26], None, ALU.is_equal, None, [mek, rk, wk_], [mkk])
            B.stt(wt_[i2][:, :], mk[i2][:, :], R_[:, 33:34], wt_[i2][:, :], ALU.mult, ALU.add, [mkk, rk, wk_], [wk_])
            B.tr(ps[2 + i2][0:32, 0:128], wt_[i2][:, :], self.identf[:, :], [wk_, "identf"], ["ps%d" % (2 + i2)])
            B.cp("act", WT[:, ti * 128:(ti + 1) * 128], ps[2 + i2][0:32, 0:128], ["ps%d" % (2 + i2)], ["WT_%d" % j])
        if "rt" in self.dbg and l == 0:
            self.dump("d_WT", WT[:, :], [32, NT], ["WT_%d" % j for j in range(5)])

    def moe(self, l):
        B = self
        ps = self.ps; xT = self.xT; hT = self.hT; WT = self.WT
        mw1 = self.W("moe_w1"); mw3 = self.W("moe_w3"); mw2 = self.W("moe_w2")
        self.sb_ptr = self.ph_base + 32 * 0 + NT * 4
        w1 = [B.sb("w1_%d" % i, [128, 8, 512], BF16) for i in range(2)]
        w3 = [B.sb("w3_%d" % i, [128, 8, 512], BF16) for i in range(2)]
        w2 = [B.sb("w2_%d" % i, [128, 4, 1024], BF16) for i in range(2)]
        gb = [B.sb("gb%d" % i, [128, 4, 512], BF16) for i in range(2)]
        s1 = [B.sb("s1_%d" % i, [128, 512]) for i in range(2)]
        s2 = [B.sb("s2_%d" % i, [128, 512]) for i in range(2)]
        self.P.fence()
        cnt = 0
        for e in range(32):
            i2 = e % 2
            B.dma(WQ, w1[i2][:, :, :], mw1[l, e, :, :].rearrange("(c p) n -> p c n", p=128), (), ["w1_%d" % i2])
            B.dma(WQ, w3[i2][:, :, :], mw3[l, e, :, :].rearrange("(c p) n -> p c n", p=128), (), ["w3_%d" % i2])
            B.dma(WQ, w2[i2][:, :, :], mw2[l, e, :, :].rearrange("(c p) n -> p c n", p=128), (), ["w2_%d" % i2])
            selT = XAP(self.identf, e, [[128, 32], [0, 128]])
            for j, (t0, n) in enumerate(TCH):
                if self.last and j == 0:
                    continue
                pw = ps[j % 2]; pwk = "ps%d" % (j % 2)
                B.mm(pw[:, :n], selT, WT[:, t0:t0 + n], True, True, ["identf", "WT_%d" % j], [pwk])
                g_ = gb[j % 2]
                for hc in range(4):
                    p1 = ps[2 + hc % 2]; p1k = "ps%d" % (2 + hc % 2)
                    p3 = ps[4 + hc % 2]; p3k = "ps%d" % (4 + hc % 2)
                    for kc in range(8):
                        B.mm(p1[:, :n], w1[i2][:, kc, hc * 128:(hc + 1) * 128], hT[:, kc, t0:t0 + n], kc == 0, kc == 7,
                             ["w1_%d" % i2, self.hk(kc, j)], [p1k])
                    for kc in range(8):
                        B.mm(p3[:, :n], w3[i2][:, kc, hc * 128:(hc + 1) * 128], hT[:, kc, t0:t0 + n], kc == 0, kc == 7,
                             ["w3_%d" % i2, self.hk(kc, j)], [p3k])
                    B.act(s1[hc % 2][:, :n], p1[:, :n], AF.Silu, [p1k], ["s1_%d" % (hc % 2)])
                    B.tt("dve", s2[hc % 2][:, :n], s1[hc % 2][:, :n], p3[:, :n], ALU.mult, ["s1_%d" % (hc % 2), p3k], ["s2_%d" % (hc % 2)])
                    B.tt("dve", g_[:, hc, :n], s2[hc % 2][:, :n], pw[:, :n], ALU.mult, ["s2_%d" % (hc % 2), pwk], ["gb%d_%d" % (j % 2, hc)])
                for o in range(8):
                    py = ps[6 + cnt % 2]; pyk = "ps%d" % (6 + cnt % 2)
                    cnt += 1
                    for hc in range(4):
                        B.mm(py[:, :n], w2[i2][:, hc, o * 128:(o + 1) * 128], g_[:, hc, :n], hc == 0, hc == 3,
                             ["w2_%d" % i2, "gb%d_%d" % (j % 2, hc)], [pyk])
                    m_, k_ = (self.cmodx, "cmodx") if j == 0 else (self.modx, "modx")
                    B.stt(xT[:, o, t0:t0 + n], py[:, :n], m_[:, 40 + o:41 + o], xT[:, o, t0:t0 + n], ALU.mult, ALU.add,
                          [pyk, k_, self.xk(o, j)], [self.xk(o, j)])

    def ssm(self, l):
        B = self
        ps = self.ps; hT = self.hT
        NLV = 12
        self.phase()
        self.ssmT = ssmT = B.sb("ssmT", [128, 4, NT], BF16)
        uT = B.sb("uT", [128, 4, NT], BF16)
        ARn = [B.sb("ARn%d" % d, [128, NLV, 32]) for d in range(2)]
        OFn = [B.sb("OFn%d" % d, [128, NLV, 32]) for d in range(2)]
        BT = [B.sb("BT%d" % d, [128, 4, 128], BF16) for d in range(2)]
        Cst = [B.sb("Cst%d" % d, [128, 512], BF16) for d in range(2)]
        dsk = B.sb("dsk", [128, 4]); bglu = B.sb("bglu", [128, 4])
        main_base = self.sb_ptr
        uk = lambda c, j: "uT%d_%d" % (c, j)
        w_in = self.W("w_in")
        self.vecload(dsk[:, :], self.W("ssm_d")[l, :], "dsk")
        self.vecload(bglu[:, :], self.W("b_glu")[l, :], "bglu")
        wu = [B.sb("wu%d" % i, [128, 8, 128], BF16) for i in range(2)]
        for oc in range(4):
            i2 = oc % 2
            B.dma(WQ, wu[i2][:, :, :], w_in[l, :, 2048 + oc * 128:2048 + (oc + 1) * 128].rearrange("(c p) n -> p c n", p=128), (), ["wu%d" % i2])
            self.proj_fm(wu[i2], "wu%d" % i2, 8, hT, self.hk, lambda t0, n, oc=oc: uT[:, oc, t0:t0 + n], lambda j, oc=oc: uk(oc, j))
        if "u" in self.dbg and l == 0:
            self.dump("d_uT", uT[:, :, :], [128, 4, NT], [uk(c, j) for c in range(4) for j in range(5)], BF16)
        a_re = self.W("ssm_a_re"); a_im = self.W("ssm_a_im"); ldt = self.W("ssm_log_dt")
        b_re = self.W("ssm_b_re"); b_im = self.W("ssm_b_im"); c_re = self.W("ssm_c_re"); c_im = self.W("ssm_c_im")
        anat = B.sb("anat", [32, 128])
        are2 = B.sb("are2", [128, 32]); aim2 = B.sb("aim2", [128, 32]); dt2 = B.sb("dt2", [128, 32])
        zre = B.sb("zre", [128, 32]); zim = B.sb("zim", [128, 32])
        NV = NLV * 32
        zn = B.sb("zn", [128, NLV, 32]); yy = B.sb("yy", [128, NLV, 32]); yi = B.sb("yi", [128, NLV, 32], I32)
        yf = B.sb("yf", [128, NLV, 32]); ff = B.sb("ff", [128, NLV, 32]); sv = B.sb("sv", [128, NLV, 32]); cvv = B.sb("cvv", [128, NLV, 32])
        AIn = B.sb("AIn", [128, NLV, 32])
        cf = B.sb("cf", [128, 12, 32])
        X1 = B.sb("X1", [128, 32, 16]); X2 = B.sb("X2", [128, 32, 16]); Bst = B.sb("Bst", [128, 512]); Btmp = B.sb("Btmp", [128, 512])
        Cnat = B.sb("Cnat", [128, 4, 128])
        fl = lambda t: t[:, :, :].rearrange("p a b -> p (a b)")
        for d in range(2):
            kd = "_%d" % d
            for src_, dst_, kk in ((a_re, are2, "are2"), (a_im, aim2, "aim2")):
                for half in range(2):
                    B.dma("sp", anat[:, half * 64:(half + 1) * 64], src_[l, d, :, :], (), ["anat"])
                B.tr(ps[4][:, 0:32], anat[:, :], self.identf[0:32, 0:32], ["anat", "identf"], ["ps4"])
                B.cp("dve", dst_[:, :], ps[4][:, 0:32], ["ps4"], [kk])
            self.bcast_load(dt2[:, :], ldt[l, d:d + 1, :], 32, "dt2")
            B.act(dt2[:, :], dt2[:, :], AF.Exp, ["dt2"], ["dt2"])
            B.tt("dve", zre[:, :], are2[:, :], dt2[:, :], ALU.mult, ["are2", "dt2"], ["zre"])
            B.tt("dve", zim[:, :], aim2[:, :], dt2[:, :], ALU.mult, ["aim2", "dt2"], ["zim"])
            for i in range(NLV):
                n_ = float(2 ** i)
                B.ts("dve", zn[:, i, :], zre[:, :], n_, None, ALU.mult, None, ["zre"], ["zn"])
                B.ts("dve", yy[:, i, :], zim[:, :], n_ / (2.0 * math.pi), 8.5, ALU.mult, ALU.add, ["zim"], ["yy"])
            B.act(fl(zn), fl(zn), AF.Exp, ["zn"], ["zn"])
            for which, dst in ((0, sv), (1, cvv)):
                if which == 1:
                    B.ts("dve", fl(yy), fl(yy), 0.25, None, ALU.add, None, ["yy"], ["yy"])
                B.cp("dve", fl(yi), fl(yy), ["yy"], ["yi"])
                B.cp("dve", fl(yf), fl(yi), ["yi"], ["yf"])
                B.tt("dve", fl(ff), fl(yy), fl(yf), ALU.subtract, ["yy", "yf"], ["ff"])
                B.stt(fl(ff), fl(ff), 0.0, fl(ff), ALU.is_lt, ALU.add, ["ff"], ["ff"])
                B.ts("dve", fl(ff), fl(ff), 1.0, None, ALU.min, None, ["ff"], ["ff"])
                B.act(fl(dst), fl(ff), AF.Sin, ["ff", "negpi"], ["sc%d" % which], scale=2.0 * math.pi, bias=self.negpi[:, :])
            B.tt("dve", fl(ARn[d]), fl(zn), fl(cvv), ALU.mult, ["zn", "sc1"], ["ARn" + kd])
            B.tt("dve", fl(AIn), fl(zn), fl(sv), ALU.mult, ["zn", "sc0"], ["AIn"])
            B.ts("dve", fl(OFn[d]), fl(AIn), self.sgnf[:, 0:1], None, ALU.mult, None, ["AIn", "sgnf"], ["OFn" + kd])
            a1r = ARn[d][:, 0, :]; a1i = AIn[:, 0, :]
            c_ = lambda i: cf[:, i, :]
            B.ts("dve", c_(0), a1r, -1.0, None, ALU.add, None, ["ARn" + kd], ["cf"])
            B.tt("dve", c_(1), c_(0), are2[:, :], ALU.mult, ["cf", "are2"], ["cf"])
            B.tt("dve", c_(2), a1i, aim2[:, :], ALU.mult, ["AIn", "aim2"], ["cf"])
            B.tt("dve", c_(1), c_(1), c_(2), ALU.add, ["cf"], ["cf"])
            B.tt("dve", c_(3), a1i, are2[:, :], ALU.mult, ["AIn", "are2"], ["cf"])
            B.tt("dve", c_(4), c_(0), aim2[:, :], ALU.mult, ["cf", "aim2"], ["cf"])
            B.tt("dve", c_(3), c_(3), c_(4), ALU.subtract, ["cf"], ["cf"])
            B.tt("dve", c_(5), are2[:, :], are2[:, :], ALU.mult, ["are2"], ["cf"])
            B.tt("dve", c_(6), aim2[:, :], aim2[:, :], ALU.mult, ["aim2"], ["cf"])
            B.tt("dve", c_(5), c_(5), c_(6), ALU.add, ["cf"], ["cf"])
            B.recip(c_(5), c_(5), ["cf"], ["cf"])
            B.tt("dve", c_(7), c_(1), c_(5), ALU.mult, ["cf"], ["cf"])
            B.tt("dve", c_(8), c_(3), c_(5), ALU.mult, ["cf"], ["cf"])
            B.ts("dve", c_(8), c_(8), self.sgnf[:, 1:2], None, ALU.mult, None, ["cf", "sgnf"], ["cf"])
            for q4 in range(4):
                gs = slice(q4 * 8, (q4 + 1) * 8)
                B.dmas("sp", X1[0:64, gs, :], b_re[l, d, gs, :, :].rearrange("g p c -> p g c"), (), ["X1"])
                B.dmas("sp", X1[64:128, gs, :], b_im[l, d, gs, :, :].rearrange("g p c -> p g c"), (), ["X1"])
                B.dmas("sp", X2[0:64, gs, :], b_im[l, d, gs, :, :].rearrange("g p c -> p g c"), (), ["X2"])
                B.dmas("sp", X2[64:128, gs, :], b_re[l, d, gs, :, :].rearrange("g p c -> p g c"), (), ["X2"])
            crb = XAP(cf, 7 * 32, [[12 * 32, 128], [1, 32], [0, 16]])
            cib = XAP(cf, 8 * 32, [[12 * 32, 128], [1, 32], [0, 16]])
            v3 = lambda t: t[:, :].rearrange("p (g c) -> p g c", c=16)
            B.tt("dve", v3(Bst), X1[:, :, :], crb, ALU.mult, ["X1", "cf"], ["Bst"])
            B.tt("dve", v3(Btmp), X2[:, :, :], cib, ALU.mult, ["X2", "cf"], ["Btmp"])
            B.tt("dve", Bst[:, :], Bst[:, :], Btmp[:, :], ALU.add, ["Bst", "Btmp"], ["Bst"])
            for k in range(4):
                B.tr(ps[k % 2][:, 0:128], Bst[:, k * 128:(k + 1) * 128], self.identf[:, :], ["Bst", "identf"], ["ps%d" % (k % 2)])
                B.cp("act", BT[d][:, k, :], ps[k % 2][:, 0:128], ["ps%d" % (k % 2)], ["BT" + kd])
            B.dma("sp", Cnat[:, :, 0:64], c_re[l, d, :, :, :].rearrange("(k a) c p -> (a c) k p", k=4), (), ["Cnat"])
            B.dma("sp", Cnat[:, :, 64:128], c_im[l, d, :, :, :].rearrange("(k a) c p -> (a c) k p", k=4), (), ["Cnat"])
            for k in range(4):
                B.tr(ps[2 + k % 2][:, 0:128], Cnat[:, k, :], self.identf[:, :], ["Cnat", "identf"], ["ps%d" % (2 + k % 2)])
                B.cp("dve", Cst[d][0:64, k * 128:(k + 1) * 128], ps[2 + k % 2][0:64, 0:128], ["ps%d" % (2 + k % 2)], ["Cst" + kd])
                B.act(Cst[d][64:128, k * 128:(k + 1) * 128], ps[2 + k % 2][64:128, 0:128], AF.Copy, ["ps%d" % (2 + k % 2)], ["Cst" + kd], scale=-1.0)
        if "coef" in self.dbg and l == 0:
            self.dump("d_ARn0", ARn[0][:, :, :], [128, NLV, 32], ["ARn_0"]); self.dump("d_OFn0", OFn[0][:, :, :], [128, NLV, 32], ["OFn_0"])
            self.dump("d_Cst0", Cst[0][:, :], [128, 512], ["Cst_0"]); self.dump("d_BT0", BT[0][:, :, :], [128, 4, 128], ["BT_0"], BF16)
        self.P.fence()
        self.sb_ptr = main_base
        NG = 4
        Hb = [B.sb("Hb%d" % s, [128, NT], BF16) for s in range(NG)]
        yacc = B.sb("yacc", [128, NT])
        Cpad = [B.sb("Cpad%d" % d, [128, 8, 128], BF16) for d in range(2)]
        Dm = [[B.sb("Dm%d_%d" % (s, i), [128, 128], BF16) for i in range(2)] for s in range(NG)]
        um = [[B.sb("um%d_%d" % (s, i), [128, 512], BF16) for i in range(1)] for s in range(NG)]
        gt = [B.sb("gt%d" % i, [128, 256]) for i in range(2)]
        for d in range(2):
            B.memset("pool", Cpad[d][:, :, :], 0.0, ["Cpad_%d" % d])
        CH = [(c * 512, min(512, NT - c * 512)) for c in range(5)]

        def pos(d, t0):
            if d == 0:
                return t0
            return t0 - NCTX if t0 >= NCTX else NLAT + t0

        def hkeys(s, a, b):
            return ["H%d_%d" % (s, c) for c in range(a // 512, (b - 1) // 512 + 1)]

        def job(s, k, g8, d):
            g = k * 8 + g8
            H = Hb[s]
            pA = ps[2 * s]; pAk = "ps%d" % (2 * s); pB = ps[2 * s + 1]; pBk = "ps%d" % (2 * s + 1)
            banks = [(pA, pAk), (pB, pBk)]
            ecnt = [0]

            def evac(dst, src, rd, wr):
                ecnt[0] += 1
                B.cp("act" if ecnt[0] % 2 else "dve", dst, src, rd, wr)
            for j, (t0, n) in enumerate(TCH):
                u_ = um[s][0]; umk = "um%d_0" % s
                pb, pk = banks[j % 2]
                B.ts("pool", u_[:, :n], uT[:, k, t0:t0 + n], self.gmaskf[:, g8:g8 + 1], None, ALU.mult, None, [uk(k, j), "gmaskf"], [umk])
                B.mm(pb[:, :n], BT[d][:, k, :], u_[:, :n], True, True, ["BT_%d" % d, umk], [pk])
                p0 = pos(d, t0)
                evac(H[:, p0:p0 + n], pb[:, :n], [pk], hkeys(s, p0, p0 + n))
                yield
            for i in range(NLV):
                sh = 2 ** i
                dm = Dm[s][i % 2]; dmk = "Dm%d_%d" % (s, i % 2)
                B.ts("pool", dm[:, :], self.identf[:, :], ARn[d][:, i, g:g + 1], None, ALU.mult, None, ["identf", "ARn_%d" % d], [dmk])
                B.stt(dm[:, :], self.swpf[:, :], OFn[d][:, i, g:g + 1], dm[:, :], ALU.mult, ALU.add, ["swpf", "OFn_%d" % d, dmk], [dmk])
                order = list(range(4, -1, -1)) if d == 0 else list(range(5))
                for ci, c in enumerate(order):
                    lo, n = CH[c]; hi = lo + n
                    pb, pk = banks[ci % 2]
                    if d == 0:
                        a = max(lo, sh); b = hi
                        has = b > a
                        sa, sb_ = a - sh, b - sh
                    else:
                        a = lo; b = min(hi, NT - sh)
                        has = b > a
                        sa, sb_ = a + sh, b + sh
                    if ci % 2 == 0:
                        B.mm(pb[:, :n], self.identb[:, :], H[:, lo:hi], True, not has, ["identb"] + hkeys(s, lo, hi), [pk])
                        if has:
                            B.mm(pb[:, a - lo:b - lo], dm[:, :], H[:, sa:sb_], False, True, [dmk] + hkeys(s, sa, sb_), [pk], skip_group_check=True)
                        B.cp("act", H[:, lo:hi], pb[:, :n], [pk], hkeys(s, lo, hi))
                    elif has:
                        B.mm(pb[:, a - lo:b - lo], dm[:, :], H[:, sa:sb_], True, True, [dmk] + hkeys(s, sa, sb_), [pk])
                        B.tt("dve", H[:, a:b], H[:, a:b], pb[:, a - lo:b - lo], ALU.add, [pk] + hkeys(s, lo, hi), hkeys(s, lo, hi))
                    yield
            for j, (t0, n) in enumerate(TCH):
                if self.last and j == 0:
                    continue
                pb, pk = banks[j % 2]
                p0 = pos(d, t0)
                B.mm(pb[:, :n], Cpad[d][:, g8, :], H[:, p0:p0 + n], True, True, ["Cpad_%d" % d] + hkeys(s, p0, p0 + n), [pk])
                B.tt("dve", yacc[:, t0:t0 + n], yacc[:, t0:t0 + n], pb[:, :n], ALU.add, ["yacc_%d" % j, pk], ["yacc_%d" % j])
                yield

        for k in range(4):
            for d in range(2):
                B.cp("pool", XAP(Cpad[d], 0, [[1024, 128], [144, 8], [1, 16]]),
                     Cst[d][:, k * 128:(k + 1) * 128].rearrange("p (g c) -> p g c", c=16), ["Cst_%d" % d], ["Cpad_%d" % d])
            for j, (t0, n) in enumerate(TCH):
                if self.last and j == 0:
                    continue
                B.ts("dve", yacc[:, t0:t0 + n], uT[:, k, t0:t0 + n], dsk[:, k:k + 1], None, ALU.mult, None, [uk(k, j), "dsk"], ["yacc_%d" % j])
            jobs = [(g8, d) for g8 in range(8) for d in range(2)]
            for r in range(0, 16, NG):
                gens = [job(s, k, jobs[r + s][0], jobs[r + s][1]) for s in range(NG)]
                alive = list(gens)
                while alive:
                    nxt = []
                    for g_ in alive:
                        try:
                            next(g_)
                            nxt.append(g_)
                        except StopIteration:
                            pass
                    alive = nxt
            if "ssm" in self.dbg and l == 0:
                self.dump("d_yacc%d" % k, yacc[:, :], [128, NT], ["yacc_%d" % j for j in range(5)])
            for j, (t0_, n_) in enumerate(TCH):
                if self.last and j == 0:
                    continue
                for t0 in range(t0_, t0_ + n_, 256):
                    n = 256
                    ya = yacc[:, t0:t0 + n]
                    ga, gb_ = gt[0], gt[1]
                    gak, gbk = "gt0", "gt1"
                    B.act(ga[:, :n], ya, AF.Square, ["yacc_%d" % j], [gak])
                    B.ts("dve", ga[:, :n], ga[:, :n], 0.044715, 1.0, ALU.mult, ALU.add, [gak], [gak])
                    B.tt("pool", ga[:, :n], ga[:, :n], ya, ALU.mult, [gak, "yacc_%d" % j], [gak])
                    B.act(gb_[:, :n], ga[:, :n], AF.Sigmoid, [gak], [gbk], scale=1.5957691216057308)
                    B.tt("dve", uT[:, k, t0:t0 + n], ya, gb_[:, :n], ALU.mult, ["yacc_%d" % j, gbk], [uk(k, j)])
        self.P.fence()
        self.sb_ptr = main_base
        wg = B.sb("wglu", [128, 4, 512], BF16)
        sgl = [B.sb("sgl%d" % i, [128, 512]) for i in range(2)]
        B.dma(WQ, wg[:, :, :], self.W("w_glu")[l, :, :].rearrange("(c p) n -> p c n", p=128), (), ["wglu"])
        cnt = 0
        for oc in range(4):
            for j, (t0, n) in enumerate(TCH):
                if self.last and j == 0:
                    continue
                pb = ps[cnt % 4]; pk = "ps%d" % (cnt % 4)
                sg_ = sgl[cnt % 2]; sgk = "sgl%d" % (cnt % 2)
                cnt += 1
                for kc in range(4):
                    B.mm(pb[:, :n], wg[:, kc, oc * 128:(oc + 1) * 128], uT[:, kc, t0:t0 + n], kc == 0, kc == 3, ["wglu", uk(kc, j)], [pk])
                B.act(sg_[:, :n], pb[:, :n], AF.Sigmoid, [pk, "bglu"], [sgk], bias=bglu[:, oc:oc + 1])
                B.tt("dve", ssmT[:, oc, t0:t0 + n], uT[:, oc, t0:t0 + n], sg_[:, :n], ALU.mult, [uk(oc, j), sgk], ["ss%d_%d" % (oc, j)])
        if "glu" in self.dbg and l == 0:
            self.dump("d_ssmT", ssmT[:, :, :], [128, 4, NT], ["ss%d_%d" % (c, j) for c in range(4) for j in range(5)], BF16)
        self.phase(reserve=4 * NT * 2)


def _consts():
    ident = np.eye(128, dtype=np.float32)
    rmat = np.zeros((128, 128), np.float32)
    for m in range(128):
        d = m % 32
        if d < 16:
            rmat[m + 16, m] = -1.0
        else:
            rmat[m - 16, m] = 1.0
    t = np.arange(NLAT)
    row = (t // 64).astype(np.float32)
    col = (t % 64).astype(np.float32)
    inv = (1.0 / (np.float32(10000.0) ** (np.arange(0, 32, 2, dtype=np.float32) / np.float32(32.0)))).astype(np.float32)
    cos = np.zeros((128, NLAT), np.float32)
    sin = np.zeros((128, NLAT), np.float32)
    for p in range(128):
        d = p % 64
        axis = d // 32
        f = d % 16
        posv = row if axis == 0 else col
        ang = (posv * inv[f]).astype(np.float32)
        cos[p] = np.cos(ang)
        sin[p] = np.sin(ang)
    swp = np.zeros((128, 128), np.float32)
    for k in range(128):
        swp[k, (k + 64) % 128] = 1.0
    gmask = np.zeros((128, 8), np.float32)
    for p in range(128):
        gmask[p, p // 16] = 1.0
    sgn = np.ones((128, 2), np.float32)
    sgn[64:, 0] = -1.0
    sgn[:64, 1] = -1.0
    return dict(ident=ident, rmat=rmat, ropecos=cos, ropesin=sin, swp=swp, gmask=gmask, sgn=sgn)


_CACHE = {}


def _get_prog(n_layers, dbg, skip):
    key = (n_layers, tuple(dbg), tuple(skip))
    if key not in _CACHE:
        b = Builder(n_layers, dbg, skip)
        b.build()
        _CACHE[key] = b
    return _CACHE[key]


def _in_maps(inputs, names, cores):
    f = lambda a: np.ascontiguousarray(np.asarray(a, dtype=np.float32))
    skipk = ("x", "c", "ctx", "c_ctx", "lam_q1", "lam_k1", "lam_q2", "lam_k2")
    shared = {k: f(v) for k, v in inputs.items() if k not in skipk and k in names}
    if "lamv" in names:
        shared["lamv"] = np.ascontiguousarray(np.stack([f(inputs["lam_q1"]), f(inputs["lam_k1"]), f(inputs["lam_q2"]), f(inputs["lam_k2"])], axis=1))
    for k, v in _consts().items():
        if k in names:
            shared[k] = v
    maps = []
    for b in range(cores):
        m = dict(shared)
        m["x"] = f(inputs["x"][b])
        m["ctx"] = f(inputs["ctx"][b])
        m["cvec"] = np.ascontiguousarray(np.stack([f(inputs["c"][b]), f(inputs["c_ctx"])], axis=0))
        maps.append(m)
    return maps


def run(inputs, n_layers=DEPTH, dbg=(), skip=(), cores=8):
    b = _get_prog(n_layers, dbg, skip)
    in_names = set(k for k, v in b.dram.items())
    maps = _in_maps(inputs, in_names, cores)
    maps = [{k: v for k, v in m.items() if k in in_names} for m in maps]
    res = run_bass_kernel_spmd(b.nc, maps, core_ids=list(range(cores)))
    return res.results


def kernel(**inputs):
    res = run(inputs)
    return np.stack([np.asarray(r["out"], dtype=np.float32) for r in res], axis=0)
```

```python
import math
import numpy as np
import concourse.bass as bass
import concourse.mybir as mybir
from concourse.bass_utils import run_bass_kernel_spmd

F32 = mybir.dt.float32
BF16 = mybir.dt.bfloat16
I32 = mybir.dt.int32
AF = mybir.ActivationFunctionType
ALU = mybir.AluOpType
AX = mybir.AxisListType

DEPTH = 4
D = 1024
NCTX = 256
NLAT = 2048
NT = NCTX + NLAT
ALPHA = (2.0 * DEPTH) ** 0.25
LN_EPS = 1e-5
TCH = [(0, 256), (256, 512), (768, 512), (1280, 512), (1792, 512)]
ENGS = ("pe", "act", "dve", "pool", "sp")
WQ = "pool"
SB_BASE = 16512
SB_TOP = 229344


class Prog:
    def __init__(self, nc, n_dma_sems=8, same_engine_sync=True):
        self.nc = nc
        self.ops = []
        self.res = {}
        self.same = same_engine_sync
        self.n_dma_sems = n_dma_sems
        self.dma_rr = {e: 0 for e in ENGS}
        self.dma_last = {}
        self.last_op = {}
        self.open_dmas = []
        self.fence_deps = set()

    def op(self, eng, fn, rd=(), wr=(), kind="c"):
        wr = list(wr) + [r for r in rd if r.startswith("ps") and r not in wr]
        deps = set()
        for r in rd:
            st = self.res.get(r)
            if st is not None and st[0] is not None:
                deps.add(st[0])
        for r in wr:
            st = self.res.get(r)
            if st is not None:
                if st[0] is not None:
                    deps.add(st[0])
                for o in st[1].values():
                    deps.add(o)
        deps.update(self.fence_deps)
        oid = len(self.ops)
        rec = dict(eng=eng, fn=fn, deps=deps, kind=kind, slot=None)
        if kind == "dma":
            self.open_dmas.append(oid)
        else:
            self.last_op[eng] = oid
        if kind == "dma":
            slot = self.dma_rr[eng] % self.n_dma_sems
            self.dma_rr[eng] += 1
            rec["slot"] = slot
            prev = self.dma_last.get((eng, slot))
            if prev is not None:
                deps.add(prev)
            self.dma_last[(eng, slot)] = oid
        self.ops.append(rec)
        for r in rd:
            st = self.res.setdefault(r, [None, {}])
            st[1][(eng, oid if kind == "dma" else -1)] = oid
        for r in wr:
            self.res[r] = [oid, {}]
        return oid

    def fence(self):
        f = set(self.last_op.values())
        f.update(self.open_dmas)
        self.open_dmas = []
        self.fence_deps = f
        self.res = {}

    def dma(self, eng, out, in_, rd=(), wr=(), **kw):
        return self.op(eng, lambda e: e.dma_start(out=out, in_=in_, **kw), rd, wr, kind="dma")

    def emit(self, final_wait_ops=()):
        nc = self.nc
        ops = self.ops
        needed = set()
        for o in ops:
            needed.update(o["deps"])
        needed.update(final_wait_ops)
        esem = {e: nc.alloc_semaphore("s_" + e) for e in ENGS}
        dsem = {}
        for (e, s) in self.dma_last:
            dsem[(e, s)] = nc.alloc_semaphore("d_%s%d" % (e, s))
        ecount = {e: 0 for e in ENGS}
        dcount = {k: 0 for k in dsem}
        for i, o in enumerate(ops):
            if o["kind"] == "dma":
                k = (o["eng"], o["slot"])
                dcount[k] += 16
                o["sig"] = (dsem[k], dcount[k], ("d",) + k)
            elif i in needed:
                ecount[o["eng"]] += 1
                o["sig"] = (esem[o["eng"]], ecount[o["eng"]], ("e", o["eng"]))
            else:
                o["sig"] = None
        streams = {e: [] for e in ENGS}
        seen = {e: {} for e in ENGS}
        for i, o in enumerate(ops):
            e = o["eng"]
            waits = {}
            for d in o["deps"]:
                od = ops[d]
                if od["kind"] != "dma" and od["eng"] == e and (e == "pe" or not self.same):
                    continue
                sem, val, key = od["sig"]
                if seen[e].get(key, 0) >= val:
                    continue
                if key not in waits or waits[key][1] < val:
                    waits[key] = (sem, val)
            for key, (sem, val) in waits.items():
                seen[e][key] = val
            streams[e].append((list(waits.values()), o))
        fin = [(ops[d]["sig"][0], ops[d]["sig"][1]) for d in final_wait_ops]
        self.n_instr = {e: len(streams[e]) for e in ENGS}

        def run(engname, engobj):
            for waits, o in streams[engname]:
                for sem, val in waits:
                    engobj.wait_ge(sem, val)
                ins = o["fn"](engobj)
                if o["sig"] is not None:
                    ins.then_inc(o["sig"][0], 16 if o["kind"] == "dma" else 1)
            if engname == "sp":
                for sem, val in fin:
                    engobj.wait_ge(sem, val)

        with nc.Block() as block:
            @block.tensor
            def _(t):
                run("pe", t)

            @block.scalar
            def _(t):
                run("act", t)

            @block.vector
            def _(t):
                run("dve", t)

            @block.gpsimd
            def _(t):
                run("pool", t)

            @block.sync
            def _(t):
                run("sp", t)


def XAP(t, offset, dims):
    return bass.AP(t, offset, [list(d) for d in dims])


class Builder:
    def __init__(self, n_layers=DEPTH, dbg=(), skip=()):
        self.stop = [x[5:] for x in skip if x.startswith("stop:")]
        self.n_layers = n_layers
        self.dbg = set(dbg)
        self.skip = set(skip)
        self.nc = bass.Bass("TRN2", target_bir_lowering=False)
        self.P = Prog(self.nc)
        self.out_ops = []
        self.dram = {}
        self.uid = 0
        self.sb_ptr = SB_BASE
        self.ph_base = None

    def din(self, name, shape, dt=F32):
        if name not in self.dram:
            self.dram[name] = self.nc.dram_tensor(name, list(shape), dt, kind="ExternalInput").ap()
        return self.dram[name]

    def dout(self, name, shape, dt=F32):
        self.dram[name] = self.nc.dram_tensor(name, list(shape), dt, kind="ExternalOutput").ap()
        return self.dram[name]

    def sb(self, name, shape, dt=F32, at=None):
        esz = 2 if dt == BF16 else 4
        nbytes = esz
        for d_ in shape[1:]:
            nbytes *= d_
        nbytes = (nbytes + 31) // 32 * 32
        if at is None:
            off = self.sb_ptr
            self.sb_ptr += nbytes
        else:
            off = self.ph_base + at
        assert off + nbytes <= SB_TOP, (name, off, nbytes)
        self.uid += 1
        return self.nc.alloc_sbuf_tensor_at("%s_%d" % (name, self.uid), list(shape), dt, offset=off)

    def phase(self, reserve=0):
        self.P.fence()
        self.sb_ptr = self.ph_base + reserve

    def mm(self, out, lhsT, rhs, start, stop, rd, wr, **kw):
        self.P.op("pe", lambda e: e.matmul(out, lhsT=lhsT, rhs=rhs, start=start, stop=stop, **kw), rd, wr)

    def tr(self, out, in_, ident, rd, wr):
        self.P.op("pe", lambda e: e.transpose(out, in_, ident), rd, wr)

    def act(self, out, in_, func, rd, wr, **kw):
        self.P.op("act", lambda e: e.activation(out=out, in_=in_, func=func, **kw), rd, wr)

    def tt(self, eng, out, in0, in1, op, rd, wr):
        self.P.op(eng, lambda e: e.tensor_tensor(out=out, in0=in0, in1=in1, op=op), rd, wr)

    def ts(self, eng, out, in0, s1, s2, op0, op1, rd, wr):
        if op1 is None and eng == "pool" and op0 == ALU.mult:
            op1 = ALU.add
            s2 = 0.0
        if op1 is None:
            self.P.op(eng, lambda e: e.tensor_scalar(out=out, in0=in0, scalar1=s1, scalar2=None, op0=op0), rd, wr)
        else:
            self.P.op(eng, lambda e: e.tensor_scalar(out=out, in0=in0, scalar1=s1, scalar2=s2, op0=op0, op1=op1), rd, wr)

    def stt(self, out, in0, scalar, in1, op0, op1, rd, wr):
        self.P.op("dve", lambda e: e.scalar_tensor_tensor(out=out, in0=in0, scalar=scalar, in1=in1, op0=op0, op1=op1), rd, wr)

    def cp(self, eng, out, in_, rd, wr):
        if eng == "act":
            self.act(out, in_, AF.Copy, rd, wr)
        else:
            self.P.op(eng, lambda e: e.tensor_copy(out=out, in_=in_), rd, wr)

    def red(self, out, in_, op, rd, wr):
        self.P.op("dve", lambda e: e.tensor_reduce(out=out, in_=in_, axis=AX.X, op=op), rd, wr)

    def recip(self, out, in_, rd, wr):
        self.P.op("dve", lambda e: e.reciprocal(out=out, in_=in_), rd, wr)

    def memset(self, eng, ap, val, wr):
        self.P.op(eng, lambda e: e.memset(ap, val), (), wr)

    def dma(self, q, out, in_, rd, wr, **kw):
        return self.P.dma(q, out, in_, rd, wr, **kw)

    def dmas(self, q, out, in_, rd, wr):
        return self.P.dma(q, out, in_, rd, wr, allow_slow_non_contiguous=True)

    def dump(self, name, ap_sb, shape, rd, dt=F32):
        d = self.dout(name, shape, F32)
        if len(shape) == 3:
            for c in range(shape[1]):
                self.out_ops.append(self.dma(WQ, d[:, c, :], ap_sb[:, c, :], rd, ()))
        else:
            self.out_ops.append(self.dma(WQ, d, ap_sb, rd, ()))

    def W(self, name):
        shapes = {
            "w_mod": [DEPTH, D, 6 * D], "b_mod": [DEPTH, 6 * D], "w_in": [DEPTH, D, 5632], "lamv": [DEPTH, 4, 64],
            "subln_g": [DEPTH, 128], "w_pa": [DEPTH, D, D], "w_ps": [DEPTH, 512, D], "w_o": [DEPTH, D, D],
            "w_glu": [DEPTH, 512, 512], "b_glu": [DEPTH, 512], "ln1_g": [DEPTH, D], "ln1_b": [DEPTH, D],
            "ln2_g": [DEPTH, D], "ln2_b": [DEPTH, D], "router_g_w": [DEPTH, D, 4], "router_g_b": [DEPTH, 4],
            "router_e_w": [DEPTH, D, 32], "router_e_b": [DEPTH, 32], "moe_w1": [DEPTH, 32, D, 512],
            "moe_w3": [DEPTH, 32, D, 512], "moe_w2": [DEPTH, 32, 512, D],
            "ssm_a_re": [DEPTH, 2, 32, 64], "ssm_a_im": [DEPTH, 2, 32, 64], "ssm_log_dt": [DEPTH, 2, 32],
            "ssm_b_re": [DEPTH, 2, 32, 64, 16], "ssm_b_im": [DEPTH, 2, 32, 64, 16],
            "ssm_c_re": [DEPTH, 2, 32, 16, 64], "ssm_c_im": [DEPTH, 2, 32, 16, 64], "ssm_d": [DEPTH, 512],
            "x": [NLAT, D], "ctx": [NCTX, D], "cvec": [2, D], "ident": [128, 128], "rmat": [128, 128],
            "ropecos": [128, NLAT], "ropesin": [128, NLAT], "swp": [128, 128], "gmask": [128, 8], "sgn": [128, 2],
        }
        return self.din(name, shapes[name])

    @staticmethod
    def hk(c, j):
        return "hT%d_%d" % (c, j)

    @staticmethod
    def xk(c, j):
        return "xT%d_%d" % (c, j)

    @staticmethod
    def bk(c, j):
        return "bg%d_%d" % (c, j)

    @staticmethod
    def tj(ti):
        return 0 if ti < 2 else 1 + (ti - 2) // 4

    def vecload(self, dst, src_row_ap, key, n=None):
        n = dst.shape[1]
        st = self.vstage[self.vcnt % 2]; sk = "vstage%d" % (self.vcnt % 2)
        self.vcnt += 1
        self.dma("sp", st[0:n, :], src_row_ap.rearrange("(c p) -> c p", p=128), (), [sk])
        self.tr(self.ps[7][:, 0:n], st[0:n, :], self.identf[0:n, 0:n], [sk, "identf"], ["ps7"])
        self.cp("dve", dst, self.ps[7][:, 0:n], ["ps7"], [key])

    def bcast_load(self, dst, src_ap, n, key):
        st = self.bstage
        self.dma("sp", st[0:1, 0:n], src_ap, (), ["bstage"])
        self.mm(self.ps[7][:, 0:n], self.ones1[0:1, :], st[0:1, 0:n], True, True, ["bstage", "ones1"], ["ps7"])
        self.cp("dve", dst, self.ps[7][:, 0:n], ["ps7"], [key])

    def build(self):
        B = self
        nc = self.nc
        L = self.n_layers
        x_d = B.W("x"); ctx_d = B.W("ctx"); cvec_d = B.W("cvec")
        out_d = B.dout("out", [NLAT, D])
        self.xT = xT = B.sb("xT", [128, 8, NT]); self.hT = B.sb("hT", [128, 8, NT], BF16)
        self.identf = B.sb("identf", [128, 128]); self.identb = B.sb("identb", [128, 128], BF16)
        self.rmatb = B.sb("rmatb", [128, 128], BF16)
        self.cosb = B.sb("cosb", [128, NLAT], BF16); self.sinb = B.sb("sinb", [128, NLAT], BF16)
        self.onesf = B.sb("onesf", [128, 128]); self.zerob = B.sb("zerob", [128, 512], BF16)
        self.epsc = B.sb("epsc", [128, 1]); self.negpi = B.sb("negpi", [128, 1])
        self.modx = B.sb("modx", [128, 48]); self.cmodx = B.sb("cmodx", [128, 48]); self.bmod = B.sb("bmod", [128, 48])
        self.cv = B.sb("cv", [128, 2, 8]); self.scv = B.sb("scv", [128, 8, 2], BF16)
        self.vstage = [B.sb("vstage%d" % i, [48, 128]) for i in range(2)]; self.vcnt = 0
        self.bstage = B.sb("bstage", [1, 256]); self.ones1 = B.sb("ones1", [1, 128])
        self.swpf = B.sb("swpf", [128, 128]); self.gmaskf = B.sb("gmaskf", [128, 8]); self.sgnf = B.sb("sgnf", [128, 2])
        self.ph_base = (self.sb_ptr + 63) // 64 * 64
        self.ps = ps = [nc.alloc_psum_tensor("ps%d" % i, [128, 512], F32) for i in range(8)]
        PSK = lambda i: "ps%d" % i
        identf = self.identf

        B.dma("sp", identf[:, :], B.W("ident")[:, :], (), ["identf"])
        B.dma(WQ, self.identb[:, :], B.W("ident")[:, :], (), ["identb"])
        B.dma(WQ, self.rmatb[:, :], B.W("rmat")[:, :], (), ["rmatb"])
        B.dma(WQ, self.cosb[:, :], B.W("ropecos")[:, :], (), ["cosb"])
        B.dma(WQ, self.sinb[:, :], B.W("ropesin")[:, :], (), ["sinb"])
        B.dma("sp", self.swpf[:, :], B.W("swp")[:, :], (), ["swpf"])
        B.dma("sp", self.gmaskf[:, :], B.W("gmask")[:, :], (), ["gmaskf"])
        B.dma("sp", self.sgnf[:, :], B.W("sgn")[:, :], (), ["sgnf"])
        B.memset("dve", self.onesf[:, :], 1.0 / D, ["onesf"])
        B.memset("dve", self.zerob[:, :], 0.0, ["zerob"])
        B.memset("dve", self.epsc[:, :], LN_EPS, ["epsc"])
        B.memset("dve", self.negpi[:, :], -math.pi, ["negpi"])
        B.memset("dve", self.ones1[:, :], 1.0, ["ones1"])

        self.phase()
        stg = [B.sb("stg%d" % i, [128, D]) for i in range(2)]
        for ti in range(NT // 128):
            s = stg[ti % 2]; sk = "stg%d" % (ti % 2)
            src = ctx_d[ti * 128:(ti + 1) * 128, :] if ti < 2 else x_d[(ti - 2) * 128:(ti - 1) * 128, :]
            B.dma("sp", s[:, :], src, (), [sk])
            j = self.tj(ti)
            for half in range(2):
                pb = ps[half]
                for cc in range(4):
                    c = half * 4 + cc
                    B.tr(pb[:, cc * 128:(cc + 1) * 128], s[:, c * 128:(c + 1) * 128], identf[:, :], [sk, "identf"], [PSK(half)])
                eng = "dve" if half == 0 else "act"
                B.cp(eng, XAP(xT, half * 4 * NT + ti * 128, [[8 * NT, 128], [NT, 4], [1, 128]]),
                     pb[:, :].rearrange("p (c t) -> p c t", t=128), [PSK(half)], [self.xk(c, j) for c in range(half * 4, half * 4 + 4)])
        for r in range(2):
            self.vecload(self.cv[:, r, :], cvec_d[r, :], "cv%d" % r)
        B.act(self.scv[:, :, :].rearrange("p c r -> p r c"), self.cv[:, :, :], AF.Silu, ["cv0", "cv1"], ["scv"])

        for l in range(L):
            self.layer(l)

        self.phase()
        ostg = [B.sb("ostg%d" % i, [128, D]) for i in range(2)]
        for ti in range(2, NT // 128):
            s = ostg[ti % 2]; sk = "ostg%d" % (ti % 2)
            j = self.tj(ti)
            for half in range(2):
                pb = ps[half]
                for cc in range(4):
                    c = half * 4 + cc
                    B.tr(pb[:, cc * 128:(cc + 1) * 128], xT[:, c, ti * 128:(ti + 1) * 128], identf[:, :], [self.xk(c, j), "identf"], [PSK(half)])
                eng = "dve" if half == 0 else "act"
                B.cp(eng, s[:, half * 512:(half + 1) * 512], pb[:, :], [PSK(half)], [sk + "_%d" % half])
            self.out_ops.append(B.dma("sp", out_d[(ti - 2) * 128:(ti - 1) * 128, :], s[:, :], [sk + "_0", sk + "_1"], ()))
        self.P.emit(final_wait_ops=self.out_ops)
        return nc

    def layer(self, l):
        B = self
        self.last = (l == DEPTH - 1)
        ps = self.ps
        PSK = lambda i: "ps%d" % i
        xT, hT = self.xT, self.hT
        modx, cmodx, bmod = self.modx, self.cmodx, self.bmod
        w_mod = self.W("w_mod"); b_mod = self.W("b_mod")
        self.phase()
        wm = [B.sb("wm%d" % i, [128, 8, 512], BF16) for i in range(2)]
        self.vecload(bmod[:, :], b_mod[l, :], "bmod")
        for blk in range(12):
            wt = wm[blk % 2]; wk_ = "wm%d" % (blk % 2)
            B.dma(WQ, wt[:, :, :], w_mod[l, :, blk * 512:(blk + 1) * 512].rearrange("(c p) n -> p c n", p=128), (), [wk_])
            for jj in range(4):
                j = blk * 4 + jj
                for kc in range(8):
                    B.mm(ps[0][:, 2 * j:2 * j + 2], wt[:, kc, jj * 128:(jj + 1) * 128], self.scv[:, kc, :], kc == 0, kc == 7,
                         [wk_, "scv"], [PSK(0)])
        pv = ps[0][:, 0:96].rearrange("p (j t) -> p j t", t=2)
        B.tt("dve", modx[:, :], pv[:, :, 0], bmod[:, :], ALU.add, [PSK(0), "bmod"], ["modx"])
        B.tt("dve", cmodx[:, :], pv[:, :, 1], bmod[:, :], ALU.add, [PSK(0), "bmod"], ["cmodx"])
        for m_, k_ in ((modx, "modx"), (cmodx, "cmodx")):
            B.ts("dve", m_[:, 8:16], m_[:, 8:16], 1.0, None, ALU.add, None, [k_], [k_])
            B.ts("dve", m_[:, 32:40], m_[:, 32:40], 1.0, None, ALU.add, None, [k_], [k_])
        if "mod" in self.dbg and l == 0:
            self.dump("d_modx", modx[:, :], [128, 48], ["modx"]); self.dump("d_cmodx", cmodx[:, :], [128, 48], ["cmodx"])
        if "mod" in self.stop:
            return
        self.modulate(0)
        if "h" in self.dbg and l == 0:
            self.dump("d_hT", hT[:, :, :], [128, 8, NT], [self.hk(c, j) for c in range(8) for j in range(5)], BF16)
        if "h" in self.stop:
            return
        self.phase()
        self.big = B.sb("attnT", [128, 8, NT], BF16)
        if "attn" not in self.skip:
            self.attn_prep(l)
            import os
            self.att_stage = int(os.environ.get("ATT_STAGE", "9"))
            for h in range(int(os.environ.get("ATT_HEADS", "8"))):
                self.attention(l, h)
            if "attn" in self.dbg and l == 0:
                self.dump("d_attnT", self.big[:, :, :], [128, 8, NT], [self.bk(c, j) for c in range(8) for j in range(5)], BF16)
        if "attn" in self.stop:
            return
        self.phase(reserve=8 * NT * 2)
        self.scale_x()
        if "attn" not in self.skip:
            self.merge(l, "attn")
        if "amerge" in self.stop:
            return
        if "ssm" not in self.skip:
            self.ssm(l)
            self.merge(l, "ssm")
        self.phase()
        self.layernorm(l, 1)
        if "mid" in self.dbg and l == 0:
            self.dump("d_xmid", xT[:, :, :], [128, 8, NT], [self.xk(c, j) for c in range(8) for j in range(5)])
        self.modulate(24)
        if "moe" not in self.skip:
            self.router(l)
        self.scale_x()
        if "moe" not in self.skip:
            self.moe(l)
        self.phase()
        self.layernorm(l, 2)

    def modulate(self, base):
        B = self
        xT, hT = self.xT, self.hT
        for c in range(8):
            for j, (t0, n) in enumerate(TCH):
                if self.last and j == 0 and base != 0:
                    continue
                m_, k_ = (self.cmodx, "cmodx") if j == 0 else (self.modx, "modx")
                B.act(hT[:, c, t0:t0 + n], xT[:, c, t0:t0 + n], AF.Identity, [self.xk(c, j), k_], [self.hk(c, j)],
                      scale=m_[:, base + 8 + c:base + 9 + c], bias=m_[:, base + c:base + c + 1])

    def scale_x(self):
        B = self
        xT = self.xT
        for c in range(8):
            for j, (t0, n) in enumerate(TCH):
                eng = "pool" if (c + j) % 2 else "dve"
                B.ts(eng, xT[:, c, t0:t0 + n], xT[:, c, t0:t0 + n], float(ALPHA), None, ALU.mult, None, [self.xk(c, j)], [self.xk(c, j)])

    def attn_prep(self, l):
        B = self
        at = {}
        at["wa"] = [B.sb("wa%d" % i, [128, 8, 128], BF16) for i in range(2)]
        at["wb"] = [B.sb("wb%d" % i, [128, 8, 128], BF16) for i in range(1)]
        at["wc"] = [B.sb("wc%d" % i, [128, 8, 128], BF16) for i in range(1)]
        at["kT"] = B.sb("kT", [128, NT], BF16); at["qT0"] = B.sb("qT0", [128, NT], BF16); at["qT1"] = B.sb("qT1", [128, NT], BF16)
        at["Vh"] = B.sb("Vh", [128, 18, 129], BF16)
        at["pt"] = [B.sb("pt%d" % i, [128, 512], BF16) for i in range(4)]
        at["tb"] = [B.sb("tb%d" % i, [128, 512], BF16) for i in range(1)]
        at["t1"] = [B.sb("t1_%d" % i, [128, 512]) for i in range(1)]
        at["t2"] = [B.sb("t2_%d" % i, [128, 512]) for i in range(1)]
        at["lamt"] = B.sb("lamt", [128, 4, 64]); at["lamp"] = B.sb("lamp", [128, 2, 64]); at["lams"] = B.sb("lams", [128, 4])
        at["nlam"] = B.sb("nlam", [128, 1]); at["g128"] = B.sb("g128", [128, 128])
        at["sm"] = [B.sb("sm%d" % i, [128, 8]) for i in range(4)]
        at["of"] = [B.sb("of%d" % i, [128, 128]) for i in range(2)]
        at["ot"] = [B.sb("ot%d" % i, [128, 128]) for i in range(2)]
        at["onb"] = [B.sb("onb%d" % i, [128, 128], BF16) for i in range(2)]
        at["osb"] = [B.sb("osb%d" % i, [128, 258]) for i in range(8)]
        self.at = at
        B.memset("pool", at["Vh"][:, :, :], 1.0, ["Vh"])
        B.memset("pool", at["qT0"][:, :], 0.0, ["qT0z"])
        B.memset("pool", at["qT1"][:, :], 0.0, ["qT1z"])
        lamv = self.W("lamv"); subg = self.W("subln_g")
        lam_init = 0.8 - 0.6 * math.exp(-0.3 * l)
        self.bcast_load(at["lamt"][:, :, :].rearrange("p a b -> p (a b)"), lamv[l:l + 1, :, :].rearrange("o a b -> o (a b)"), 256, "lamt")
        self.bcast_load(at["g128"][:, :], subg[l:l + 1, :], 128, "g128")
        B.ts("dve", at["g128"][:, :], at["g128"][:, :], float(1.0 - lam_init), None, ALU.mult, None, ["g128"], ["g128"])
        B.tt("dve", at["lamp"][:, 0, :], at["lamt"][:, 0, :], at["lamt"][:, 1, :], ALU.mult, ["lamt"], ["lamp"])
        B.tt("dve", at["lamp"][:, 1, :], at["lamt"][:, 2, :], at["lamt"][:, 3, :], ALU.mult, ["lamt"], ["lamp"])
        B.red(at["lams"][:, 0:2], at["lamp"][:, :, :], ALU.add, ["lamp"], ["lams"])
        B.act(at["lams"][:, 2:4], at["lams"][:, 0:2], AF.Exp, ["lams"], ["lams2"])
        B.tt("dve", at["nlam"][:, :], at["lams"][:, 3:4], at["lams"][:, 2:3], ALU.subtract, ["lams2"], ["nlam"])
        B.ts("dve", at["nlam"][:, :], at["nlam"][:, :], float(-lam_init), None, ALU.add, None, ["nlam"], ["nlam"])

    def proj_fm(self, wt, wkey, nkc, src, srck, dst_fn, dkey_fn, rope=False, tiles=None, evac="act", split=None):
        B = self
        ps = self.ps
        for j, (t0, n) in enumerate(TCH):
            pb = ps[j % 2]; pk = "ps%d" % (j % 2)
            for kc in range(nkc):
                B.mm(pb[:, :n], wt[:, kc, :], src[:, kc, t0:t0 + n], kc == 0, kc == nkc - 1, [wkey, srck(kc, j)], [pk])
            import os
            rm = int(os.environ.get("ROPE_MODE", "2"))
            if split is not None and (j == 0 or not rope):
                B.cp("act", split[0](t0, n), pb[0:64, :n], [pk] + split[2], [dkey_fn(j)])
                B.cp("act", split[1](t0, n), pb[64:128, :n], [pk] + split[2], [dkey_fn(j)])
            elif j == 0 or not rope or rm == 0:
                B.cp(evac if j % 2 == 0 else "dve", dst_fn(t0, n), pb[:, :n], [pk], [dkey_fn(j)])
            else:
                tb = tiles["tb"][0]; tbk = "tb0"
                t1 = tiles["t1"][0]; t1k = "t1_0"
                t2 = tiles["t2"][0]; t2k = "t2_0"
                pr = ps[2 + j % 2]; prk = "ps%d" % (2 + j % 2)
                B.cp("act", tb[:, :n], pb[:, :n], [pk], [tbk])
                B.mm(pr[:, :n], self.rmatb[:, :], tb[:, :n], True, True, ["rmatb", tbk], [prk])
                lo = t0 - NCTX
                if rm in (3, 4, 5):
                    if rm >= 4:
                        B.tt("dve", t1[:, :n], pb[:, :n], self.cosb[:, lo:lo + n], ALU.mult, [pk, "cosb", tbk], [t1k])
                    if rm >= 5:
                        B.tt("dve", t2[:, :n], pr[:, :n], self.sinb[:, lo:lo + n], ALU.mult, [prk, "sinb"], [t2k])
                    B.cp("dve", dst_fn(t0, n), pb[:, :n], [pk, prk], [dkey_fn(j)])
                    continue
                B.tt("dve", t1[:, :n], pb[:, :n], self.cosb[:, lo:lo + n], ALU.mult, [pk, "cosb", tbk], [t1k])
                B.tt("dve", t2[:, :n], pr[:, :n], self.sinb[:, lo:lo + n], ALU.mult, [prk, "sinb"], [t2k])
                if split is not None:
                    B.tt("pool", split[0](t0, n), t1[0:64, :n], t2[0:64, :n], ALU.add, [t1k, t2k] + split[2], [dkey_fn(j)])
                    B.tt("pool", split[1](t0, n), t1[64:128, :n], t2[64:128, :n], ALU.add, [t1k, t2k] + split[2], [dkey_fn(j)])
                else:
                    B.tt("pool" if rm == 2 else "dve", dst_fn(t0, n), t1[:, :n], t2[:, :n], ALU.add, [t1k, t2k], [dkey_fn(j)])

    def attention(self, l, h):
        B = self
        at = self.at; ps = self.ps; hT = self.hT; big = self.big
        w_in = self.W("w_in")
        i2 = h % 2
        wk, wq, wv = at["wa"][i2], at["wb"][0], at["wc"][0]
        wkk, wqk, wvk = "wa%d" % i2, "wb0", "wc0"
        for wt, key, off in ((wk, wkk, 0), (wq, wqk, 2560), (wv, wvk, 1024)):
            B.dma(WQ, wt[:, :, :], w_in[l, :, off + h * 128:off + (h + 1) * 128].rearrange("(c p) n -> p c n", p=128), (), [key])
        kT, Vh = at["kT"], at["Vh"]
        qT0, qT1 = at["qT0"], at["qT1"]
        self.proj_fm(wk, wkk, 8, hT, self.hk, lambda t0, n: kT[:, t0:t0 + n], lambda j: "kT_%d" % j, True, at)
        self.proj_fm(wq, wqk, 8, hT, self.hk, None, lambda j: "qT_%d" % j, True, at,
                     split=(lambda t0, n: qT0[0:64, t0:t0 + n], lambda t0, n: qT1[64:128, t0:t0 + n], ["qT0z", "qT1z"]))
        if self.att_stage < 1:
            return
        if "qk" in self.dbg and l == 0 and h == 0:
            self.dump("d_kT", kT[:, :], [128, NT], ["kT_%d" % j for j in range(5)], BF16)
            pass
        for t4 in range(5):
            tiles = list(range(t4 * 4, min(18, t4 * 4 + 4)))
            pb = ps[t4 % 2]; pk = "ps%d" % (t4 % 2)
            for ii, ti in enumerate(tiles):
                j = self.tj(ti)
                for kc in range(8):
                    B.mm(pb[:, ii * 128:(ii + 1) * 128], hT[:, kc, ti * 128:(ti + 1) * 128], wv[:, kc, :], kc == 0, kc == 7,
                         [wvk, self.hk(kc, j)], [pk])
            nt_ = len(tiles)
            B.cp("act", XAP(Vh, tiles[0] * 129, [[18 * 129, 128], [129, nt_], [1, 128]]),
                 pb[:, 0:nt_ * 128].rearrange("p (a b) -> p a b", b=128), [pk], ["Vh"])
        if self.att_stage < 2:
            return
        blocks = [(0, 0, 256, 2)] + [(1 + i, 256 + 512 * i, 512, 18) for i in range(4)]
        if self.last:
            blocks = blocks[1:]
        cnt = 0
        pending_epi = None
        for bi, (qi, q0, qn, nk) in enumerate(blocks):
            nsub = qn // 128
            nb = (nsub + 1) // 2
            ob = (bi % 2) * 4
            for m in range(2):
                for bb in range(nb):
                    bk_ = 4 + 2 * m + bb
                    B.mm(ps[bk_][:, :], self.zerob[:, 0:128], self.zerob[:, :], True, True, ["zerob"], ["ps%d" % bk_])
            for m in range(2):
                queue = []
                for kt in range(nk + 2):
                    if kt < nk:
                        sbk = cnt % 3
                        pt = at["pt"][cnt % 4]; ptk = "pt%d" % (cnt % 4)
                        cnt += 1
                        jk = self.tj(kt)
                        B.mm(ps[sbk][:, :qn], kT[:, kt * 128:(kt + 1) * 128], (qT0 if m == 0 else qT1)[:, q0:q0 + qn], True, True,
                             ["kT_%d" % jk, "qT_%d" % qi, "qT0z", "qT1z"], ["ps%d" % sbk])
                        B.act(pt[:, :qn], ps[sbk][:, :qn], AF.Exp, ["ps%d" % sbk], [ptk], scale=0.125)
                        queue.append((pt, ptk, kt))
                    if queue and (len(queue) > 2 or kt >= nk):
                        ppt, pptk, pkt = queue.pop(0)
                        for sub in range(nsub):
                            bk_ = 4 + 2 * m + sub // 2
                            col = (sub % 2) * 129
                            B.mm(ps[bk_][:, col:col + 129], ppt[:, sub * 128:(sub + 1) * 128], Vh[:, pkt, :], False, pkt == nk - 1,
                                 [pptk, "Vh"], ["ps%d" % bk_], skip_group_check=True)
            osb = at["osb"]
            for m in range(2):
                for bb in range(nb):
                    bk_ = 4 + 2 * m + bb
                    B.cp("dve", osb[ob + bk_ - 4][:, :], ps[bk_][:, 0:258], ["ps%d" % bk_], ["osb%d" % (ob + bk_ - 4)])
            if pending_epi is not None:
                self.attn_epilogue(h, *pending_epi)
            pending_epi = (q0, nsub, ob)
        if pending_epi is not None:
            self.attn_epilogue(h, *pending_epi)

    def attn_epilogue(self, h, q0, nsub, ob):
        B = self
        at = self.at; ps = self.ps; big = self.big
        if self.att_stage < 3:
            return
        for sub in range(nsub):
            b0 = ob + sub // 2; b1 = ob + 2 + sub // 2
            col = (sub % 2) * 129
            sm = at["sm"][sub % 4]; smk = "sm%d" % (sub % 4)
            of = at["of"][sub % 2]; ofk = "of%d" % (sub % 2)
            ot = at["ot"][sub % 2]; otk = "ot%d" % (sub % 2)
            onb = at["onb"][sub % 2]; onk = "onb%d" % (sub % 2)
            o0, o1 = at["osb"][b0], at["osb"][b1]
            B.recip(sm[:, 0:1], o0[:, col + 128:col + 129], ["osb%d" % b0], [smk])
            B.recip(sm[:, 1:2], o1[:, col + 128:col + 129], ["osb%d" % b1], [smk])
            B.tt("dve", sm[:, 2:3], sm[:, 1:2], at["nlam"][:, :], ALU.mult, [smk, "nlam"], [smk])
            B.ts("dve", ot[:, :], o1[:, col:col + 128], sm[:, 2:3], None, ALU.mult, None, ["osb%d" % b1, smk], [otk])
            B.stt(of[:, :], o0[:, col:col + 128], sm[:, 0:1], ot[:, :], ALU.mult, ALU.add, ["osb%d" % b0, smk, otk], [ofk])
            B.tt("dve", ot[:, :], of[:, :], of[:, :], ALU.mult, [ofk], [otk])
            B.red(sm[:, 3:4], ot[:, :], ALU.add, [otk], [smk])
            B.act(sm[:, 4:5], sm[:, 3:4], AF.Ln, [smk], [smk], scale=1.0 / 128.0, bias=self.epsc[:, :])
            B.act(sm[:, 5:6], sm[:, 4:5], AF.Exp, [smk], [smk], scale=-0.5)
            B.stt(onb[:, :], of[:, :], sm[:, 5:6], at["g128"][:, :], ALU.mult, ALU.mult, [ofk, smk, "g128"], [onk])
            pbf = ps[3][:, (sub % 2) * 64:(sub % 2) * 64 + 64].bitcast(BF16)
            B.tr(pbf, onb[:, :], self.identb[:, :], [onk, "identb"], ["ps3"])
            tq = q0 + sub * 128
            jq = 0 if tq < 256 else 1 + (tq - 256) // 512
            B.cp("dve", big[:, h, tq:tq + 128], pbf, ["ps3"], [self.bk(h, jq)])

    def merge(self, l, kind):
        B = self
        ps = self.ps; hT = self.hT; xT = self.xT
        w_in = self.W("w_in"); w_o = self.W("w_o")
        if kind == "attn":
            goff = 3584; wp = self.W("w_pa"); nkc = 8; src = self.big; srck = self.bk
        else:
            goff = 4608; wp = self.W("w_ps"); nkc = 4; src = self.ssmT; srck = lambda c, j: "ss%d_%d" % (c, j)
        wa = [B.sb("mwa%d" % i, [128, 8, 128], BF16) for i in range(2)]
        wb = [B.sb("mwb%d" % i, [128, 8, 128], BF16) for i in range(2)]
        wo = [B.sb("mwo%d" % i, [128, 1024], BF16) for i in range(2)]
        mC = [B.sb("mC%d" % i, [128, NT], BF16) for i in range(2)]
        sg = [B.sb("msg%d" % i, [128, 512]) for i in range(2)]
        ycnt = 0
        for c in range(8):
            i2 = c % 2
            B.dma(WQ, wa[i2][:, :, :], w_in[l, :, goff + c * 128:goff + (c + 1) * 128].rearrange("(c p) n -> p c n", p=128), (), ["mwa%d" % i2])
            B.dma(WQ, wb[i2][:, 0:nkc, :], wp[l, :, c * 128:(c + 1) * 128].rearrange("(c p) n -> p c n", p=128), (), ["mwb%d" % i2])
            B.dma(WQ, wo[i2][:, :], w_o[l, c * 128:(c + 1) * 128, :], (), ["mwo%d" % i2])
            for j, (t0, n) in enumerate(TCH):
                if self.last and j == 0:
                    continue
                pg = ps[j % 2]; pgk = "ps%d" % (j % 2)
                pp = ps[2 + j % 2]; ppk = "ps%d" % (2 + j % 2)
                for kc in range(8):
                    B.mm(pg[:, :n], wa[i2][:, kc, :], hT[:, kc, t0:t0 + n], kc == 0, kc == 7, ["mwa%d" % i2, self.hk(kc, j)], [pgk])
                for kc in range(nkc):
                    B.mm(pp[:, :n], wb[i2][:, kc, :], src[:, kc, t0:t0 + n], kc == 0, kc == nkc - 1, ["mwb%d" % i2, srck(kc, j)], [ppk])
                B.act(sg[j % 2][:, :n], pg[:, :n], AF.Sigmoid, [pgk], ["msg%d" % (j % 2)])
                B.tt("dve", mC[i2][:, t0:t0 + n], sg[j % 2][:, :n], pp[:, :n], ALU.mult, ["msg%d" % (j % 2), ppk], ["mC%d_%d" % (i2, j)])
            for o in range(8):
                for j, (t0, n) in enumerate(TCH):
                    if self.last and j == 0:
                        continue
                    py = ps[4 + ycnt % 4]; pyk = "ps%d" % (4 + ycnt % 4)
                    ycnt += 1
                    B.mm(py[:, :n], wo[i2][:, o * 128:(o + 1) * 128], mC[i2][:, t0:t0 + n], True, True, ["mwo%d" % i2, "mC%d_%d" % (i2, j)], [pyk])
                    m_, k_ = (self.cmodx, "cmodx") if j == 0 else (self.modx, "modx")
                    B.stt(xT[:, o, t0:t0 + n], py[:, :n], m_[:, 16 + o:17 + o], xT[:, o, t0:t0 + n], ALU.mult, ALU.add,
                          [pyk, k_, self.xk(o, j)], [self.xk(o, j)])

    def layernorm(self, l, which):
        B = self
        ps = self.ps; xT = self.xT
        g_d = self.W("ln%d_g" % which); b_d = self.W("ln%d_b" % which)
        lng = B.sb("lng", [128, 8]); lnb = B.sb("lnb", [128, 8])
        sq = [B.sb("lnsq%d" % i, [128, 512]) for i in range(2)]
        ta = [B.sb("lnta%d" % i, [128, 512]) for i in range(2)]
        tv = [B.sb("lntv%d" % i, [128, 512]) for i in range(2)]
        self.vecload(lng[:, :], g_d[l, :], "lng"); self.vecload(lnb[:, :], b_d[l, :], "lnb")
        for j, (t0, n) in enumerate(TCH):
            if self.last and j == 0:
                continue
            a = (j % 2) * 2
            p0 = ps[a]; p0k = "ps%d" % a; p1 = ps[a + 1]; p1k = "ps%d" % (a + 1)
            i2 = j % 2
            for c in range(8):
                B.mm(p0[:, :n], self.onesf[:, :], xT[:, c, t0:t0 + n], c == 0, c == 7, ["onesf", self.xk(c, j)], [p0k])
            for c in range(8):
                s_ = sq[c % 2]; sk_ = "lnsq%d" % (c % 2)
                B.act(s_[:, :n], xT[:, c, t0:t0 + n], AF.Square, [self.xk(c, j)], [sk_])
                B.mm(p1[:, :n], self.onesf[:, :], s_[:, :n], c == 0, c == 7, ["onesf", sk_], [p1k])
            tak = "lnta%d" % i2; tvk = "lntv%d" % i2
            B.act(ta[i2][:, :n], p0[:, :n], AF.Square, [p0k], [tak])
            B.tt("dve", tv[i2][:, :n], p1[:, :n], ta[i2][:, :n], ALU.subtract, [p1k, tak], [tvk])
            B.act(tv[i2][:, :n], tv[i2][:, :n], AF.Ln, [tvk], [tvk], bias=self.epsc[:, :])
            B.act(tv[i2][:, :n], tv[i2][:, :n], AF.Exp, [tvk], [tvk], scale=-0.5)
            B.stt(ta[i2][:, :n], p0[:, :n], -1.0, tv[i2][:, :n], ALU.mult, ALU.mult, [p0k, tvk], [tak])
            for c in range(8):
                xs = xT[:, c, t0:t0 + n]
                B.tt("dve", xs, xs, tv[i2][:, :n], ALU.mult, [self.xk(c, j), tvk], [self.xk(c, j)])
                B.tt("pool", xs, xs, ta[i2][:, :n], ALU.add, [self.xk(c, j), tak], [self.xk(c, j)])
                B.act(xs, xs, AF.Identity, [self.xk(c, j), "lng", "lnb"], [self.xk(c, j)], scale=lng[:, c:c + 1], bias=lnb[:, c:c + 1])

    def router(self, l):
        B = self
        ps = self.ps; xT = self.xT
        self.phase()
        self.WT = WT = B.sb("WT", [32, NT])
        wr = B.sb("wr", [128, 8, 36]); rb = B.sb("rb", [128, 36])
        h2 = [B.sb("h2_%d" % i, [128, 8, 128]) for i in range(2)]
        lg = [B.sb("lg%d" % i, [128, 36]) for i in range(2)]
        me = [B.sb("me%d" % i, [128, 32]) for i in range(2)]
        rs = [B.sb("rs%d" % i, [128, 48]) for i in range(2)]
        wt_ = [B.sb("rwt%d" % i, [128, 32]) for i in range(2)]
        mk = [B.sb("rmk%d" % i, [128, 32]) for i in range(2)]
        rgw = self.W("router_g_w"); rgb = self.W("router_g_b"); rew = self.W("router_e_w"); reb = self.W("router_e_b")
        B.dmas("sp", wr[:, :, 0:4], rgw[l, :, :].rearrange("(c p) n -> p c n", p=128), (), ["wr"])
        B.dmas("sp", wr[:, :, 4:36], rew[l, :, :].rearrange("(c p) n -> p c n", p=128), (), ["wr"])
        self.bcast_load(rb[:, 0:4], rgb[l:l + 1, :], 4, "rb")
        self.bcast_load(rb[:, 4:36], reb[l:l + 1, :], 32, "rb")
        for ti in range(18):
            if self.last and ti < 2:
                continue
            i2 = ti % 2
            j = self.tj(ti)
            m_, k_ = (self.cmodx, "cmodx") if ti < 2 else (self.modx, "modx")
            hk_ = "h2_%d" % i2; lk = "lg%d" % i2; mek = "me%d" % i2; rk = "rs%d" % i2; wk_ = "rwt%d" % i2; mkk = "rmk%d" % i2
            for c in range(8):
                B.act(h2[i2][:, c, :], xT[:, c, ti * 128:(ti + 1) * 128], AF.Identity, [self.xk(c, j), k_], [hk_],
                      scale=m_[:, 32 + c:33 + c], bias=m_[:, 24 + c:25 + c])
            for c in range(8):
                B.mm(ps[i2][:, 0:36], h2[i2][:, c, :], wr[:, c, :], c == 0, c == 7, [hk_, "wr"], ["ps%d" % i2])
            L_ = lg[i2]; R_ = rs[i2]; M_ = me[i2]
            B.tt("dve", L_[:, :], ps[i2][:, 0:36], rb[:, :], ALU.add, ["ps%d" % i2, "rb"], [lk])
            B.red(R_[:, 0:1], L_[:, 0:4], ALU.max, [lk], [rk])
            B.ts("dve", R_[:, 1:2], R_[:, 0:1], -1.0, None, ALU.mult, None, [rk], [rk])
            B.ts("dve", R_[:, 8:12], L_[:, 0:4], R_[:, 0:1], None, ALU.is_equal, None, [lk, rk], [rk])
            B.act(R_[:, 12:16], L_[:, 0:4], AF.Exp, [lk, rk], [rk], bias=R_[:, 1:2])
            B.red(R_[:, 2:3], R_[:, 12:16], ALU.add, [rk], [rk])
            B.recip(R_[:, 3:4], R_[:, 2:3], [rk], [rk])
            B.ts("dve", R_[:, 16:20], R_[:, 8:12], -1.0, 1.0e30, ALU.add, ALU.mult, [rk], [rk])
            B.tt("dve", M_[:, :].rearrange("p (a b) -> p a b", b=8), L_[:, 4:36].rearrange("p (a b) -> p a b", b=8),
                 XAP(R_, 16, [[48, 128], [1, 4], [0, 8]]), ALU.add, [lk, rk], [mek])
            B.P.op("dve", lambda e, R_=R_, M_=M_: e.max(out=R_[:, 24:32], in_=M_[:, :]), [mek], [rk])
            B.ts("dve", mk[i2][:, :], M_[:, :], R_[:, 24:25], None, ALU.is_equal, None, [mek, rk], [mkk])
            B.tt("dve", R_[:, 4:5], R_[:, 25:26], R_[:, 24:25], ALU.subtract, [rk], [rk])
            B.act(R_[:, 5:6], R_[:, 4:5], AF.Exp, [rk], [rk])
            B.ts("dve", R_[:, 6:7], R_[:, 5:6], 1.0, None, ALU.add, None, [rk], [rk])
            B.recip(R_[:, 6:7], R_[:, 6:7], [rk], [rk])
            B.tt("dve", R_[:, 7:8], R_[:, 5:6], R_[:, 6:7], ALU.mult, [rk], [rk])
            B.tt("dve", R_[:, 32:33], R_[:, 6:7], R_[:, 3:4], ALU.mult, [rk], [rk])
            B.tt("dve", R_[:, 33:34], R_[:, 7:8], R_[:, 3:4], ALU.mult, [rk], [rk])
            B.ts("dve", wt_[i2][:, :], mk[i2][:, :], R_[:, 32:33], None, ALU.mult, None, [mkk, rk], [wk_])
            B.ts("dve", mk[i2][:, :], M_[:, :], R_[:, 25:26], None, ALU.is_equal, None, [mek, rk, wk_], [mkk])
            B.stt(wt_[i2][:, :], mk[i2][:, :], R_[:, 33:34], wt_[i2][:, :], ALU.mult, ALU.add, [mkk, rk, wk_], [wk_])
            B.tr(ps[2 + i2][0:32, 0:128], wt_[i2][:, :], self.identf[:, :], [wk_, "identf"], ["ps%d" % (2 + i2)])
            B.cp("act", WT[:, ti * 128:(ti + 1) * 128], ps[2 + i2][0:32, 0:128], ["ps%d" % (2 + i2)], ["WT_%d" % j])
        if "rt" in self.dbg and l == 0:
            self.dump("d_WT", WT[:, :], [32, NT], ["WT_%d" % j for j in range(5)])

    def moe(self, l):
        B = self
        ps = self.ps; xT = self.xT; hT = self.hT; WT = self.WT
        mw1 = self.W("moe_w1"); mw3 = self.W("moe_w3"); mw2 = self.W("moe_w2")
        self.sb_ptr = self.ph_base + 32 * 0 + NT * 4
        w1 = [B.sb("w1_%d" % i, [128, 8, 512], BF16) for i in range(2)]
        w3 = [B.sb("w3_%d" % i, [128, 8, 512], BF16) for i in range(2)]
        w2 = [B.sb("w2_%d" % i, [128, 4, 1024], BF16) for i in range(2)]
        gb = [B.sb("gb%d" % i, [128, 4, 512], BF16) for i in range(2)]
        s1 = [B.sb("s1_%d" % i, [128, 512]) for i in range(2)]
        s2 = [B.sb("s2_%d" % i, [128, 512]) for i in range(2)]
        self.P.fence()
        cnt = 0
        for e in range(32):
            i2 = e % 2
            B.dma(WQ, w1[i2][:, :, :], mw1[l, e, :, :].rearrange("(c p) n -> p c n", p=128), (), ["w1_%d" % i2])
            B.dma(WQ, w3[i2][:, :, :], mw3[l, e, :, :].rearrange("(c p) n -> p c n", p=128), (), ["w3_%d" % i2])
            B.dma(WQ, w2[i2][:, :, :], mw2[l, e, :, :].rearrange("(c p) n -> p c n", p=128), (), ["w2_%d" % i2])
            selT = XAP(self.identf, e, [[128, 32], [0, 128]])
            for j, (t0, n) in enumerate(TCH):
                if self.last and j == 0:
                    continue
                pw = ps[j % 2]; pwk = "ps%d" % (j % 2)
                B.mm(pw[:, :n], selT, WT[:, t0:t0 + n], True, True, ["identf", "WT_%d" % j], [pwk])
                g_ = gb[j % 2]
                for hc in range(4):
                    p1 = ps[2 + hc % 2]; p1k = "ps%d" % (2 + hc % 2)
                    p3 = ps[4 + hc % 2]; p3k = "ps%d" % (4 + hc % 2)
                    for kc in range(8):
                        B.mm(p1[:, :n], w1[i2][:, kc, hc * 128:(hc + 1) * 128], hT[:, kc, t0:t0 + n], kc == 0, kc == 7,
                             ["w1_%d" % i2, self.hk(kc, j)], [p1k])
                    for kc in range(8):
                        B.mm(p3[:, :n], w3[i2][:, kc, hc * 128:(hc + 1) * 128], hT[:, kc, t0:t0 + n], kc == 0, kc == 7,
                             ["w3_%d" % i2, self.hk(kc, j)], [p3k])
                    B.act(s1[hc % 2][:, :n], p1[:, :n], AF.Silu, [p1k], ["s1_%d" % (hc % 2)])
                    B.tt("dve", s2[hc % 2][:, :n], s1[hc % 2][:, :n], p3[:, :n], ALU.mult, ["s1_%d" % (hc % 2), p3k], ["s2_%d" % (hc % 2)])
                    B.tt("dve", g_[:, hc, :n], s2[hc % 2][:, :n], pw[:, :n], ALU.mult, ["s2_%d" % (hc % 2), pwk], ["gb%d_%d" % (j % 2, hc)])
                for o in range(8):
                    py = ps[6 + cnt % 2]; pyk = "ps%d" % (6 + cnt % 2)
                    cnt += 1
                    for hc in range(4):
                        B.mm(py[:, :n], w2[i2][:, hc, o * 128:(o + 1) * 128], g_[:, hc, :n], hc == 0, hc == 3,
                             ["w2_%d" % i2, "gb%d_%d" % (j % 2, hc)], [pyk])
                    m_, k_ = (self.cmodx, "cmodx") if j == 0 else (self.modx, "modx")
                    B.stt(xT[:, o, t0:t0 + n], py[:, :n], m_[:, 40 + o:41 + o], xT[:, o, t0:t0 + n], ALU.mult, ALU.add,
                          [pyk, k_, self.xk(o, j)], [self.xk(o, j)])

    def ssm(self, l):
        B = self
        ps = self.ps; hT = self.hT
        NLV = 12
        self.phase()
        self.ssmT = ssmT = B.sb("ssmT", [128, 4, NT], BF16)
        uT = B.sb("uT", [128, 4, NT], BF16)
        ARn = [B.sb("ARn%d" % d, [128, NLV, 32]) for d in range(2)]
        OFn = [B.sb("OFn%d" % d, [128, NLV, 32]) for d in range(2)]
        BT = [B.sb("BT%d" % d, [128, 4, 128], BF16) for d in range(2)]
        Cst = [B.sb("Cst%d" % d, [128, 512], BF16) for d in range(2)]
        dsk = B.sb("dsk", [128, 4]); bglu = B.sb("bglu", [128, 4])
        main_base = self.sb_ptr
        uk = lambda c, j: "uT%d_%d" % (c, j)
        w_in = self.W("w_in")
        self.vecload(dsk[:, :], self.W("ssm_d")[l, :], "dsk")
        self.vecload(bglu[:, :], self.W("b_glu")[l, :], "bglu")
        wu = [B.sb("wu%d" % i, [128, 8, 128], BF16) for i in range(2)]
        for oc in range(4):
            i2 = oc % 2
            B.dma(WQ, wu[i2][:, :, :], w_in[l, :, 2048 + oc * 128:2048 + (oc + 1) * 128].rearrange("(c p) n -> p c n", p=128), (), ["wu%d" % i2])
            self.proj_fm(wu[i2], "wu%d" % i2, 8, hT, self.hk, lambda t0, n, oc=oc: uT[:, oc, t0:t0 + n], lambda j, oc=oc: uk(oc, j))
        if "u" in self.dbg and l == 0:
            self.dump("d_uT", uT[:, :, :], [128, 4, NT], [uk(c, j) for c in range(4) for j in range(5)], BF16)
        a_re = self.W("ssm_a_re"); a_im = self.W("ssm_a_im"); ldt = self.W("ssm_log_dt")
        b_re = self.W("ssm_b_re"); b_im = self.W("ssm_b_im"); c_re = self.W("ssm_c_re"); c_im = self.W("ssm_c_im")
        anat = B.sb("anat", [32, 128])
        are2 = B.sb("are2", [128, 32]); aim2 = B.sb("aim2", [128, 32]); dt2 = B.sb("dt2", [128, 32])
        zre = B.sb("zre", [128, 32]); zim = B.sb("zim", [128, 32])
        NV = NLV * 32
        zn = B.sb("zn", [128, NLV, 32]); yy = B.sb("yy", [128, NLV, 32]); yi = B.sb("yi", [128, NLV, 32], I32)
        yf = B.sb("yf", [128, NLV, 32]); ff = B.sb("ff", [128, NLV, 32]); sv = B.sb("sv", [128, NLV, 32]); cvv = B.sb("cvv", [128, NLV, 32])
        AIn = B.sb("AIn", [128, NLV, 32])
        cf = B.sb("cf", [128, 12, 32])
        X1 = B.sb("X1", [128, 32, 16]); X2 = B.sb("X2", [128, 32, 16]); Bst = B.sb("Bst", [128, 512]); Btmp = B.sb("Btmp", [128, 512])
        Cnat = B.sb("Cnat", [128, 4, 128])
        fl = lambda t: t[:, :, :].rearrange("p a b -> p (a b)")
        for d in range(2):
            kd = "_%d" % d
            for src_, dst_, kk in ((a_re, are2, "are2"), (a_im, aim2, "aim2")):
                for half in range(2):
                    B.dma("sp", anat[:, half * 64:(half + 1) * 64], src_[l, d, :, :], (), ["anat"])
                B.tr(ps[4][:, 0:32], anat[:, :], self.identf[0:32, 0:32], ["anat", "identf"], ["ps4"])
                B.cp("dve", dst_[:, :], ps[4][:, 0:32], ["ps4"], [kk])
            self.bcast_load(dt2[:, :], ldt[l, d:d + 1, :], 32, "dt2")
            B.act(dt2[:, :], dt2[:, :], AF.Exp, ["dt2"], ["dt2"])
            B.tt("dve", zre[:, :], are2[:, :], dt2[:, :], ALU.mult, ["are2", "dt2"], ["zre"])
            B.tt("dve", zim[:, :], aim2[:, :], dt2[:, :], ALU.mult, ["aim2", "dt2"], ["zim"])
            for i in range(NLV):
                n_ = float(2 ** i)
                B.ts("dve", zn[:, i, :], zre[:, :], n_, None, ALU.mult, None, ["zre"], ["zn"])
                B.ts("dve", yy[:, i, :], zim[:, :], n_ / (2.0 * math.pi), 8.5, ALU.mult, ALU.add, ["zim"], ["yy"])
            B.act(fl(zn), fl(zn), AF.Exp, ["zn"], ["zn"])
            for which, dst in ((0, sv), (1, cvv)):
                if which == 1:
                    B.ts("dve", fl(yy), fl(yy), 0.25, None, ALU.add, None, ["yy"], ["yy"])
                B.cp("dve", fl(yi), fl(yy), ["yy"], ["yi"])
                B.cp("dve", fl(yf), fl(yi), ["yi"], ["yf"])
                B.tt("dve", fl(ff), fl(yy), fl(yf), ALU.subtract, ["yy", "yf"], ["ff"])
                B.stt(fl(ff), fl(ff), 0.0, fl(ff), ALU.is_lt, ALU.add, ["ff"], ["ff"])
                B.ts("dve", fl(ff), fl(ff), 1.0, None, ALU.min, None, ["ff"], ["ff"])
                B.act(fl(dst), fl(ff), AF.Sin, ["ff", "negpi"], ["sc%d" % which], scale=2.0 * math.pi, bias=self.negpi[:, :])
            B.tt("dve", fl(ARn[d]), fl(zn), fl(cvv), ALU.mult, ["zn", "sc1"], ["ARn" + kd])
            B.tt("dve", fl(AIn), fl(zn), fl(sv), ALU.mult, ["zn", "sc0"], ["AIn"])
            B.ts("dve", fl(OFn[d]), fl(AIn), self.sgnf[:, 0:1], None, ALU.mult, None, ["AIn", "sgnf"], ["OFn" + kd])
            a1r = ARn[d][:, 0, :]; a1i = AIn[:, 0, :]
            c_ = lambda i: cf[:, i, :]
            B.ts("dve", c_(0), a1r, -1.0, None, ALU.add, None, ["ARn" + kd], ["cf"])
            B.tt("dve", c_(1), c_(0), are2[:, :], ALU.mult, ["cf", "are2"], ["cf"])
            B.tt("dve", c_(2), a1i, aim2[:, :], ALU.mult, ["AIn", "aim2"], ["cf"])
            B.tt("dve", c_(1), c_(1), c_(2), ALU.add, ["cf"], ["cf"])
            B.tt("dve", c_(3), a1i, are2[:, :], ALU.mult, ["AIn", "are2"], ["cf"])
            B.tt("dve", c_(4), c_(0), aim2[:, :], ALU.mult, ["cf", "aim2"], ["cf"])
            B.tt("dve", c_(3), c_(3), c_(4), ALU.subtract, ["cf"], ["cf"])
            B.tt("dve", c_(5), are2[:, :], are2[:, :], ALU.mult, ["are2"], ["cf"])
            B.tt("dve", c_(6), aim2[:, :], aim2[:, :], ALU.mult, ["aim2"], ["cf"])
            B.tt("dve", c_(5), c_(5), c_(6), ALU.add, ["cf"], ["cf"])
            B.recip(c_(5), c_(5), ["cf"], ["cf"])
            B.tt("dve", c_(7), c_(1), c_(5), ALU.mult, ["cf"], ["cf"])
            B.tt("dve", c_(8), c_(3), c_(5), ALU.mult, ["cf"], ["cf"])
            B.ts("dve", c_(8), c_(8), self.sgnf[:, 1:2], None, ALU.mult, None, ["cf", "sgnf"], ["cf"])
            for q4 in range(4):
                gs = slice(q4 * 8, (q4 + 1) * 8)
                B.dmas("sp", X1[0:64, gs, :], b_re[l, d, gs, :, :].rearrange("g p c -> p g c"), (), ["X1"])
                B.dmas("sp", X1[64:128, gs, :], b_im[l, d, gs, :, :].rearrange("g p c -> p g c"), (), ["X1"])
                B.dmas("sp", X2[0:64, gs, :], b_im[l, d, gs, :, :].rearrange("g p c -> p g c"), (), ["X2"])
                B.dmas("sp", X2[64:128, gs, :], b_re[l, d, gs, :, :].rearrange("g p c -> p g c"), (), ["X2"])
            crb = XAP(cf, 7 * 32, [[12 * 32, 128], [1, 32], [0, 16]])
            cib = XAP(cf, 8 * 32, [[12 * 32, 128], [1, 32], [0, 16]])
            v3 = lambda t: t[:, :].rearrange("p (g c) -> p g c", c=16)
            B.tt("dve", v3(Bst), X1[:, :, :], crb, ALU.mult, ["X1", "cf"], ["Bst"])
            B.tt("dve", v3(Btmp), X2[:, :, :], cib, ALU.mult, ["X2", "cf"], ["Btmp"])
            B.tt("dve", Bst[:, :], Bst[:, :], Btmp[:, :], ALU.add, ["Bst", "Btmp"], ["Bst"])
            for k in range(4):
                B.tr(ps[k % 2][:, 0:128], Bst[:, k * 128:(k + 1) * 128], self.identf[:, :], ["Bst", "identf"], ["ps%d" % (k % 2)])
                B.cp("act", BT[d][:, k, :], ps[k % 2][:, 0:128], ["ps%d" % (k % 2)], ["BT" + kd])
            B.dma("sp", Cnat[:, :, 0:64], c_re[l, d, :, :, :].rearrange("(k a) c p -> (a c) k p", k=4), (), ["Cnat"])
            B.dma("sp", Cnat[:, :, 64:128], c_im[l, d, :, :, :].rearrange("(k a) c p -> (a c) k p", k=4), (), ["Cnat"])
            for k in range(4):
                B.tr(ps[2 + k % 2][:, 0:128], Cnat[:, k, :], self.identf[:, :], ["Cnat", "identf"], ["ps%d" % (2 + k % 2)])
                B.cp("dve", Cst[d][0:64, k * 128:(k + 1) * 128], ps[2 + k % 2][0:64, 0:128], ["ps%d" % (2 + k % 2)], ["Cst" + kd])
                B.act(Cst[d][64:128, k * 128:(k + 1) * 128], ps[2 + k % 2][64:128, 0:128], AF.Copy, ["ps%d" % (2 + k % 2)], ["Cst" + kd], scale=-1.0)
        if "coef" in self.dbg and l == 0:
            self.dump("d_ARn0", ARn[0][:, :, :], [128, NLV, 32], ["ARn_0"]); self.dump("d_OFn0", OFn[0][:, :, :], [128, NLV, 32], ["OFn_0"])
            self.dump("d_Cst0", Cst[0][:, :], [128, 512], ["Cst_0"]); self.dump("d_BT0", BT[0][:, :, :], [128, 4, 128], ["BT_0"], BF16)
        self.P.fence()
        self.sb_ptr = main_base
        NG = 4
        Hb = [B.sb("Hb%d" % s, [128, NT], BF16) for s in range(NG)]
        yacc = B.sb("yacc", [128, NT])
        Cpad = [B.sb("Cpad%d" % d, [128, 8, 128], BF16) for d in range(2)]
        Dm = [[B.sb("Dm%d_%d" % (s, i), [128, 128], BF16) for i in range(2)] for s in range(NG)]
        um = [[B.sb("um%d_%d" % (s, i), [128, 512], BF16) for i in range(1)] for s in range(NG)]
        gt = [B.sb("gt%d" % i, [128, 256]) for i in range(2)]
        for d in range(2):
            B.memset("pool", Cpad[d][:, :, :], 0.0, ["Cpad_%d" % d])
        CH = [(c * 512, min(512, NT - c * 512)) for c in range(5)]

        def pos(d, t0):
            if d == 0:
                return t0
            return t0 - NCTX if t0 >= NCTX else NLAT + t0

        def hkeys(s, a, b):
            return ["H%d_%d" % (s, c) for c in range(a // 512, (b - 1) // 512 + 1)]

        def job(s, k, g8, d):
            g = k * 8 + g8
            H = Hb[s]
            pA = ps[2 * s]; pAk = "ps%d" % (2 * s); pB = ps[2 * s + 1]; pBk = "ps%d" % (2 * s + 1)
            banks = [(pA, pAk), (pB, pBk)]
            ecnt = [0]

            def evac(dst, src, rd, wr):
                ecnt[0] += 1
                B.cp("act" if ecnt[0] % 2 else "dve", dst, src, rd, wr)
            for j, (t0, n) in enumerate(TCH):
                u_ = um[s][0]; umk = "um%d_0" % s
                pb, pk = banks[j % 2]
                B.ts("pool", u_[:, :n], uT[:, k, t0:t0 + n], self.gmaskf[:, g8:g8 + 1], None, ALU.mult, None, [uk(k, j), "gmaskf"], [umk])
                B.mm(pb[:, :n], BT[d][:, k, :], u_[:, :n], True, True, ["BT_%d" % d, umk], [pk])
                p0 = pos(d, t0)
                evac(H[:, p0:p0 + n], pb[:, :n], [pk], hkeys(s, p0, p0 + n))
                yield
            for i in range(NLV):
                sh = 2 ** i
                dm = Dm[s][i % 2]; dmk = "Dm%d_%d" % (s, i % 2)
                B.ts("pool", dm[:, :], self.identf[:, :], ARn[d][:, i, g:g + 1], None, ALU.mult, None, ["identf", "ARn_%d" % d], [dmk])
                B.stt(dm[:, :], self.swpf[:, :], OFn[d][:, i, g:g + 1], dm[:, :], ALU.mult, ALU.add, ["swpf", "OFn_%d" % d, dmk], [dmk])
                order = list(range(4, -1, -1)) if d == 0 else list(range(5))
                for ci, c in enumerate(order):
                    lo, n = CH[c]; hi = lo + n
                    pb, pk = banks[ci % 2]
                    if d == 0:
                        a = max(lo, sh); b = hi
                        has = b > a
                        sa, sb_ = a - sh, b - sh
                    else:
                        a = lo; b = min(hi, NT - sh)
                        has = b > a
                        sa, sb_ = a + sh, b + sh
                    if ci % 2 == 0:
                        B.mm(pb[:, :n], self.identb[:, :], H[:, lo:hi], True, not has, ["identb"] + hkeys(s, lo, hi), [pk])
                        if has:
                            B.mm(pb[:, a - lo:b - lo], dm[:, :], H[:, sa:sb_], False, True, [dmk] + hkeys(s, sa, sb_), [pk], skip_group_check=True)
                        B.cp("act", H[:, lo:hi], pb[:, :n], [pk], hkeys(s, lo, hi))
                    elif has:
                        B.mm(pb[:, a - lo:b - lo], dm[:, :], H[:, sa:sb_], True, True, [dmk] + hkeys(s, sa, sb_), [pk])
                        B.tt("dve", H[:, a:b], H[:, a:b], pb[:, a - lo:b - lo], ALU.add, [pk] + hkeys(s, lo, hi), hkeys(s, lo, hi))
                    yield
            for j, (t0, n) in enumerate(TCH):
                if self.last and j == 0:
                    continue
                pb, pk = banks[j % 2]
                p0 = pos(d, t0)
                B.mm(pb[:, :n], Cpad[d][:, g8, :], H[:, p0:p0 + n], True, True, ["Cpad_%d" % d] + hkeys(s, p0, p0 + n), [pk])
                B.tt("dve", yacc[:, t0:t0 + n], yacc[:, t0:t0 + n], pb[:, :n], ALU.add, ["yacc_%d" % j, pk], ["yacc_%d" % j])
                yield

        for k in range(4):
            for d in range(2):
                B.cp("pool", XAP(Cpad[d], 0, [[1024, 128], [144, 8], [1, 16]]),
                     Cst[d][:, k * 128:(k + 1) * 128].rearrange("p (g c) -> p g c", c=16), ["Cst_%d" % d], ["Cpad_%d" % d])
            for j, (t0, n) in enumerate(TCH):
                if self.last and j == 0:
                    continue
                B.ts("dve", yacc[:, t0:t0 + n], uT[:, k, t0:t0 + n], dsk[:, k:k + 1], None, ALU.mult, None, [uk(k, j), "dsk"], ["yacc_%d" % j])
            jobs = [(g8, d) for g8 in range(8) for d in range(2)]
            for r in range(0, 16, NG):
                gens = [job(s, k, jobs[r + s][0], jobs[r + s][1]) for s in range(NG)]
                alive = list(gens)
                while alive:
                    nxt = []
                    for g_ in alive:
                        try:
                            next(g_)
                            nxt.append(g_)
                        except StopIteration:
                            pass
                    alive = nxt
            if "ssm" in self.dbg and l == 0:
                self.dump("d_yacc%d" % k, yacc[:, :], [128, NT], ["yacc_%d" % j for j in range(5)])
            for j, (t0_, n_) in enumerate(TCH):
                if self.last and j == 0:
                    continue
                for t0 in range(t0_, t0_ + n_, 256):
                    n = 256
                    ya = yacc[:, t0:t0 + n]
                    ga, gb_ = gt[0], gt[1]
                    gak, gbk = "gt0", "gt1"
                    B.act(ga[:, :n], ya, AF.Square, ["yacc_%d" % j], [gak])
                    B.ts("dve", ga[:, :n], ga[:, :n], 0.044715, 1.0, ALU.mult, ALU.add, [gak], [gak])
                    B.tt("pool", ga[:, :n], ga[:, :n], ya, ALU.mult, [gak, "yacc_%d" % j], [gak])
                    B.act(gb_[:, :n], ga[:, :n], AF.Sigmoid, [gak], [gbk], scale=1.5957691216057308)
                    B.tt("dve", uT[:, k, t0:t0 + n], ya, gb_[:, :n], ALU.mult, ["yacc_%d" % j, gbk], [uk(k, j)])
        self.P.fence()
        self.sb_ptr = main_base
        wg = B.sb("wglu", [128, 4, 512], BF16)
        sgl = [B.sb("sgl%d" % i, [128, 512]) for i in range(2)]
        B.dma(WQ, wg[:, :, :], self.W("w_glu")[l, :, :].rearrange("(c p) n -> p c n", p=128), (), ["wglu"])
        cnt = 0
        for oc in range(4):
            for j, (t0, n) in enumerate(TCH):
                if self.last and j == 0:
                    continue
                pb = ps[cnt % 4]; pk = "ps%d" % (cnt % 4)
                sg_ = sgl[cnt % 2]; sgk = "sgl%d" % (cnt % 2)
                cnt += 1
                for kc in range(4):
                    B.mm(pb[:, :n], wg[:, kc, oc * 128:(oc + 1) * 128], uT[:, kc, t0:t0 + n], kc == 0, kc == 3, ["wglu", uk(kc, j)], [pk])
                B.act(sg_[:, :n], pb[:, :n], AF.Sigmoid, [pk, "bglu"], [sgk], bias=bglu[:, oc:oc + 1])
                B.tt("dve", ssmT[:, oc, t0:t0 + n], uT[:, oc, t0:t0 + n], sg_[:, :n], ALU.mult, [uk(oc, j), sgk], ["ss%d_%d" % (oc, j)])
        if "glu" in self.dbg and l == 0:
            self.dump("d_ssmT", ssmT[:, :, :], [128, 4, NT], ["ss%d_%d" % (c, j) for c in range(4) for j in range(5)], BF16)
        self.phase(reserve=4 * NT * 2)


def _consts():
    ident = np.eye(128, dtype=np.float32)
    rmat = np.zeros((128, 128), np.float32)
    for m in range(128):
        d = m % 32
        if d < 16:
            rmat[m + 16, m] = -1.0
        else:
            rmat[m - 16, m] = 1.0
    t = np.arange(NLAT)
    row = (t // 64).astype(np.float32)
    col = (t % 64).astype(np.float32)
    inv = (1.0 / (np.float32(10000.0) ** (np.arange(0, 32, 2, dtype=np.float32) / np.float32(32.0)))).astype(np.float32)
    cos = np.zeros((128, NLAT), np.float32)
    sin = np.zeros((128, NLAT), np.float32)
    for p in range(128):
        d = p % 64
        axis = d // 32
        f = d % 16
        posv = row if axis == 0 else col
        ang = (posv * inv[f]).astype(np.float32)
        cos[p] = np.cos(ang)
        sin[p] = np.sin(ang)
    swp = np.zeros((128, 128), np.float32)
    for k in range(128):
        swp[k, (k + 64) % 128] = 1.0
    gmask = np.zeros((128, 8), np.float32)
    for p in range(128):
        gmask[p, p // 16] = 1.0
    sgn = np.ones((128, 2), np.float32)
    sgn[64:, 0] = -1.0
    sgn[:64, 1] = -1.0
    return dict(ident=ident, rmat=rmat, ropecos=cos, ropesin=sin, swp=swp, gmask=gmask, sgn=sgn)


_CACHE = {}


def _get_prog(n_layers, dbg, skip):
    key = (n_layers, tuple(dbg), tuple(skip))
    if key not in _CACHE:
        b = Builder(n_layers, dbg, skip)
        b.build()
        _CACHE[key] = b
    return _CACHE[key]


def _in_maps(inputs, names, cores):
    f = lambda a: np.ascontiguousarray(np.asarray(a, dtype=np.float32))
    skipk = ("x", "c", "ctx", "c_ctx", "lam_q1", "lam_k1", "lam_q2", "lam_k2")
    shared = {k: f(v) for k, v in inputs.items() if k not in skipk and k in names}
    if "lamv" in names:
        shared["lamv"] = np.ascontiguousarray(np.stack([f(inputs["lam_q1"]), f(inputs["lam_k1"]), f(inputs["lam_q2"]), f(inputs["lam_k2"])], axis=1))
    for k, v in _consts().items():
        if k in names:
            shared[k] = v
    maps = []
    for b in range(cores):
        m = dict(shared)
        m["x"] = f(inputs["x"][b])
        m["ctx"] = f(inputs["ctx"][b])
        m["cvec"] = np.ascontiguousarray(np.stack([f(inputs["c"][b]), f(inputs["c_ctx"])], axis=0))
        maps.append(m)
    return maps


def run(inputs, n_layers=DEPTH, dbg=(), skip=(), cores=8):
    b = _get_prog(n_layers, dbg, skip)
    in_names = set(k for k, v in b.dram.items())
    maps = _in_maps(inputs, in_names, cores)
    maps = [{k: v for k, v in m.items() if k in in_names} for m in maps]
    res = run_bass_kernel_spmd(b.nc, maps, core_ids=list(range(cores)))
    return res.results


def kernel(**inputs):
    res = run(inputs)
    return np.stack([np.asarray(r["out"], dtype=np.float32) for r in res], axis=0)
```
